# Optimizing a Trainium2 kernel written in Bass

```python
import math
import jax, jax.numpy as jnp
from jax import lax
import numpy as np

D_MODEL = 2048
BATCH = 8
SEQ = 2048
DEPTH = 1

D_MIX = D_MODEL
POOL_WIDTH = D_MIX // 2
N_POOL_GROUPS = 4
POOL_GROUP_WIDTH = POOL_WIDTH // N_POOL_GROUPS
POOL_WINDOWS = (2, 4, 8, 16)
ATTN_WIDTH = D_MIX - POOL_WIDTH
HEAD_DIM = 128
N_HEADS = ATTN_WIDTH // HEAD_DIM
N_KV_HEADS = 2
HEAD_GROUP = N_HEADS // N_KV_HEADS
IDX_HEADS = 16
IDX_DIM = 64
TOPK_MAX = 256
ATTN_BLOCK = 128
N_BUCKETS = 32
MAX_DISTANCE = 128
N_GROUPS = 4
EXPERTS_PER_GROUP = 8
N_EXPERTS = N_GROUPS * EXPERTS_PER_GROUP
TOPK_EXPERTS = 2
D_EXPERT = D_MODEL // 4
MOE_BLOCK = 128
EPS = 1e-6
NEG_INF = -1e30
P_IN = (POOL_WIDTH + N_HEADS * HEAD_DIM + 2 * N_KV_HEADS * HEAD_DIM
        + IDX_HEADS * IDX_DIM + IDX_DIM + IDX_HEADS)

kernel_name = "hybrid_pool_dsa_hmoe_block"


def rmsnorm(x, g):
    xf = x.astype(jnp.float32)
    y = xf * lax.rsqrt(jnp.mean(xf * xf, axis=-1, keepdims=True) + EPS)
    return (y * g.astype(jnp.float32)).astype(x.dtype)


def modulate(h, shift, scale):
    return h * (1.0 + scale[:, None, :]) + shift[:, None, :]


def t5_bucket(rel):
    max_exact = N_BUCKETS // 2
    is_small = rel < max_exact
    relf = jnp.maximum(rel, 1).astype(jnp.float32)
    large = max_exact + (jnp.log(relf / max_exact) / math.log(MAX_DISTANCE / max_exact)
                         * (N_BUCKETS - max_exact)).astype(jnp.int32)
    large = jnp.minimum(large, N_BUCKETS - 1)
    return jnp.where(is_small, rel, large)


def pool_mixer(u, w_pool, pool_scale):
    b, s, _ = u.shape
    ug = u.astype(jnp.float32).reshape(b, s, N_POOL_GROUPS, POOL_GROUP_WIDTH)
    cs = jnp.cumsum(ug, axis=1)
    t = jnp.arange(s)
    outs = []
    for g, w in enumerate(POOL_WINDOWS):
        cp = jnp.pad(cs[:, :, g], ((0, 0), (w, 0), (0, 0)))
        win = cp[:, w:w + s] - cp[:, :s]
        cnt = jnp.minimum(t + 1, w).astype(jnp.float32)
        outs.append(win / cnt[None, :, None] - ug[:, :, g])
    p = jnp.stack(outs, axis=2).astype(u.dtype)
    y = jnp.einsum('bsgc,gcd->bsgd', p, w_pool)
    return y.reshape(b, s, POOL_WIDTH) * pool_scale


def dsa_mixer(q, k, v, q_idx, k_idx, w_idx, rel_bias):
    b, s = q.shape[0], q.shape[1]
    topk = min(TOPK_MAX, s // 4)
    nblk = s // ATTN_BLOCK

    def to_blocks(a):
        return a.reshape(b, nblk, ATTN_BLOCK, *a.shape[2:]).swapaxes(0, 1)

    qb, qib, wib = to_blocks(q), to_blocks(q_idx), to_blocks(w_idx)
    kif = k_idx.astype(jnp.float32)
    key_pos = jnp.arange(s)
    idx_scale = (IDX_DIM ** -0.5) * (IDX_HEADS ** -0.5)
    gather = jax.vmap(lambda kb, ib: kb[ib])

    def block_fn(args):
        blk, qblk, qiblk, wiblk = args
        qpos = blk * ATTN_BLOCK + jnp.arange(ATTN_BLOCK)
        dots = jnp.einsum('bqhd,bsd->bqhs', qiblk.astype(jnp.float32), kif)
        score = jnp.einsum('bqh,bqhs->bqs', wiblk.astype(jnp.float32), jax.nn.relu(dots)) * idx_scale
        causal = key_pos[None, :] <= qpos[:, None]
        score = jnp.where(causal[None], score, NEG_INF)
        _, sel = lax.top_k(score, topk)
        valid = sel <= qpos[None, :, None]
        k_sel = gather(k, sel)
        v_sel = gather(v, sel)
        qg = qblk.reshape(b, ATTN_BLOCK, N_KV_HEADS, HEAD_GROUP, HEAD_DIM)
        logits = jnp.einsum('bqhgd,bqjhd->bqhgj', qg, k_sel).astype(jnp.float32) * (HEAD_DIM ** -0.5)
        bucket = t5_bucket(jnp.maximum(qpos[None, :, None] - sel, 0))
        bias = rel_bias.astype(jnp.float32)[bucket]
        bias = bias.reshape(b, ATTN_BLOCK, topk, N_KV_HEADS, HEAD_GROUP).transpose(0, 1, 3, 4, 2)
        logits = jnp.where(valid[:, :, None, None, :], logits + bias, NEG_INF)
        probs = jax.nn.softmax(logits, axis=-1).astype(v_sel.dtype)
        o = jnp.einsum('bqhgj,bqjhd->bqhgd', probs, v_sel)
        return o.reshape(b, ATTN_BLOCK, N_HEADS * HEAD_DIM)

    out = lax.map(block_fn, (jnp.arange(nblk), qb, qib, wib))
    return out.swapaxes(0, 1).reshape(b, s, N_HEADS * HEAD_DIM)


def hier_moe(h, w_group, b_group, w_router, b_router, w_gate, w_up, w_down):
    b, s, d = h.shape
    n_tok = b * s
    ht = h.reshape(n_tok, d)
    glog = (ht @ w_group).astype(jnp.float32) + b_group
    gprob = jax.nn.softmax(glog, axis=-1)
    g_sel = jnp.argmax(glog, axis=-1)
    onehot = jax.nn.one_hot(g_sel, N_GROUPS, dtype=jnp.float32)
    p_g = jnp.sum(gprob * onehot, axis=-1)
    elog = ((ht @ w_router).astype(jnp.float32) + b_router).reshape(n_tok, N_GROUPS, EXPERTS_PER_GROUP)
    elog_sel = jnp.einsum('tg,tge->te', onehot, elog)
    eprob = jax.nn.softmax(elog_sel, axis=-1)
    top_p, top_i = lax.top_k(eprob, TOPK_EXPERTS)
    gates = p_g[:, None] * top_p / jnp.sum(top_p, axis=-1, keepdims=True)
    expert_ids = g_sel[:, None].astype(jnp.int32) * EXPERTS_PER_GROUP + top_i.astype(jnp.int32)

    n_asg = n_tok * TOPK_EXPERTS
    e_flat = expert_ids.reshape(n_asg)
    tok_flat = jnp.repeat(jnp.arange(n_tok, dtype=jnp.int32), TOPK_EXPERTS)
    gate_flat = gates.reshape(n_asg)
    order = jnp.argsort(e_flat)
    e_sorted = e_flat[order]
    counts = jnp.zeros((N_EXPERTS,), jnp.int32).at[e_flat].add(1)
    padded = (counts + MOE_BLOCK - 1) // MOE_BLOCK * MOE_BLOCK
    starts = jnp.cumsum(counts) - counts
    pends = jnp.cumsum(padded)
    pstarts = pends - padded
    dest = pstarts[e_sorted] + (jnp.arange(n_asg, dtype=jnp.int32) - starts[e_sorted])
    n_slots = (-(-n_asg // MOE_BLOCK) + N_EXPERTS) * MOE_BLOCK
    n_blocks = n_slots // MOE_BLOCK
    slot_tok = jnp.zeros((n_slots,), jnp.int32).at[dest].set(tok_flat[order])
    slot_gate = jnp.zeros((n_slots,), jnp.float32).at[dest].set(gate_flat[order])
    block_start = jnp.arange(n_blocks, dtype=jnp.int32) * MOE_BLOCK
    block_exp = jnp.minimum(jnp.searchsorted(pends, block_start, side='right'), N_EXPERTS - 1)
    xs = ht[slot_tok].reshape(n_blocks, MOE_BLOCK, d)

    def expert_block(args):
        xb, e = args
        a = jax.nn.silu(xb @ w_gate[e]) * (xb @ w_up[e])
        return a @ w_down[e]

    ys = lax.map(expert_block, (xs, block_exp)).reshape(n_slots, d)
    out = jnp.zeros((n_tok, d), jnp.float32).at[slot_tok].add(ys.astype(jnp.float32) * slot_gate[:, None])
    return out.astype(h.dtype).reshape(b, s, d)


def setup_inputs(seed: int = 0) -> dict:
    key = jax.random.key(seed)
    ks = jax.random.split(key, 20)
    f32 = jnp.float32
    nrm = lambda k, shape, sc: jax.random.normal(k, shape, f32) * sc
    return {
        "x": nrm(ks[0], (BATCH, SEQ, D_MODEL), 1.0),
        "c": nrm(ks[1], (BATCH, D_MODEL), 1.0),
        "w_ada": nrm(ks[2], (DEPTH, D_MODEL, 6 * D_MODEL), 0.5 * D_MODEL ** -0.5),
        "b_ada": nrm(ks[3], (DEPTH, 6 * D_MODEL), 0.02),
        "g_mix": 1.0 + nrm(ks[4], (DEPTH, D_MODEL), 0.05),
        "w_in": nrm(ks[5], (DEPTH, D_MODEL, P_IN), D_MODEL ** -0.5),
        "w_pool": nrm(ks[6], (DEPTH, N_POOL_GROUPS, POOL_GROUP_WIDTH, POOL_GROUP_WIDTH), POOL_GROUP_WIDTH ** -0.5),
        "pool_scale": 1.0 + nrm(ks[7], (DEPTH, POOL_WIDTH), 0.1),
        "rel_bias": nrm(ks[8], (N_BUCKETS, N_HEADS), 0.5),
        "w_out": nrm(ks[9], (DEPTH, D_MIX, D_MODEL), D_MIX ** -0.5),
        "g_ffn": 1.0 + nrm(ks[10], (DEPTH, D_MODEL), 0.05),
        "w_group": nrm(ks[11], (DEPTH, D_MODEL, N_GROUPS), D_MODEL ** -0.5),
        "b_group": nrm(ks[12], (DEPTH, N_GROUPS), 0.01),
        "w_router": nrm(ks[13], (DEPTH, D_MODEL, N_EXPERTS), D_MODEL ** -0.5),
        "b_router": nrm(ks[14], (DEPTH, N_EXPERTS), 0.01),
        "w_gate": nrm(ks[15], (DEPTH, N_EXPERTS, D_MODEL, D_EXPERT), D_MODEL ** -0.5),
        "w_up": nrm(ks[16], (DEPTH, N_EXPERTS, D_MODEL, D_EXPERT), D_MODEL ** -0.5),
        "w_down": nrm(ks[17], (DEPTH, N_EXPERTS, D_EXPERT, D_MODEL), D_EXPERT ** -0.5),
        "g_final": 1.0 + nrm(ks[18], (D_MODEL,), 0.05),
    }


def reference(x, c, w_ada, b_ada, g_mix, w_in, w_pool, pool_scale, rel_bias, w_out,
              g_ffn, w_group, b_group, w_router, b_router, w_gate, w_up, w_down, g_final):
    b, s, _ = x.shape
    sizes = [POOL_WIDTH, N_HEADS * HEAD_DIM, N_KV_HEADS * HEAD_DIM, N_KV_HEADS * HEAD_DIM,
             IDX_HEADS * IDX_DIM, IDX_DIM]
    cuts = [int(v) for v in np.cumsum(sizes)]
    for i in range(DEPTH):
        mod = jax.nn.silu(c) @ w_ada[i] + b_ada[i]
        sh1, sc1, gt1, sh2, sc2, gt2 = jnp.split(mod, 6, axis=-1)
        h = modulate(rmsnorm(x, g_mix[i]), sh1, sc1)
        proj = h @ w_in[i]
        u, q, k, v, qi, ki, wi = jnp.split(proj, cuts, axis=-1)
        pool_out = pool_mixer(u, w_pool[i], pool_scale[i])
        attn_out = dsa_mixer(q.reshape(b, s, N_HEADS, HEAD_DIM),
                             k.reshape(b, s, N_KV_HEADS, HEAD_DIM),
                             v.reshape(b, s, N_KV_HEADS, HEAD_DIM),
                             qi.reshape(b, s, IDX_HEADS, IDX_DIM), ki, wi, rel_bias)
        mixed = jnp.concatenate([pool_out, attn_out], axis=-1) @ w_out[i]
        x = x + gt1[:, None, :] * mixed
        h2 = modulate(rmsnorm(x, g_ffn[i]), sh2, sc2)
        x = x + gt2[:, None, :] * hier_moe(h2, w_group[i], b_group[i], w_router[i], b_router[i],
                                           w_gate[i], w_up[i], w_down[i])
    return rmsnorm(x, g_final)
```

```python
import numpy as np
import ml_dtypes
import concourse.bass as bass
import concourse.mybir as mybir
from concourse.bass_utils import run_bass_kernel_spmd

F32 = mybir.dt.float32
BF16 = mybir.dt.bfloat16
I32 = mybir.dt.int32
U32 = mybir.dt.uint32
ALU = mybir.AluOpType
AF = mybir.ActivationFunctionType
AX = mybir.AxisListType


class _Op:
    __slots__ = ("eng", "fn", "reads", "writes", "dma", "deps", "signal", "waits",
                 "sigval", "dsem", "dval", "after", "is_bar")

    def __init__(self, eng, fn, reads, writes, dma, after):
        self.eng = eng; self.fn = fn; self.reads = reads; self.writes = writes
        self.dma = dma; self.deps = (); self.signal = False; self.waits = []
        self.sigval = 0; self.dsem = None; self.dval = 0; self.after = after


class Prog:
    ENGS = ("pe", "dve", "act", "pool", "sp")

    def __init__(self, nc):
        self.nc = nc
        self.ops = []
        self.off = 16512
        self.nalloc = 0

    def sb(self, shape, dtype, name=None):
        esz = {F32: 4, BF16: 2, I32: 4, U32: 4}[dtype]
        n = 1
        for s in shape[1:]:
            n *= s
        nbytes = (n * esz + 63) // 64 * 64
        self.nalloc += 1
        name = "%s_%d" % (name or "t", self.nalloc)
        t = self.nc.alloc_sbuf_tensor_at(name, list(shape), dtype, offset=self.off)
        self.off += nbytes
        assert self.off <= 229376, ("sbuf overflow", self.off)
        return t

    def add(self, eng, fn, reads=(), writes=(), dma=False, after=()):
        self.ops.append(_Op(eng, fn, tuple(reads), tuple(writes), dma, tuple(after)))
        return len(self.ops) - 1

    def dma(self, q, out, in_, reads=(), writes=(), **kw):
        return self.add(q, lambda e: e.dma_start(out=out, in_=in_, **kw), reads, writes, dma=True)

    def barrier(self):
        deps = [i for i, o in enumerate(self.ops) if o.dma]
        for e in ("pe", "dve", "act", "pool"):
            for i in range(len(self.ops) - 1, -1, -1):
                o = self.ops[i]
                if o.eng == e and not o.dma and o.fn is not None and not getattr(o, "is_bar", False):
                    deps.append(i)
                    break
        src, dst = self.bar_src, self.bar_dst
        b = self.add("sp", lambda en: en.dma_start(out=dst, in_=src), dma=True, after=deps)
        for e in ("pe", "dve", "act", "pool"):
            self.add(e, None, after=[b])

    def emit(self):
        nc = self.nc
        ops = self.ops
        last_w = {}
        readers = {}
        for i, op in enumerate(ops):
            deps = set(op.after)
            for t in op.reads:
                if t in last_w:
                    deps.add(last_w[t])
            for t in op.writes:
                if t in last_w:
                    deps.add(last_w[t])
                deps.update(readers.get(t, ()))
            for t in op.reads:
                readers.setdefault(t, []).append(i)
            for t in op.writes:
                last_w[t] = i
                readers[t] = []
            deps.discard(i)
            op.deps = deps
        seen = {e: {e2: -1 for e2 in self.ENGS} for e in self.ENGS}
        dma_seen = {e: set() for e in self.ENGS}
        NQ = {"sp": 20, "act": 8, "pool": 16}
        NOUT = {"sp": 20, "act": 8, "pool": 4}
        dma_count = {q: 0 for q in NQ}
        dma_hist = {q: [] for q in NQ}
        for i, op in enumerate(ops):
            E = op.eng
            waits = []
            if op.dma:
                k = dma_count[E]
                dma_count[E] += 1
                dma_hist[E].append(i)
                if k >= NOUT[E]:
                    prev = dma_hist[E][k - NOUT[E]]
                    if prev not in dma_seen[E]:
                        waits.append(("d", prev))
                        dma_seen[E].add(prev)
                op.dsem = (E, k % NQ[E])
                op.dval = 16 * (k // NQ[E] + 1)
            for j in sorted(op.deps):
                oj = ops[j]
                if oj.dma:
                    if j not in dma_seen[E]:
                        dma_seen[E].add(j)
                        waits.append(("d", j))
                else:
                    E2 = oj.eng
                    if E2 == E and E == "pe" and not op.dma:
                        continue
                    if seen[E][E2] >= j:
                        continue
                    seen[E][E2] = j
                    oj.signal = True
                    waits.append(("c", j))
            op.waits = waits
        CH = 1000
        cnt = {e: 0 for e in self.ENGS}
        for op in ops:
            if op.signal:
                op.sigval = (cnt[op.eng] // CH, cnt[op.eng] % CH + 1)
                cnt[op.eng] += 1
        self.stats = dict(cnt)
        self.stats["nops"] = len(ops)
        from contextlib import ExitStack
        with ExitStack() as st:
            csem = {}
            for e in self.ENGS:
                for c in range(cnt[e] // CH + 1):
                    csem[(e, c)] = st.enter_context(nc.semaphore("c_%s_%d" % (e, c)))
            dsem = {}
            for q, n in NQ.items():
                for k in range(n):
                    dsem[(q, k)] = st.enter_context(nc.semaphore("d_%s_%d" % (q, k)))
            block = st.enter_context(nc.Block())

            def run(engname):
                def body(eng):
                    for op in ops:
                        if op.eng != engname:
                            continue
                        for kind, j in op.waits:
                            oj = ops[j]
                            if kind == "d":
                                eng.wait_ge(dsem[oj.dsem], oj.dval)
                            else:
                                eng.wait_ge(csem[(oj.eng, oj.sigval[0])], oj.sigval[1])
                        if op.fn is None:
                            assert not op.signal and not op.dma
                            continue
                        ins = op.fn(eng)
                        if op.dma:
                            ins.then_inc(dsem[op.dsem], 16)
                        elif op.signal:
                            ins.then_inc(csem[(op.eng, op.sigval[0])], 1)
                return body

            block.tensor(run("pe"))
            block.vector(run("dve"))
            block.scalar(run("act"))
            block.gpsimd(run("pool"))
            block.sync(run("sp"))


S = 2048
D = 2048
NT = 16
NBLK = 63
NSLOT = NBLK * 128
SM_SCALE = 128 ** -0.5


def host_consts():
    c = {}
    c["ident_f"] = np.eye(128, dtype=np.float32)
    c["ident_b"] = np.eye(128, dtype=np.float32).astype(ml_dtypes.bfloat16)
    q = np.arange(128)[:, None]; s = np.arange(128)[None, :]
    c["causal_neg"] = np.where(s <= q, 0.0, -1e30).astype(np.float32)
    c["ltri"] = (q < s).astype(np.float32).astype(ml_dtypes.bfloat16)
    c["ones_b"] = np.ones((128, 128), np.float32).astype(ml_dtypes.bfloat16)
    c["ones_f"] = np.ones((128, 128), np.float32)
    c["anti_f"] = np.eye(128, dtype=np.float32)[::-1].copy()
    corr = np.ones((4, 16), np.float32)
    for g, w in enumerate((2, 4, 8, 16)):
        for t in range(16):
            corr[g, t] = w / min(t + 1, w)
    c["corr"] = np.broadcast_to(corr[None], (128, 4, 16)).copy()
    oh = np.zeros((32, 384), np.float32)
    for m in range(383):
        r = max(m - 127, 0)
        if r < 16:
            b = r
        else:
            rf = np.float32(max(r, 1))
            b = 16 + int(np.float32(np.log(rf / np.float32(16)) / np.float32(np.log(8.0)) * np.float32(16)))
            b = min(b, 31)
        oh[b, m] = 1.0
    c["bucket_oh"] = oh
    e1 = np.arange(32)[:, None]; e2 = np.arange(32)[None, :]
    c["u_strict"] = (e1 < e2).astype(np.float32)
    c["u_incl"] = (e1 <= e2).astype(np.float32)
    c["thr16"] = np.broadcast_to((np.arange(16) * 128).astype(np.float32)[None], (32, 16)).copy()
    bg = np.broadcast_to(np.arange(NBLK, dtype=np.float32)[None, :, None], (128, NBLK, 32)).copy()
    c["bgrid"] = bg
    return c


CONST_SPECS = [("ident_f", [128, 128], F32), ("ident_b", [128, 128], BF16), ("causal_neg", [128, 128], F32),
               ("ltri", [128, 128], BF16), ("ones_b", [128, 128], BF16), ("ones_f", [128, 128], F32), ("anti_f", [128, 128], F32),
               ("corr", [128, 4, 16], F32), ("bucket_oh", [32, 384], F32), ("u_strict", [32, 32], F32),
               ("u_incl", [32, 32], F32), ("thr16", [32, 16], F32), ("bgrid", [128, NBLK, 32], F32)]


def w_in_view_(w_in, c0, n):
    return w_in[:, c0:c0 + n].rearrange("(kc p) n -> p kc n", p=128)


def build(stop=None):
    nc = bass.Bass("TRN2", target_bir_lowering=False)

    def din(name, shape, dt=F32):
        return nc.dram_tensor(name, list(shape), dt, kind="ExternalInput").ap()

    def dscratch(name, shape, dt=F32):
        return nc.dram_tensor(name, list(shape), dt, kind="Internal").ap()

    x = din("x", [S, D]); cT = din("cT", [128, 16]); w_ada = din("w_ada", [D, 6 * D]); b_ada = din("b_ada", [1, 6 * D])
    w_kw = din("w_kw", [D, 80])
    g_mixT = din("g_mixT", [128, 16]); w_in = din("w_in", [D, 3664]); w_pool = din("w_pool", [4, 256, 256])
    pool_scaleT = din("pool_scaleT", [128, 8]); rel_bias = din("rel_bias", [32, 8]); w_out = din("w_out", [D, D])
    g_ffn = din("g_ffn", [1, D]); wr = din("wr", [128, 16, 64]); br = din("br", [128, 36])
    NE_DECL = 32 if stop in (None, "E", "F") else 1
    w_gate = [din("w_gate_%d" % j, [NE_DECL * 128, 2048]) for j in range(4)]
    w_up = [din("w_up_%d" % j, [NE_DECL * 128, 2048]) for j in range(4)]
    w_down = [din("w_down_%d" % j, [NE_DECL * 128, 2048]) for j in range(4)]
    g_final = din("g_final", [1, D])
    cst = {n: din("c_" + n, shp, dt) for n, shp, dt in CONST_SPECS}
    out = nc.dram_tensor("out", [S, D], F32, kind="ExternalOutput").ap()
    dbg = {}

    def dbg_out(name, shape, dt=F32):
        dbg[name] = nc.dram_tensor("dbg_" + name, list(shape), dt, kind="ExternalOutput").ap()
        return dbg[name]

    modd = dscratch("modd", [1, 6 * D])
    fextd = dscratch("fextd", [8, 384])
    x1d = dscratch("x1d", [S, D])
    h2d = dscratch("h2d", [S, D], BF16)
    Xs = dscratch("Xs", [NSLOT, D], BF16)
    Ys = dscratch("Ys", [NSLOT, D])

    P = Prog(nc)
    bar_t = P.sb([1, 16], F32, "bar_t")
    P.add("pool", lambda e: e.memset(bar_t[:], 0.0), writes=["bar_t"])
    P.bar_src = bar_t[:]
    P.bar_dst = dscratch("bar_d", [1, 16])
    from contextlib import ExitStack
    st = ExitStack()
    ps = [st.enter_context(nc.psum_tensor("ps%d" % k, [128, 512], F32)) for k in range(6)]
    pb = [st.enter_context(nc.psum_tensor("pb%d" % k, [128, 1024], BF16)) for k in range(2)]
    PS = ["ps%d" % k for k in range(6)]
    PB = ["pb0", "pb1"]

    ident_f = P.sb([128, 128], F32, "ident_f"); ident_b = P.sb([128, 128], BF16, "ident_b")
    causal_neg = P.sb([128, 128], F32, "causal_neg"); ltri = P.sb([128, 128], BF16, "ltri")
    ones_b = P.sb([128, 128], BF16, "ones_b"); ones_f = P.sb([128, 128], F32, "ones_f")
    corr = P.sb([128, 4, 16], F32, "corr")
    for n, t in (("ident_f", ident_f), ("ident_b", ident_b), ("causal_neg", causal_neg), ("ltri", ltri),
                 ("ones_b", ones_b), ("ones_f", ones_f), ("corr", corr)):
        P.dma("sp", t[:], cst[n], writes=[n])
    modT = P.sb([128, 96], F32, "modT")
    s1T = P.sb([128, 16], F32, "s1T")
    gmT = P.sb([128, 16], F32, "gmT")
    pscT = P.sb([128, 8], F32, "pscT")
    eps_t = P.sb([128, 1], F32, "eps")
    P.dma("sp", gmT[:], g_mixT, writes=["gmT"])
    P.dma("sp", pscT[:], pool_scaleT, writes=["pscT"])
    P.add("pool", lambda e: e.memset(eps_t[:], 1e-6), writes=["eps"])
    base_mark = P.off

    scT = P.sb([128, 16], F32, "scT")
    wa = [P.sb([128, 16, 512], F32, "wa%d" % k) for k in range(2)]
    mod_row = P.sb([1, 6 * D], F32, "mod_row")
    ba_row = P.sb([1, 6 * D], F32, "ba_row")
    gf_row = P.sb([1, D], F32, "gf_row")
    P.dma("sp", scT[:], cT, writes=["scT"])
    P.dma("sp", ba_row[:], b_ada, writes=["ba_row"])
    P.dma("sp", gf_row[:], g_ffn, writes=["gf_row"])
    P.add("act", lambda e: e.activation(out=scT[:], in_=scT[:], func=AF.Silu), reads=["scT"], writes=["scT"])
    zt = P.sb([128, D], BF16, "zt")
    P.add("pool", lambda e: e.memset(zt[:], 0.0), writes=["zt"])
    for b in range(NBLK):
        P.dma("pool", Xs[b * 128:(b + 1) * 128, :], zt[:], reads=["zt"], writes=["Xs_zero_%d" % b])
    for n in range(24):
        wt = wa[n % 2]; wn = "wa%d" % (n % 2)
        P.dma("sp" if n % 2 == 0 else "act", wt[:], w_ada[:, n * 512:(n + 1) * 512].rearrange("(kc p) n -> p kc n", p=128), writes=[wn])
        pt = ps[n % 2]; pn = PS[n % 2]
        for kc in range(16):
            P.add("pe", lambda e, pt=pt, wt=wt, kc=kc: e.matmul(pt[0:1, :], lhsT=scT[:, kc:kc + 1], rhs=wt[:, kc, :], start=(kc == 0), stop=(kc == 15)),
                  reads=["scT", wn], writes=[pn])
        P.add("dve", lambda e, pt=pt, n=n: e.tensor_tensor(out=mod_row[0:1, n * 512:(n + 1) * 512], in0=pt[0:1, :], in1=ba_row[0:1, n * 512:(n + 1) * 512], op=ALU.add),
              reads=[pn, "ba_row"], writes=["mod_row"])
    P.add("dve", lambda e: e.scalar_tensor_tensor(out=mod_row[0:1, 4 * D:5 * D], in0=mod_row[0:1, 4 * D:5 * D], scalar=1.0, in1=gf_row[0:1, :], op0=ALU.add, op1=ALU.mult),
          reads=["mod_row", "gf_row"], writes=["mod_row"])
    P.dma("sp", modd, mod_row[:], reads=["mod_row"], writes=["modd"])
    for j in range(96):
        P.add("pe", lambda e, j=j: e.matmul(ps[2][:, j:j + 1], lhsT=mod_row[0:1, j * 128:(j + 1) * 128], rhs=ones_f[0:1, 0:1], start=True, stop=True),
              reads=["mod_row", "ones_f"], writes=[PS[2]])
    P.add("dve", lambda e: e.tensor_copy(out=modT[:], in_=ps[2][:, 0:96]), reads=[PS[2]], writes=["modT"])
    P.add("dve", lambda e: e.scalar_tensor_tensor(out=s1T[:], in0=modT[:, 16:32], scalar=1.0, in1=gmT[:], op0=ALU.add, op1=ALU.mult),
          reads=["modT", "gmT"], writes=["s1T"])
    if stop == "A":
        d = dbg_out("modT", [128, 96])
        P.dma("sp", d, modT[:], reads=["modT"], writes=["dbg"])
        return nc, P, st, dbg
    P.barrier()
    P.off = base_mark

    hT = P.sb([128, 16, S], BF16, "hT")
    proj_mark = P.off
    xs = [P.sb([128, D], F32, "xs%d" % k) for k in range(2)]
    xn = [P.sb([128, D], BF16, "xn%d" % k) for k in range(2)]
    junk = P.sb([128, D], BF16, "junk")
    ss = P.sb([128, 16], F32, "ss"); rt = P.sb([128, 16], F32, "rt"); rstd = P.sb([128, 16], F32, "rstd")
    P.add("pool", lambda e: e.memset(ss[:], 0.0), writes=["ss"])
    for i in range(NT):
        xt = xs[i % 2]; xtn = "xs%d" % (i % 2); xb = xn[i % 2]; xbn = "xn%d" % (i % 2)
        P.dma("sp", xt[:], x[i * 128:(i + 1) * 128, :], writes=[xtn])
        P.add("act", lambda e, xt=xt, i=i: e.activation(out=junk[:], in_=xt[:], func=AF.Square, accum_out=ss[:, i:i + 1]),
              reads=[xtn, "ss"], writes=["junk", "ss"])
        P.add("act", lambda e, i=i: e.activation(out=rt[:, i:i + 1], in_=ss[:, i:i + 1], func=AF.Sqrt, bias=eps_t[:, 0:1], scale=1.0 / D),
              reads=["ss", "eps"], writes=["rt"])
        P.add("dve", lambda e, i=i: e.reciprocal(out=rstd[:, i:i + 1], in_=rt[:, i:i + 1]), reads=["rt"], writes=["rstd"])
        P.add("dve", lambda e, xt=xt, xb=xb, i=i: e.tensor_scalar(out=xb[:], in0=xt[:], scalar1=rstd[:, i:i + 1], scalar2=None, op0=ALU.mult),
              reads=[xtn, "rstd"], writes=[xbn])
        for c in range(16):
            pbt = pb[c // 8]; pbn = PB[c // 8]
            P.add("pe", lambda e, xb=xb, c=c, pbt=pbt: e.transpose(out=pbt[:, (c % 8) * 128:(c % 8 + 1) * 128], in_=xb[:, c * 128:(c + 1) * 128], identity=ident_b[:]),
                  reads=[xbn, "ident_b"], writes=[pbn])
        for c in range(16):
            pbt = pb[c // 8]; pbn = PB[c // 8]
            P.add("act", lambda e, c=c, i=i, pbt=pbt: e.activation(out=hT[:, c, i * 128:(i + 1) * 128], in_=pbt[:, (c % 8) * 128:(c % 8 + 1) * 128], func=AF.Identity,
                                                                   bias=modT[:, c:c + 1], scale=s1T[:, c:c + 1]),
                  reads=[pbn, "modT", "s1T"], writes=["hT_%d" % i])
    HT = ["hT_%d" % i for i in range(NT)]
    if stop == "B1":
        d = dbg_out("hT", [128, 16, S], BF16)
        P.dma("sp", d, hT[:], reads=HT, writes=["dbg"])
        return nc, P, st, dbg
    P.barrier()
    P.off = proj_mark

    uT = P.sb([128, 8, S], BF16, "uT"); qT = P.sb([128, 8, S], BF16, "qT"); qiT = P.sb([128, 8, S], BF16, "qiT")
    kT = P.sb([128, 2, S], BF16, "kT"); vtok = P.sb([128, NT, 256], BF16, "vtok"); kiT2 = P.sb([128, S], BF16, "kiT2")
    wi_all = P.sb([128, NT, 16], F32, "wi_all")
    after_proj_mark = P.off
    wst = [P.sb([128, 16, 272], BF16, "wst%d" % k) for k in range(2)]
    kw_st = P.sb([128, 16, 80], F32, "kw_st")
    P.dma("sp", kw_st[:], w_kw.rearrange("(kc p) n -> p kc n", p=128), writes=["kw_st"])
    evac_n = [0]

    def evac(dst, src, reads, writes, eng=None):
        k = evac_n[0]; evac_n[0] += 1
        if eng == "act" or (eng is None and k % 2 == 0):
            P.add("act", lambda e: e.copy(out=dst, in_=src), reads=reads, writes=writes)
        else:
            P.add("dve", lambda e: e.tensor_copy(out=dst, in_=src), reads=reads, writes=writes)

    def w_in_view(c0, n):
        return w_in_view_(w_in, c0, n)

    groups = []
    for g in range(4):
        groups.append((g * 256, "fm", [(uT, 2 * g), (uT, 2 * g + 1)]))
    for g in range(4):
        groups.append((1024 + g * 256, "fm", [(qT, 2 * g), (qT, 2 * g + 1)]))
    groups.append((2048, "fm", [(kT, 0), (kT, 1)]))
    for g in range(4):
        groups.append((2560 + g * 256, "fm", [(qiT, 2 * g), (qiT, 2 * g + 1)]))
    groups.append((3584, "ki", None))
    groups.append((2304, "tm", None))
    gi = 0
    pk = 0
    for c0, kind, dests in groups:
        if stop == "B2a" and gi == 1:
            d = dbg_out("uT01", [128, 2, S], BF16)
            P.dma("sp", d, uT[:, 0:2, :], reads=[uT.name + "_0", uT.name + "_1"], writes=["dbg_uT01"])
            return nc, P, st, dbg
        if stop == "B2c" and kind == "tm":
            d = dbg_out("kiT2", [128, S], BF16)
            P.dma("sp", d, kiT2[:], reads=["kiT2"], writes=["dbg_kiT2"])
            return nc, P, st, dbg
        if stop == "B2b" and kind == "ki":
            d = dbg_out("qiT", [128, 8, S], BF16)
            P.dma("sp", d, qiT[:], reads=[qiT.name + "_%d" % c for c in range(8)], writes=["dbg_qiT"])
            return nc, P, st, dbg
        wt = wst[gi % 2]; wn = "wst%d" % (gi % 2); gi += 1
        if kind == "fm":
            P.dma("pool", wt[:, :, 0:256], w_in_view(c0, 256), writes=[wn])
            for sub, (dt_, ch) in enumerate(dests):
                for tc in range(4):
                    pt = ps[pk % 4]; pn = PS[pk % 4]; pk += 1
                    for kc in range(16):
                        P.add("pe", lambda e, pt=pt, wt=wt, kc=kc, sub=sub, tc=tc: e.matmul(pt[:], lhsT=wt[:, kc, sub * 128:(sub + 1) * 128], rhs=hT[:, kc, tc * 512:(tc + 1) * 512], start=(kc == 0), stop=(kc == 15)),
                              reads=[wn] + HT[tc * 4:tc * 4 + 4], writes=[pn])
                    evac(dt_[:, ch, tc * 512:(tc + 1) * 512], pt[:], [pn], [dt_.name + "_%d" % ch])
        elif kind == "ki":
            P.add("dve", lambda e, wt=wt: e.tensor_copy(out=wt[:, :, 0:64], in_=kw_st[:, :, 0:64]), reads=["kw_st"], writes=[wn])
            P.add("dve", lambda e, wt=wt: e.tensor_copy(out=wt[:, :, 64:128], in_=kw_st[:, :, 0:64]), reads=["kw_st"], writes=[wn])
            for tc in range(4):
                pt = ps[pk % 4]; pn = PS[pk % 4]; pk += 1
                for kc in range(16):
                    P.add("pe", lambda e, pt=pt, wt=wt, kc=kc, tc=tc: e.matmul(pt[:], lhsT=wt[:, kc, 0:128], rhs=hT[:, kc, tc * 512:(tc + 1) * 512], start=(kc == 0), stop=(kc == 15)),
                          reads=[wn] + HT[tc * 4:tc * 4 + 4], writes=[pn])
                evac(kiT2[:, tc * 512:(tc + 1) * 512], pt[:], [pn], ["kiT2"])
        else:
            P.dma("pool", wt[:, :, 0:256], w_in_view(2304, 256), writes=[wn])
            P.add("dve", lambda e, wt=wt: e.tensor_copy(out=wt[:, :, 256:272], in_=kw_st[:, :, 64:80]), reads=["kw_st"], writes=[wn])
            for i in range(NT):
                pt = ps[pk % 4]; pn = PS[pk % 4]; pk += 1
                for kc in range(16):
                    P.add("pe", lambda e, pt=pt, wt=wt, kc=kc, i=i: e.matmul(pt[:, 0:256], lhsT=hT[:, kc, i * 128:(i + 1) * 128], rhs=wt[:, kc, 0:256], start=(kc == 0), stop=(kc == 15)),
                          reads=[wn, HT[i]], writes=[pn])
                P.add("act", lambda e, pt=pt, i=i: e.copy(out=vtok[:, i, :], in_=pt[:, 0:256]), reads=[pn], writes=["vtok"])
                pt2 = ps[4 + i % 2]; pn2 = PS[4 + i % 2]
                for kc in range(16):
                    P.add("pe", lambda e, pt2=pt2, wt=wt, kc=kc, i=i: e.matmul(pt2[:, 0:16], lhsT=hT[:, kc, i * 128:(i + 1) * 128], rhs=wt[:, kc, 256:272], start=(kc == 0), stop=(kc == 15)),
                          reads=[wn, HT[i]], writes=[pn2])
                P.add("dve", lambda e, pt2=pt2, i=i: e.tensor_copy(out=wi_all[:, i, :], in_=pt2[:, 0:16]), reads=[pn2], writes=["wi_all"])
    UT = [uT.name + "_%d" % c for c in range(8)]; QT = [qT.name + "_%d" % c for c in range(8)]
    QIT = [qiT.name + "_%d" % c for c in range(8)]; KT = [kT.name + "_%d" % c for c in range(2)]
    if stop == "B2":
        for nm, t, toks, shp, dt_ in (("uT", uT, UT, [128, 8, S], BF16), ("qT", qT, QT, [128, 8, S], BF16), ("qiT", qiT, QIT, [128, 8, S], BF16),
                                      ("kT", kT, KT, [128, 2, S], BF16), ("vtok", vtok, ["vtok"], [128, NT, 256], BF16),
                                      ("kiT2", kiT2, ["kiT2"], [128, S], BF16), ("wi_all", wi_all, ["wi_all"], [128, NT, 16], F32)):
            d = dbg_out(nm, shp, dt_)
            P.dma("sp", d, t[:], reads=toks, writes=["dbg_" + nm])
        return nc, P, st, dbg
    P.barrier()
    P.off = base_mark
    pool_outT = P.sb([128, 8, S], BF16, "pool_outT"); attn_outT = P.sb([128, 8, S], BF16, "attn_outT")
    POT = ["pool_outT_%d" % c for c in range(8)]; AOT = ["attn_outT_%d" % i for i in range(NT)]
    P.off = after_proj_mark
    A_ = P.sb([128, S], F32, "poolA"); B_ = P.sb([128, S], F32, "poolB"); wp = P.sb([128, 4, 2, 256], BF16, "wp")
    P.dma("pool", wp[:], w_pool.rearrange("g (cc p) d -> p g cc d", p=128), writes=["wp"])
    for c in range(8):
        g = c // 2; w = 2 << g
        u = uT[:, c, :]; un = UT[c]
        P.add("dve", lambda e, u=u: e.tensor_tensor(out=A_[:, 1:S], in0=u[:, 1:S], in1=u[:, 0:S - 1], op=ALU.add), reads=[un], writes=["poolA"])
        P.add("dve", lambda e, u=u: e.tensor_copy(out=A_[:, 0:1], in_=u[:, 0:1]), reads=[un], writes=["poolA"])
        cur, curn, oth, othn = A_, "poolA", B_, "poolB"
        d = 2
        while d < w:
            P.add("dve", lambda e, cur=cur, oth=oth, d=d: e.tensor_tensor(out=oth[:, d:S], in0=cur[:, d:S], in1=cur[:, 0:S - d], op=ALU.add), reads=[curn], writes=[othn])
            P.add("dve", lambda e, cur=cur, oth=oth, d=d: e.tensor_copy(out=oth[:, 0:d], in_=cur[:, 0:d]), reads=[curn], writes=[othn])
            cur, curn, oth, othn = oth, othn, cur, curn
            d *= 2
        P.add("dve", lambda e, cur=cur, g=g: e.tensor_tensor(out=cur[:, 0:16], in0=cur[:, 0:16], in1=corr[:, g, :], op=ALU.mult), reads=[curn, "corr"], writes=[curn])
        P.add("dve", lambda e, cur=cur, u=u, w=w: e.scalar_tensor_tensor(out=u, in0=cur[:], scalar=1.0 / w, in1=u, op0=ALU.mult, op1=ALU.subtract), reads=[curn, un], writes=[un])
        if c % 2 == 1:
            for dch in range(2):
                for tc in range(4):
                    pt = ps[pk % 4]; pn = PS[pk % 4]; pk += 1
                    for cc in range(2):
                        P.add("pe", lambda e, pt=pt, g=g, cc=cc, dch=dch, tc=tc: e.matmul(pt[:], lhsT=wp[:, g, cc, dch * 128:(dch + 1) * 128], rhs=uT[:, 2 * g + cc, tc * 512:(tc + 1) * 512], start=(cc == 0), stop=(cc == 1)),
                              reads=["wp", UT[2 * g], UT[2 * g + 1]], writes=[pn])
                    P.add("act", lambda e, pt=pt, g=g, dch=dch, tc=tc: e.activation(out=pool_outT[:, 2 * g + dch, tc * 512:(tc + 1) * 512], in_=pt[:], func=AF.Identity, scale=pscT[:, 2 * g + dch:2 * g + dch + 1]),
                          reads=[pn, "pscT"], writes=[POT[2 * g + dch]])
    if stop == "B3":
        d = dbg_out("pool_outT", [128, 8, S], BF16)
        P.dma("sp", d, pool_outT[:], reads=POT, writes=["dbg_pool_outT"])
        return nc, P, st, dbg
    P.barrier()

    P.off = proj_mark
    acc = P.sb([128, S], F32, "acc"); maskq = P.sb([128, S], BF16, "maskq"); junkb = P.sb([128, S], BF16, "junkb")
    maskT = P.sb([128, NT, 128], BF16, "maskT"); EB3 = P.sb([128, 8, 384], F32, "EB3")
    assert P.off == proj_mark + 32768
    P.off = after_proj_mark
    rtile = [P.sb([128, 512], F32, "rtile%d" % k) for k in range(2)]
    Et = [P.sb([128, 512], BF16, "Et%d" % k) for k in range(2)]
    P1 = [P.sb([128, 512], BF16, "P1%d" % k) for k in range(2)]
    Pt = [P.sb([128, 512], BF16, "Pt%d" % k) for k in range(2)]
    rinv = P.sb([128, 512], F32, "rinv")
    lo = P.sb([128, 1], F32, "lo"); hi = P.sb([128, 1], F32, "hi"); mid = P.sb([128, 1], F32, "mid"); cnt = P.sb([128, 1], F32, "cnt")
    ge = P.sb([128, 1], U32, "ge"); lt = P.sb([128, 1], U32, "lt")
    relb = P.sb([32, 8], F32, "relb"); boh = P.sb([32, 384], F32, "boh"); fext = P.sb([8, 384], F32, "fext")
    hank = P.sb([128, 256], F32, "hank"); anti = P.sb([128, 128], F32, "anti")
    P.dma("sp", relb[:], rel_bias, writes=["relb"]); P.dma("sp", boh[:], cst["bucket_oh"], writes=["boh"])
    P.dma("sp", anti[:], cst["anti_f"], writes=["anti"])
    P.add("pe", lambda e: e.matmul(ps[0][0:8, 0:384], lhsT=relb[:], rhs=boh[:], start=True, stop=True), reads=["relb", "boh"], writes=[PS[0]])
    P.add("dve", lambda e: e.tensor_copy(out=fext[:], in_=ps[0][0:8, 0:384]), reads=[PS[0]], writes=["fext"])
    P.dma("sp", fextd, fext[:], reads=["fext"], writes=["fextd"])
    for h in range(8):
        P.dma("sp", hank[:], bass.AP(fextd.tensor, h * 384, [[1, 128], [1, 256]]), reads=["fextd"], writes=["hank"])
        P.add("pe", lambda e: e.matmul(ps[1][:, 0:256], lhsT=anti[:], rhs=hank[:], start=True, stop=True), reads=["anti", "hank"], writes=[PS[1]])
        P.add("act", lambda e, h=h: e.activation(out=EB3[:, h, 0:256], in_=ps[1][:, 0:256], func=AF.Exp), reads=[PS[1]], writes=["EB3"])
        P.add("act", lambda e, h=h: e.activation(out=EB3[:, h, 256:384], in_=ps[1][:, 255:256].to_broadcast([128, 128]), func=AF.Exp), reads=[PS[1]], writes=["EB3"])
    NIT = 24
    ek = 0
    for i in range(NT):
        nk = (i + 1) * 128
        nch = (nk + 511) // 512
        for h in range(16):
            pr, hh = h // 2, h % 2
            b0 = 64 * hh
            for kc in range(nch):
                wd = min(512, nk - kc * 512)
                pt = ps[ek % 2]; pn = PS[ek % 2]; rtl = rtile[ek % 2]; rn = "rtile%d" % (ek % 2); ek += 1
                P.add("pe", lambda e, pt=pt, pr=pr, b0=b0, i=i, kc=kc, wd=wd: e.matmul(pt[:, 0:wd], lhsT=qiT[b0:b0 + 64, pr, i * 128:(i + 1) * 128], rhs=kiT2[b0:b0 + 64, kc * 512:kc * 512 + wd], start=True, stop=True),
                      reads=[QIT[pr], "kiT2"], writes=[pn])
                P.add("act", lambda e, pt=pt, rtl=rtl, wd=wd: e.activation(out=rtl[:, 0:wd], in_=pt[:, 0:wd], func=AF.Relu), reads=[pn], writes=[rn])
                if h == 0:
                    P.add("dve", lambda e, rtl=rtl, kc=kc, wd=wd, i=i, h=h: e.tensor_scalar(out=acc[:, kc * 512:kc * 512 + wd], in0=rtl[:, 0:wd], scalar1=wi_all[:, i, h:h + 1], scalar2=None, op0=ALU.mult),
                          reads=[rn, "wi_all"], writes=["acc"])
                else:
                    P.add("dve", lambda e, rtl=rtl, kc=kc, wd=wd, i=i, h=h: e.scalar_tensor_tensor(out=acc[:, kc * 512:kc * 512 + wd], in0=rtl[:, 0:wd], scalar=wi_all[:, i, h:h + 1], in1=acc[:, kc * 512:kc * 512 + wd], op0=ALU.mult, op1=ALU.add),
                          reads=[rn, "wi_all", "acc"], writes=["acc"])
        P.add("dve", lambda e, i=i: e.tensor_tensor(out=acc[:, i * 128:(i + 1) * 128], in0=acc[:, i * 128:(i + 1) * 128], in1=causal_neg[:], op=ALU.add), reads=["acc", "causal_neg"], writes=["acc"])
        if i >= 2:
            P.add("dve", lambda e, nk=nk: e.tensor_reduce(out=hi[:], in_=acc[:, 0:nk], axis=AX.X, op=ALU.max), reads=["acc"], writes=["hi"])
            P.add("dve", lambda e, i=i: e.tensor_reduce(out=lo[:], in_=acc[:, 0:i * 128], axis=AX.X, op=ALU.min), reads=["acc"], writes=["lo"])
            for it in range(NIT):
                P.add("dve", lambda e: e.tensor_scalar(out=mid[:], in0=lo[:], scalar1=hi[:, 0:1], scalar2=0.5, op0=ALU.add, op1=ALU.mult), reads=["lo", "hi"], writes=["mid"])
                P.add("dve", lambda e, nk=nk: e.tensor_scalar(out=junkb[:, 0:nk], in0=acc[:, 0:nk], scalar1=mid[:, 0:1], scalar2=None, op0=ALU.is_ge, op1=ALU.add, accum_out=cnt[:, 0:1]),
                      reads=["acc", "mid"], writes=["junkb", "cnt"])
                P.add("dve", lambda e: e.tensor_scalar(out=ge[:], in0=cnt[:], scalar1=255.5, scalar2=None, op0=ALU.is_ge), reads=["cnt"], writes=["ge"])
                P.add("dve", lambda e: e.tensor_scalar(out=lt[:], in0=cnt[:], scalar1=255.5, scalar2=None, op0=ALU.is_lt), reads=["cnt"], writes=["lt"])
                P.add("dve", lambda e: e.copy_predicated(out=lo[:], mask=ge[:], data=mid[:]), reads=["ge", "mid", "lo"], writes=["lo"])
                P.add("dve", lambda e: e.copy_predicated(out=hi[:], mask=lt[:], data=mid[:]), reads=["lt", "mid", "hi"], writes=["hi"])
            P.add("dve", lambda e, nk=nk: e.tensor_scalar(out=maskq[:, 0:nk], in0=acc[:, 0:nk], scalar1=lo[:, 0:1], scalar2=None, op0=ALU.is_ge), reads=["acc", "lo"], writes=["maskq"])
        else:
            P.add("dve", lambda e, nk=nk: e.tensor_scalar(out=maskq[:, 0:nk], in0=acc[:, 0:nk], scalar1=-1e29, scalar2=None, op0=ALU.is_ge), reads=["acc"], writes=["maskq"])
        for j0 in range(0, i + 1, 8):
            jj = list(range(j0, min(i + 1, j0 + 8)))
            pbt = pb[(j0 // 8) % 2]; pbn = PB[(j0 // 8) % 2]
            for j in jj:
                P.add("pe", lambda e, j=j, pbt=pbt: e.transpose(out=pbt[:, (j % 8) * 128:(j % 8 + 1) * 128], in_=maskq[:, j * 128:(j + 1) * 128], identity=ident_b[:]), reads=["maskq", "ident_b"], writes=[pbn])
            for j in jj:
                P.add("act", lambda e, j=j, pbt=pbt: e.copy(out=maskT[:, j, :], in_=pbt[:, (j % 8) * 128:(j % 8 + 1) * 128]), reads=[pbn], writes=["maskT"])
        for kv in range(2):
            for j in range(i + 1):
                off = 0 if j == i else (128 if j == i - 1 else 256)
                pS = ps[2 + ek % 2]; pSn = PS[2 + ek % 2]
                et = Et[ek % 2]; etn = "Et%d" % (ek % 2); p1 = P1[ek % 2]; p1n = "P1%d" % (ek % 2); ptt = Pt[ek % 2]; ptn = "Pt%d" % (ek % 2); ek += 1
                P.add("pe", lambda e, pS=pS, kv=kv, j=j, i=i: e.matmul(pS[:], lhsT=kT[:, kv, j * 128:(j + 1) * 128], rhs=qT[:, 4 * kv:4 * kv + 4, i * 128:(i + 1) * 128], start=True, stop=True),
                      reads=[KT[kv]] + QT[4 * kv:4 * kv + 4], writes=[pSn])
                P.add("act", lambda e, pS=pS, et=et: e.activation(out=et[:], in_=pS[:], func=AF.Exp, scale=SM_SCALE), reads=[pSn], writes=[etn])
                P.add("pool", lambda e, et=et, p1=p1, j=j: e.tensor_tensor(out=p1[:].rearrange("p (h q) -> p h q", h=4), in0=et[:].rearrange("p (h q) -> p h q", h=4), in1=maskT[:, j:j + 1, :].to_broadcast([128, 4, 128]), op=ALU.mult),
                      reads=[etn, "maskT"], writes=[p1n])
                P.add("dve", lambda e, p1=p1, ptt=ptt, kv=kv, off=off: e.tensor_tensor(out=ptt[:].rearrange("p (h q) -> p h q", h=4), in0=p1[:].rearrange("p (h q) -> p h q", h=4), in1=EB3[:, 4 * kv:4 * kv + 4, off:off + 128], op=ALU.mult),
                      reads=[p1n, "EB3"], writes=[ptn])
                P.add("pe", lambda e, ptt=ptt, kv=kv, j=j, i=i: e.matmul(ps[4][:], lhsT=vtok[:, j, kv * 128:(kv + 1) * 128], rhs=ptt[:], start=(j == 0), stop=(j == i)), reads=["vtok", ptn], writes=[PS[4]])
                P.add("pe", lambda e, ptt=ptt, j=j, i=i: e.matmul(ps[5][:], lhsT=ones_b[:], rhs=ptt[:], start=(j == 0), stop=(j == i)), reads=["ones_b", ptn], writes=[PS[5]])
            P.add("dve", lambda e: e.reciprocal(out=rinv[:], in_=ps[5][:]), reads=[PS[5]], writes=["rinv"])
            P.add("dve", lambda e, kv=kv, i=i: e.tensor_tensor(out=attn_outT[:, 4 * kv:4 * kv + 4, i * 128:(i + 1) * 128], in0=ps[4][:].rearrange("p (h q) -> p h q", h=4), in1=rinv[:].rearrange("p (h q) -> p h q", h=4), op=ALU.mult),
                  reads=[PS[4], "rinv"], writes=[AOT[i]])
        if stop == "B4a" and i == 3:
            d = dbg_out("acc", [128, 512]); P.dma("sp", d, acc[:, 0:512], reads=["acc"], writes=["dbg_acc"])
            d = dbg_out("maskq", [128, 512], BF16); P.dma("sp", d, maskq[:, 0:512], reads=["maskq"], writes=["dbg_maskq"])
            d = dbg_out("thr", [128, 1]); P.dma("sp", d, lo[:], reads=["lo"], writes=["dbg_thr"])
            d = dbg_out("EB3", [128, 8, 384]); P.dma("sp", d, EB3[:], reads=["EB3"], writes=["dbg_EB3"])
            d = dbg_out("attn_outT", [128, 8, 512], BF16); P.dma("sp", d, attn_outT[:, :, 0:512], reads=AOT[0:4], writes=["dbg_attn_outT"])
            return nc, P, st, dbg
    if stop == "B4":
        d = dbg_out("attn_outT", [128, 8, S], BF16)
        P.dma("sp", d, attn_outT[:], reads=AOT, writes=["dbg_attn_outT"])
        return nc, P, st, dbg
    P.barrier()
    P.off = proj_mark
    O1 = P.sb([128, NT, 32], BF16, "O1"); O2 = P.sb([128, NT, 32], BF16, "O2"); Osum = P.sb([128, NT, 32], BF16, "Osum")
    g1 = P.sb([128, NT], F32, "g1"); g2 = P.sb([128, NT], F32, "g2")
    dest_i = P.sb([128, NT, 2], U32, "dest_i")
    blk_i = P.sb([128, NBLK], I32, "blk_i"); act_i = P.sb([128, NBLK], I32, "act_i"); widx = P.sb([128, NBLK], U32, "widx")
    route_mark = P.off
    wo = P.sb([128, 16, D], BF16, "wo")
    xt2 = [P.sb([128, D], F32, "xt2_%d" % k) for k in range(2)]
    gt1_bc = P.sb([128, D], F32, "gt1_bc"); s2_bc = P.sb([128, D], F32, "s2_bc"); sh2_bc = P.sb([128, D], F32, "sh2_bc")
    h2 = P.sb([128, D], F32, "h2"); h2b = P.sb([128, D], BF16, "h2b"); h2T = P.sb([128, 16, 128], F32, "h2T")
    wr_sb = P.sb([128, 16, 64], F32, "wr_sb"); br_bc = P.sb([128, 36], F32, "br_bc")
    ss2 = P.sb([128, NT], F32, "ss2"); rt2 = P.sb([128, NT], F32, "rt2"); rstd2 = P.sb([128, NT], F32, "rstd2")
    L_all = P.sb([128, NT, 36], F32, "L_all")
    for n in range(4):
        P.dma("pool", wo[:, :, n * 512:(n + 1) * 512], w_out[:, n * 512:(n + 1) * 512].rearrange("(kc p) n -> p kc n", p=128), writes=["wo"])
    P.dma("sp", gt1_bc[:], modd[0:1, 2 * D:3 * D].to_broadcast([128, D]), reads=["modd"], writes=["gt1_bc"])
    P.dma("sp", sh2_bc[:], modd[0:1, 3 * D:4 * D].to_broadcast([128, D]), reads=["modd"], writes=["sh2_bc"])
    P.dma("sp", s2_bc[:], modd[0:1, 4 * D:5 * D].to_broadcast([128, D]), reads=["modd"], writes=["s2_bc"])
    P.dma("sp", wr_sb[:], wr, writes=["wr_sb"])
    P.dma("sp", br_bc[:], br, writes=["br_bc"])
    P.add("pool", lambda e: e.memset(ss2[:], 0.0), writes=["ss2"])

    for i in range(NT):
        xt = xt2[i % 2]; xtn = "xt2_%d" % (i % 2)
        P.dma("sp", xt[:], x[i * 128:(i + 1) * 128, :], writes=[xtn])
        for n in range(4):
            for kc in range(16):
                src = pool_outT if kc < 8 else attn_outT
                srcn = POT[kc] if kc < 8 else AOT[i]
                P.add("pe", lambda e, n=n, kc=kc, src=src, i=i: e.matmul(ps[n][:], lhsT=src[:, kc % 8, i * 128:(i + 1) * 128], rhs=wo[:, kc, n * 512:(n + 1) * 512], start=(kc == 0), stop=(kc == 15)),
                      reads=[srcn, "wo"], writes=[PS[n]])
            P.add("dve", lambda e, n=n: e.tensor_tensor(out=h2[:, n * 512:(n + 1) * 512], in0=ps[n][:], in1=gt1_bc[:, n * 512:(n + 1) * 512], op=ALU.mult), reads=[PS[n], "gt1_bc"], writes=["h2"])
            P.add("pool", lambda e, n=n, xt=xt: e.tensor_tensor(out=xt[:, n * 512:(n + 1) * 512], in0=xt[:, n * 512:(n + 1) * 512], in1=h2[:, n * 512:(n + 1) * 512], op=ALU.add), reads=[xtn, "h2"], writes=[xtn])
        P.dma("sp", x1d[i * 128:(i + 1) * 128, :], xt[:], reads=[xtn], writes=["x1d_%d" % i])
        P.add("act", lambda e, xt=xt, i=i: e.activation(out=h2b[:], in_=xt[:], func=AF.Square, accum_out=ss2[:, i:i + 1]), reads=[xtn, "ss2"], writes=["h2b", "ss2"])
        P.add("act", lambda e, i=i: e.activation(out=rt2[:, i:i + 1], in_=ss2[:, i:i + 1], func=AF.Sqrt, bias=eps_t[:, 0:1], scale=1.0 / D), reads=["ss2", "eps"], writes=["rt2"])
        P.add("dve", lambda e, i=i: e.reciprocal(out=rstd2[:, i:i + 1], in_=rt2[:, i:i + 1]), reads=["rt2"], writes=["rstd2"])
        P.add("dve", lambda e, xt=xt, i=i: e.scalar_tensor_tensor(out=h2[:], in0=xt[:], scalar=rstd2[:, i:i + 1], in1=s2_bc[:], op0=ALU.mult, op1=ALU.mult), reads=[xtn, "rstd2", "s2_bc", "h2"], writes=["h2"])
        P.add("pool", lambda e: e.tensor_tensor(out=h2[:], in0=h2[:], in1=sh2_bc[:], op=ALU.add), reads=["h2", "sh2_bc"], writes=["h2"])
        P.add("act", lambda e: e.copy(out=h2b[:], in_=h2[:]), reads=["h2"], writes=["h2b"])
        P.dma("sp", h2d[i * 128:(i + 1) * 128, :], h2b[:], reads=["h2b"], writes=["h2d_%d" % i])
        for c0 in range(0, 16, 4):
            for c in range(c0, c0 + 4):
                P.add("pe", lambda e, c=c: e.transpose(out=ps[4][:, (c % 4) * 128:(c % 4 + 1) * 128], in_=h2[:, c * 128:(c + 1) * 128], identity=ident_f[:]), reads=["h2", "ident_f"], writes=[PS[4]])
            for c in range(c0, c0 + 4):
                evac(h2T[:, c, :], ps[4][:, (c % 4) * 128:(c % 4 + 1) * 128], [PS[4]], ["h2T"], eng=("act" if (c0 // 4) % 2 == 0 else "dve"))
        for c in range(16):
            P.add("pe", lambda e, c=c: e.matmul(ps[5][:, 0:64], lhsT=h2T[:, c, :], rhs=wr_sb[:, c, :], start=(c == 0), stop=(c == 15)), reads=["h2T", "wr_sb"], writes=[PS[5]])
        P.add("dve", lambda e, i=i: e.tensor_tensor(out=L_all[:, i, :], in0=ps[5][:, 0:36], in1=br_bc[:], op=ALU.add), reads=[PS[5], "br_bc"], writes=["L_all"])
    def bc(t, k):
        return t[:].unsqueeze(2).to_broadcast([128, NT, k])
    R = {n: P.sb([128, NT], F32, "R_" + n) for n in ("gmax", "gsum", "pg", "m1", "m2", "dd", "e2", "den", "rden")}
    R3 = {n: P.sb([128, NT, k], F32, "R3_" + n) for n, k in (("og", 4), ("eg", 4), ("es", 8), ("tmp", 8), ("o1", 8), ("es2", 8), ("o2", 8))}
    Lg = L_all[:, :, 0:4]
    def rop(eng, fn, reads, writes):
        P.add(eng, fn, reads=reads, writes=writes)
    rop("dve", lambda e: e.tensor_reduce(out=R["gmax"][:], in_=Lg, axis=AX.X, op=ALU.max), ["L_all"], ["gmax"])
    rop("dve", lambda e: e.tensor_tensor(out=R3["og"][:], in0=Lg, in1=bc(R["gmax"], 4), op=ALU.is_ge), ["L_all", "gmax"], ["og"])
    rop("dve", lambda e: e.tensor_tensor(out=R3["eg"][:], in0=Lg, in1=bc(R["gmax"], 4), op=ALU.subtract), ["L_all", "gmax"], ["eg"])
    rop("act", lambda e: e.activation(out=R3["eg"][:], in_=R3["eg"][:], func=AF.Exp), ["eg"], ["eg"])
    rop("dve", lambda e: e.tensor_reduce(out=R["gsum"][:], in_=R3["eg"][:], axis=AX.X, op=ALU.add), ["eg"], ["gsum"])
    rop("dve", lambda e: e.reciprocal(out=R["pg"][:], in_=R["gsum"][:]), ["gsum"], ["pg"])
    rop("dve", lambda e: e.tensor_tensor(out=R3["es"][:], in0=L_all[:, :, 4:12], in1=R3["og"][:, :, 0:1].to_broadcast([128, NT, 8]), op=ALU.mult), ["L_all", "og"], ["es"])
    for g in range(1, 4):
        rop("dve", lambda e, g=g: e.tensor_tensor(out=R3["tmp"][:], in0=L_all[:, :, 4 + 8 * g:12 + 8 * g], in1=R3["og"][:, :, g:g + 1].to_broadcast([128, NT, 8]), op=ALU.mult), ["L_all", "og"], ["tmp"])
        rop("dve", lambda e: e.tensor_tensor(out=R3["es"][:], in0=R3["es"][:], in1=R3["tmp"][:], op=ALU.add), ["es", "tmp"], ["es"])
    rop("dve", lambda e: e.tensor_reduce(out=R["m1"][:], in_=R3["es"][:], axis=AX.X, op=ALU.max), ["es"], ["m1"])
    rop("dve", lambda e: e.tensor_tensor(out=R3["o1"][:], in0=R3["es"][:], in1=bc(R["m1"], 8), op=ALU.is_ge), ["es", "m1"], ["o1"])
    rop("dve", lambda e: e.scalar_tensor_tensor(out=R3["es2"][:], in0=R3["o1"][:], scalar=-1e30, in1=R3["es"][:], op0=ALU.mult, op1=ALU.add), ["o1", "es"], ["es2"])
    rop("dve", lambda e: e.tensor_reduce(out=R["m2"][:], in_=R3["es2"][:], axis=AX.X, op=ALU.max), ["es2"], ["m2"])
    rop("dve", lambda e: e.tensor_tensor(out=R3["o2"][:], in0=R3["es2"][:], in1=bc(R["m2"], 8), op=ALU.is_ge), ["es2", "m2"], ["o2"])
    rop("dve", lambda e: e.tensor_tensor(out=R["dd"][:], in0=R["m2"][:], in1=R["m1"][:], op=ALU.subtract), ["m1", "m2"], ["dd"])
    rop("act", lambda e: e.activation(out=R["e2"][:], in_=R["dd"][:], func=AF.Exp), ["dd"], ["e2"])
    rop("dve", lambda e: e.tensor_scalar(out=R["den"][:], in0=R["e2"][:], scalar1=1.0, scalar2=None, op0=ALU.add), ["e2"], ["den"])
    rop("dve", lambda e: e.reciprocal(out=R["rden"][:], in_=R["den"][:]), ["den"], ["rden"])
    rop("dve", lambda e: e.tensor_tensor(out=g1[:], in0=R["pg"][:], in1=R["rden"][:], op=ALU.mult), ["pg", "rden"], ["g1"])
    rop("dve", lambda e: e.tensor_tensor(out=g2[:], in0=g1[:], in1=R["e2"][:], op=ALU.mult), ["g1", "e2"], ["g2"])
    for g in range(4):
        rop("dve", lambda e, g=g: e.tensor_tensor(out=O1[:, :, 8 * g:8 * g + 8], in0=R3["o1"][:], in1=R3["og"][:, :, g:g + 1].to_broadcast([128, NT, 8]), op=ALU.mult), ["o1", "og"], ["O1"])
        rop("dve", lambda e, g=g: e.tensor_tensor(out=O2[:, :, 8 * g:8 * g + 8], in0=R3["o2"][:], in1=R3["og"][:, :, g:g + 1].to_broadcast([128, NT, 8]), op=ALU.mult), ["o2", "og"], ["O2"])
    rop("dve", lambda e: e.tensor_tensor(out=Osum[:], in0=O1[:], in1=O2[:], op=ALU.add), ["O1", "O2"], ["Osum"])
    X1D = ["x1d_%d" % i for i in range(NT)]; H2D = ["h2d_%d" % i for i in range(NT)]
    if stop == "C":
        for nm, t, shp, dt_ in (("O1", O1, [128, NT, 32], BF16), ("O2", O2, [128, NT, 32], BF16), ("g1", g1, [128, NT], F32), ("g2", g2, [128, NT], F32)):
            d = dbg_out(nm, shp, dt_); P.dma("sp", d, t[:], reads=[nm], writes=["dbg_" + nm])
        d = dbg_out("x1", [S, D]); P.dma("sp", d, x1d, reads=X1D, writes=["dbg_x1"])
        d = dbg_out("h2", [S, D], BF16); P.dma("sp", d, h2d, reads=H2D, writes=["dbg_h2"])
        return nc, P, st, dbg
    P.barrier()
    P.off = route_mark
    us_sb = P.sb([32, 32], F32, "us_sb"); ui_sb = P.sb([32, 32], F32, "ui_sb"); thr16 = P.sb([32, 16], F32, "thr16")
    cmp16 = P.sb([32, 16], F32, "cmp16"); nblkT = P.sb([32, 1], F32, "nblkT"); nb_bc = P.sb([32, 128], F32, "nb_bc")
    pstart_s = P.sb([128, 32], F32, "pstart_s"); pend_sb = P.sb([128, 32], F32, "pend_sb")
    bgrid = P.sb([128, NBLK, 32], F32, "bgrid"); cmpb = P.sb([128, NBLK, 32], F32, "cmpb")
    blkf = P.sb([128, NBLK], F32, "blkf"); actf = P.sb([128, NBLK], F32, "actf")
    pidx = P.sb([128, 1], I32, "pidx"); pidxf = P.sb([128, 1], F32, "pidxf"); widxf = P.sb([128, NBLK], F32, "widxf")
    destf = P.sb([128, 32], F32, "destf"); dtmp = P.sb([128, 32], F32, "dtmp"); d12 = P.sb([128, 2], F32, "d12")
    h2bt = [P.sb([128, D], BF16, "h2bt%d" % k) for k in range(2)]
    P.dma("sp", us_sb[:], cst["u_strict"], writes=["us_sb"]); P.dma("sp", ui_sb[:], cst["u_incl"], writes=["ui_sb"])
    P.dma("sp", thr16[:], cst["thr16"], writes=["thr16"]); P.dma("sp", bgrid[:], cst["bgrid"], writes=["bgrid"])
    for i in range(NT):
        P.add("pe", lambda e, i=i: e.matmul(ps[0][0:32, 0:128], lhsT=Osum[:, i, :], rhs=ones_b[:], start=(i == 0), stop=(i == NT - 1)), reads=["Osum", "ones_b"], writes=[PS[0]])
    P.add("dve", lambda e: e.tensor_tensor(out=cmp16[:], in0=ps[0][0:32, 0:1].to_broadcast([32, 16]), in1=thr16[:], op=ALU.is_gt), reads=[PS[0], "thr16"], writes=["cmp16"])
    P.add("dve", lambda e: e.tensor_reduce(out=nblkT[:], in_=cmp16[:], axis=AX.X, op=ALU.add), reads=["cmp16"], writes=["nblkT"])
    P.add("dve", lambda e: e.tensor_scalar(out=nb_bc[:], in0=ones_f[0:32, :], scalar1=nblkT[:, 0:1], scalar2=None, op0=ALU.mult), reads=["nblkT", "ones_f"], writes=["nb_bc"])
    P.add("pe", lambda e: e.matmul(ps[1][:, 0:32], lhsT=nb_bc[:], rhs=us_sb[:], start=True, stop=True), reads=["nb_bc", "us_sb"], writes=[PS[1]])
    P.add("pe", lambda e: e.matmul(ps[1][:, 32:64], lhsT=nb_bc[:], rhs=ui_sb[:], start=True, stop=True), reads=["nb_bc", "ui_sb"], writes=[PS[1]])
    P.add("dve", lambda e: e.tensor_scalar(out=pstart_s[:], in0=ps[1][:, 0:32], scalar1=128.0, scalar2=None, op0=ALU.mult), reads=[PS[1]], writes=["pstart_s"])
    P.add("dve", lambda e: e.tensor_copy(out=pend_sb[:], in_=ps[1][:, 32:64]), reads=[PS[1]], writes=["pend_sb"])
    P.add("dve", lambda e: e.tensor_tensor(out=cmpb[:], in0=pend_sb[:].unsqueeze(1).to_broadcast([128, NBLK, 32]), in1=bgrid[:], op=ALU.is_le), reads=["pend_sb", "bgrid"], writes=["cmpb"])
    P.add("dve", lambda e: e.tensor_reduce(out=blkf[:], in_=cmpb[:], axis=AX.X, op=ALU.add), reads=["cmpb"], writes=["blkf"])
    P.add("dve", lambda e: e.tensor_scalar(out=blkf[:], in0=blkf[:], scalar1=31.0, scalar2=None, op0=ALU.min), reads=["blkf"], writes=["blkf"])
    P.add("dve", lambda e: e.tensor_copy(out=blk_i[:], in_=blkf[:]), reads=["blkf"], writes=["blk_i"])
    P.add("dve", lambda e: e.tensor_scalar(out=actf[:], in0=bgrid[:, :, 0], scalar1=pend_sb[:, 31:32], scalar2=None, op0=ALU.is_lt), reads=["bgrid", "pend_sb"], writes=["actf"])
    P.add("dve", lambda e: e.tensor_copy(out=act_i[:], in_=actf[:]), reads=["actf"], writes=["act_i"])
    P.add("pool", lambda e: e.iota(pidx[:], pattern=[[0, 1]], base=0, channel_multiplier=1), writes=["pidx"])
    P.add("dve", lambda e: e.tensor_copy(out=pidxf[:], in_=pidx[:]), reads=["pidx"], writes=["pidxf"])
    P.add("dve", lambda e: e.tensor_scalar(out=widxf[:], in0=blkf[:], scalar1=128.0, scalar2=pidxf[:, 0:1], op0=ALU.mult, op1=ALU.add), reads=["blkf", "pidxf"], writes=["widxf"])
    P.add("dve", lambda e: e.tensor_scalar(out=actf[:], in0=actf[:], scalar1=-1.0, scalar2=-8192.0, op0=ALU.add, op1=ALU.mult), reads=["actf"], writes=["actf"])
    P.add("dve", lambda e: e.tensor_tensor(out=widxf[:], in0=widxf[:], in1=actf[:], op=ALU.add), reads=["widxf", "actf"], writes=["widxf"])
    P.add("dve", lambda e: e.tensor_copy(out=widx[:], in_=widxf[:]), reads=["widxf"], writes=["widx"])
    for i in range(NT):
        pt = ps[2 + i % 2]; pn = PS[2 + i % 2]
        for j in range(i + 1):
            P.add("pe", lambda e, pt=pt, i=i, j=j: e.matmul(pt[:, 0:32], lhsT=(ones_b if j < i else ltri)[:], rhs=Osum[:, j, :], start=(j == 0), stop=(j == i)), reads=["Osum", "ones_b", "ltri"], writes=[pn])
        P.add("dve", lambda e, pt=pt: e.tensor_tensor(out=destf[:], in0=pt[:, 0:32], in1=pstart_s[:], op=ALU.add), reads=[pn, "pstart_s"], writes=["destf"])
        for a, Oa, on in ((0, O1, "O1"), (1, O2, "O2")):
            P.add("dve", lambda e, Oa=Oa, i=i: e.tensor_tensor(out=dtmp[:], in0=destf[:], in1=Oa[:, i, :], op=ALU.mult), reads=["destf", on], writes=["dtmp"])
            P.add("dve", lambda e, a=a: e.tensor_reduce(out=d12[:, a:a + 1], in_=dtmp[:], axis=AX.X, op=ALU.add), reads=["dtmp"], writes=["d12"])
        P.add("dve", lambda e, i=i: e.tensor_copy(out=dest_i[:, i, :], in_=d12[:]), reads=["d12"], writes=["dest_i_%d" % i])
        hb = h2bt[i % 2]; hbn = "h2bt%d" % (i % 2)
        P.dma("sp", hb[:], h2d[i * 128:(i + 1) * 128, :], reads=[H2D[i]], writes=[hbn])
        for a in range(2):
            P.add("pool", lambda e, hb=hb, i=i, a=a: e.indirect_dma_start(out=Xs, out_offset=bass.IndirectOffsetOnAxis(ap=dest_i[:, i, a:a + 1], axis=0), in_=hb[:], in_offset=None),
                  reads=[hbn, "dest_i_%d" % i] + ["Xs_zero_%d" % bb for bb in range(NBLK)], writes=["Xs_s_%d_%d" % (i, a)], dma=True)
    DEST = ["dest_i_%d" % i for i in range(NT)]
    XS = ["Xs_s_%d_%d" % (i, a) for i in range(NT) for a in range(2)]
    if stop == "D":
        d = dbg_out("dest", [128, NT, 2], U32); P.dma("sp", d, dest_i[:], reads=DEST, writes=["dbg_dest"])
        d = dbg_out("blk", [128, NBLK], I32); P.dma("sp", d, blk_i[:], reads=["blk_i"], writes=["dbg_blk"])
        d = dbg_out("act", [128, NBLK], I32); P.dma("sp", d, act_i[:], reads=["act_i"], writes=["dbg_act"])
        d = dbg_out("Xs", [NSLOT, D], BF16); P.dma("sp", d, Xs, reads=XS, writes=["dbg_Xs"])
        return nc, P, st, dbg
    P.barrier()

    P.off = route_mark
    wg = [P.sb([128, 16, 512], BF16, "wg%d" % k) for k in range(2)]
    wu = [P.sb([128, 16, 512], BF16, "wu%d" % k) for k in range(2)]
    wd = [P.sb([128, 4, D], BF16, "wd%d" % k) for k in range(2)]
    xe = [P.sb([128, D], BF16, "xe%d" % k) for k in range(2)]
    xeT = [P.sb([128, 16, 128], BF16, "xeT%d" % k) for k in range(2)]
    sg = P.sb([128, 512], F32, "sg"); a_bf = P.sb([128, 512], BF16, "a_bf"); aT = P.sb([128, 4, 128], BF16, "aT")
    yb = [P.sb([128, D], F32, "yb%d" % k) for k in range(2)]
    ET = mybir.EngineType
    bcreg = {}

    def mk_bc(e):
        bcreg["r"] = e.alloc_register("wbound")
        return e.reg_mov(bcreg["r"], 32 * 128 - 1)
    P.add("pool", mk_bc)
    for b in range(NBLK):
        k = b % 2
        for wt_, wsrc, wn_, is_down in ((wg[k], w_gate, "wg%d" % k, False), (wu[k], w_up, "wu%d" % k, False), (wd[k], w_down, "wd%d" % k, True)):
            for j in range(4):
                dst = wt_[:, j, :] if is_down else wt_[:, 4 * j:4 * j + 4, :].rearrange("p c n -> p (c n)")
                P.add("pool", lambda e, dst=dst, src=wsrc[j], b=b: e.indirect_dma_start(out=dst, out_offset=None, in_=src, in_offset=bass.IndirectOffsetOnAxis(ap=widx[:, b:b + 1], axis=0),
                                                                                     bounds_check=bcreg["r"], oob_is_err=False),
                      reads=["widx"], writes=[wn_ + "_%d" % j], dma=True)
        P.dma("sp", xe[k][:], Xs[b * 128:(b + 1) * 128, :], reads=XS, writes=["xe%d" % k])
        for c in range(16):
            sl = c % 8; pbt = pb[c // 8]
            P.add("pe", lambda e, c=c, sl=sl, pbt=pbt, k=k: e.transpose(out=pbt[:, sl * 128:(sl + 1) * 128], in_=xe[k][:, c * 128:(c + 1) * 128], identity=ident_b[:]), reads=["xe%d" % k, "ident_b"], writes=[PB[c // 8]])
        for c in range(16):
            sl = c % 8; pbt = pb[c // 8]
            evac(xeT[k][:, c, :], pbt[:, sl * 128:(sl + 1) * 128], [PB[c // 8]], ["xeT%d" % k], eng=("act" if c < 8 else "dve"))
        for c in range(16):
            P.add("pe", lambda e, c=c, k=k: e.matmul(ps[0][:], lhsT=xeT[k][:, c, :], rhs=wg[k][:, c, :], start=(c == 0), stop=(c == 15)), reads=["xeT%d" % k, "wg%d_%d" % (k, c // 4)], writes=[PS[0]])
        for c in range(16):
            P.add("pe", lambda e, c=c, k=k: e.matmul(ps[1][:], lhsT=xeT[k][:, c, :], rhs=wu[k][:, c, :], start=(c == 0), stop=(c == 15)), reads=["xeT%d" % k, "wu%d_%d" % (k, c // 4)], writes=[PS[1]])
        P.add("act", lambda e: e.activation(out=sg[:], in_=ps[0][:], func=AF.Silu), reads=[PS[0]], writes=["sg"])
        P.add("dve", lambda e: e.tensor_tensor(out=a_bf[:], in0=sg[:], in1=ps[1][:], op=ALU.mult), reads=["sg", PS[1]], writes=["a_bf"])
        for f_ in range(4):
            P.add("pe", lambda e, f_=f_: e.transpose(out=pb[0][:, f_ * 128:(f_ + 1) * 128], in_=a_bf[:, f_ * 128:(f_ + 1) * 128], identity=ident_b[:]), reads=["a_bf", "ident_b"], writes=[PB[0]])
        for f_ in range(4):
            evac(aT[:, f_, :], pb[0][:, f_ * 128:(f_ + 1) * 128], [PB[0]], ["aT"], eng="dve")
        for n in range(4):
            for f_ in range(4):
                P.add("pe", lambda e, n=n, f_=f_, k=k: e.matmul(ps[2 + n][:], lhsT=aT[:, f_, :], rhs=wd[k][:, f_, n * 512:(n + 1) * 512], start=(f_ == 0), stop=(f_ == 3)), reads=["aT", "wd%d_%d" % (k, f_)], writes=[PS[2 + n]])
            evac(yb[k][:, n * 512:(n + 1) * 512], ps[2 + n][:], [PS[2 + n]], ["yb%d" % k])
        P.dma("sp", Ys[b * 128:(b + 1) * 128, :], yb[k][:], reads=["yb%d" % k], writes=["Ys"])
    if stop == "E":
        for bi, b in enumerate((0, 10, 30)):
            P.dma("sp", yb[bi % 2][:], Ys[b * 128:(b + 1) * 128, :], reads=["Ys", "yb%d" % (bi % 2)], writes=["yb%d" % (bi % 2)])
            d = dbg_out("Ys%d" % b, [128, D]); P.dma("sp", d, yb[bi % 2][:], reads=["yb%d" % (bi % 2)], writes=["dbg_Ys%d" % b])
        return nc, P, st, dbg
    P.barrier()

    P.off = route_mark
    x1t = [P.sb([128, D], F32, "x1t%d" % k) for k in range(2)]
    y1 = [P.sb([128, D], F32, "y1_%d" % k) for k in range(2)]
    y2 = [P.sb([128, D], F32, "y2_%d" % k) for k in range(2)]
    ob = [P.sb([128, D], F32, "ob%d" % k) for k in range(2)]
    gt2_bc = P.sb([128, D], F32, "gt2_bc"); gfin_bc = P.sb([128, D], F32, "gfin_bc")
    junk3 = P.sb([128, D], BF16, "junk3")
    ss3 = P.sb([128, NT], F32, "ss3"); rt3 = P.sb([128, NT], F32, "rt3"); rstd3 = P.sb([128, NT], F32, "rstd3")
    P.dma("sp", gt2_bc[:], modd[0:1, 5 * D:6 * D].to_broadcast([128, D]), reads=["modd"], writes=["gt2_bc"])
    P.dma("sp", gfin_bc[:], g_final[0:1, :].to_broadcast([128, D]), writes=["gfin_bc"])
    P.add("pool", lambda e: e.memset(ss3[:], 0.0), writes=["ss3"])
    for i in range(NT):
        k = i % 2
        P.dma("sp", x1t[k][:], x1d[i * 128:(i + 1) * 128, :], reads=[X1D[i]], writes=["x1t%d" % k])
        for a, yt, yn in ((0, y1[k], "y1_%d" % k), (1, y2[k], "y2_%d" % k)):
            P.add("pool", lambda e, yt=yt, i=i, a=a: e.indirect_dma_start(out=yt[:], out_offset=None, in_=Ys, in_offset=bass.IndirectOffsetOnAxis(ap=dest_i[:, i, a:a + 1], axis=0)),
                  reads=["Ys", DEST[i]], writes=[yn], dma=True)
        P.add("dve", lambda e, k=k, i=i: e.tensor_scalar(out=y1[k][:], in0=y1[k][:], scalar1=g1[:, i:i + 1], scalar2=None, op0=ALU.mult), reads=["y1_%d" % k, "g1"], writes=["y1_%d" % k])
        P.add("dve", lambda e, k=k, i=i: e.scalar_tensor_tensor(out=y1[k][:], in0=y2[k][:], scalar=g2[:, i:i + 1], in1=y1[k][:], op0=ALU.mult, op1=ALU.add), reads=["y1_%d" % k, "y2_%d" % k, "g2"], writes=["y1_%d" % k])
        P.add("pool", lambda e, k=k: e.tensor_tensor(out=y1[k][:], in0=y1[k][:], in1=gt2_bc[:], op=ALU.mult), reads=["y1_%d" % k, "gt2_bc"], writes=["y1_%d" % k])
        P.add("pool", lambda e, k=k: e.tensor_tensor(out=x1t[k][:], in0=x1t[k][:], in1=y1[k][:], op=ALU.add), reads=["y1_%d" % k, "x1t%d" % k], writes=["x1t%d" % k])
        P.add("act", lambda e, k=k, i=i: e.activation(out=junk3[:], in_=x1t[k][:], func=AF.Square, accum_out=ss3[:, i:i + 1]), reads=["x1t%d" % k, "ss3"], writes=["junk3", "ss3"])
        P.add("act", lambda e, i=i: e.activation(out=rt3[:, i:i + 1], in_=ss3[:, i:i + 1], func=AF.Sqrt, bias=eps_t[:, 0:1], scale=1.0 / D), reads=["ss3", "eps"], writes=["rt3"])
        P.add("dve", lambda e, i=i: e.reciprocal(out=rstd3[:, i:i + 1], in_=rt3[:, i:i + 1]), reads=["rt3"], writes=["rstd3"])
        P.add("dve", lambda e, k=k, i=i: e.scalar_tensor_tensor(out=ob[k][:], in0=x1t[k][:], scalar=rstd3[:, i:i + 1], in1=gfin_bc[:], op0=ALU.mult, op1=ALU.mult), reads=["x1t%d" % k, "rstd3", "gfin_bc"], writes=["ob%d" % k])
        P.dma("sp", out[i * 128:(i + 1) * 128, :], ob[k][:], reads=["ob%d" % k], writes=["out_%d" % i])
    dbg["__final__"] = ["out_%d" % i for i in range(NT)]
    return nc, P, st, dbg


def finish(nc, P, st, out_tokens):
    P.add("sp", None, reads=list(out_tokens))
    P.emit()
    st.close()


def make_shared(inputs, consts):
    f = np.ascontiguousarray
    m = {
        "w_ada": f(inputs["w_ada"][0]),
        "b_ada": f(inputs["b_ada"][0].reshape(1, -1)),
        "g_mixT": f(inputs["g_mix"][0].reshape(16, 128).T),
        "w_in": f(inputs["w_in"][0]),
        "w_kw": f(inputs["w_in"][0][:, 3584:3664]),
        "w_pool": f(inputs["w_pool"][0]),
        "pool_scaleT": f(inputs["pool_scale"][0].reshape(8, 128).T),
        "rel_bias": f(inputs["rel_bias"]),
        "w_out": f(inputs["w_out"][0]),
        "g_ffn": f(inputs["g_ffn"][0].reshape(1, -1)),
        "wr": f(np.concatenate([inputs["w_group"][0], inputs["w_router"][0], np.zeros((D, 28), np.float32)], axis=1).reshape(16, 128, 64).transpose(1, 0, 2)),
        "br": f(np.broadcast_to(np.concatenate([inputs["b_group"][0], inputs["b_router"][0]]).reshape(1, -1), (128, 36))),
        "g_final": f(inputs["g_final"].reshape(1, -1)),
    }
    wgl = inputs["w_gate"][0].reshape(32, 16, 128, 512).transpose(0, 2, 1, 3).reshape(32 * 128, 16, 512)
    wul = inputs["w_up"][0].reshape(32, 16, 128, 512).transpose(0, 2, 1, 3).reshape(32 * 128, 16, 512)
    wdl = inputs["w_down"][0].reshape(32, 4, 128, D).transpose(0, 2, 1, 3).reshape(32 * 128, 4, D)
    for j in range(4):
        m["w_gate_%d" % j] = f(wgl[:, 4 * j:4 * j + 4, :].reshape(32 * 128, 2048))
        m["w_up_%d" % j] = f(wul[:, 4 * j:4 * j + 4, :].reshape(32 * 128, 2048))
        m["w_down_%d" % j] = f(wdl[:, j, :])
    for k, v in consts.items():
        m["c_" + k] = v
    return m


def make_in_map(inputs, b, consts, shared=None):
    if shared is None:
        shared = make_shared(inputs, consts)
    m = dict(shared)
    m["x"] = np.ascontiguousarray(inputs["x"][b])
    m["cT"] = np.ascontiguousarray(inputs["c"][b].reshape(16, 128).T)
    return m


_CACHE = {}


def kernel(**inputs):
    inputs = {k: np.asarray(v) for k, v in inputs.items()}
    if "nc" not in _CACHE:
        nc, P, st, dbg = build()
        finish(nc, P, st, dbg.pop("__final__"))
        _CACHE["nc"] = nc
    nc = _CACHE["nc"]
    consts = host_consts()
    shared = make_shared(inputs, consts)
    in_maps = [make_in_map(inputs, b, consts, shared) for b in range(8)]
    res = run_bass_kernel_spmd(nc, in_maps, core_ids=list(range(8)))
    return np.stack([np.asarray(r["out"], dtype=np.float32) for r in res.results], axis=0)
```

```python
import numpy as np
import ml_dtypes
import concourse.bass as bass
import concourse.mybir as mybir
from concourse.bass_utils import run_bass_kernel_spmd

F32 = mybir.dt.float32
BF16 = mybir.dt.bfloat16
I32 = mybir.dt.int32
U32 = mybir.dt.uint32
ALU = mybir.AluOpType
AF = mybir.ActivationFunctionType
AX = mybir.AxisListType


class _Op:
    __slots__ = ("eng", "fn", "reads", "writes", "dma", "deps", "signal", "waits",
                 "sigval", "dsem", "dval", "after", "is_bar")

    def __init__(self, eng, fn, reads, writes, dma, after):
        self.eng = eng; self.fn = fn; self.reads = reads; self.writes = writes
        self.dma = dma; self.deps = (); self.signal = False; self.waits = []
        self.sigval = 0; self.dsem = None; self.dval = 0; self.after = after


class Prog:
    ENGS = ("pe", "dve", "act", "pool", "sp")

    def __init__(self, nc):
        self.nc = nc
        self.ops = []
        self.off = 16512
        self.nalloc = 0

    def sb(self, shape, dtype, name=None):
        esz = {F32: 4, BF16: 2, I32: 4, U32: 4}[dtype]
        n = 1
        for s in shape[1:]:
            n *= s
        nbytes = (n * esz + 63) // 64 * 64
        self.nalloc += 1
        name = "%s_%d" % (name or "t", self.nalloc)
        t = self.nc.alloc_sbuf_tensor_at(name, list(shape), dtype, offset=self.off)
        self.off += nbytes
        assert self.off <= 229376, ("sbuf overflow", self.off)
        return t

    def add(self, eng, fn, reads=(), writes=(), dma=False, after=()):
        self.ops.append(_Op(eng, fn, tuple(reads), tuple(writes), dma, tuple(after)))
        return len(self.ops) - 1

    def dma(self, q, out, in_, reads=(), writes=(), **kw):
        return self.add(q, lambda e: e.dma_start(out=out, in_=in_, **kw), reads, writes, dma=True)

    def barrier(self):
        deps = [i for i, o in enumerate(self.ops) if o.dma]
        for e in ("pe", "dve", "act", "pool"):
            for i in range(len(self.ops) - 1, -1, -1):
                o = self.ops[i]
                if o.eng == e and not o.dma and o.fn is not None and not getattr(o, "is_bar", False):
                    deps.append(i)
                    break
        src, dst = self.bar_src, self.bar_dst
        b = self.add("sp", lambda en: en.dma_start(out=dst, in_=src), dma=True, after=deps)
        for e in ("pe", "dve", "act", "pool"):
            self.add(e, None, after=[b])

    def emit(self):
        nc = self.nc
        ops = self.ops
        last_w = {}
        readers = {}
        for i, op in enumerate(ops):
            deps = set(op.after)
            for t in op.reads:
                if t in last_w:
                    deps.add(last_w[t])
            for t in op.writes:
                if t in last_w:
                    deps.add(last_w[t])
                deps.update(readers.get(t, ()))
            for t in op.reads:
                readers.setdefault(t, []).append(i)
            for t in op.writes:
                last_w[t] = i
                readers[t] = []
            deps.discard(i)
            op.deps = deps
        seen = {e: {e2: -1 for e2 in self.ENGS} for e in self.ENGS}
        dma_seen = {e: set() for e in self.ENGS}
        NQ = {"sp": 20, "act": 8, "pool": 16}
        NOUT = {"sp": 20, "act": 8, "pool": 10}
        dma_count = {q: 0 for q in NQ}
        dma_hist = {q: [] for q in NQ}
        for i, op in enumerate(ops):
            E = op.eng
            waits = []
            if op.dma:
                k = dma_count[E]
                dma_count[E] += 1
                dma_hist[E].append(i)
                if k >= NOUT[E]:
                    prev = dma_hist[E][k - NOUT[E]]
                    if prev not in dma_seen[E]:
                        waits.append(("d", prev))
                        dma_seen[E].add(prev)
                op.dsem = (E, k % NQ[E])
                op.dval = 16 * (k // NQ[E] + 1)
            for j in sorted(op.deps):
                oj = ops[j]
                if oj.dma:
                    if j not in dma_seen[E]:
                        dma_seen[E].add(j)
                        waits.append(("d", j))
                else:
                    E2 = oj.eng
                    if E2 == E and E == "pe" and not op.dma:
                        continue
                    if seen[E][E2] >= j:
                        continue
                    seen[E][E2] = j
                    oj.signal = True
                    waits.append(("c", j))
            op.waits = waits
        CH = 1000
        cnt = {e: 0 for e in self.ENGS}
        for op in ops:
            if op.signal:
                op.sigval = (cnt[op.eng] // CH, cnt[op.eng] % CH + 1)
                cnt[op.eng] += 1
        self.stats = dict(cnt)
        self.stats["nops"] = len(ops)
        from contextlib import ExitStack
        with ExitStack() as st:
            csem = {}
            for e in self.ENGS:
                for c in range(cnt[e] // CH + 1):
                    csem[(e, c)] = st.enter_context(nc.semaphore("c_%s_%d" % (e, c)))
            dsem = {}
            for q, n in NQ.items():
                for k in range(n):
                    dsem[(q, k)] = st.enter_context(nc.semaphore("d_%s_%d" % (q, k)))
            block = st.enter_context(nc.Block())

            def run(engname):
                def body(eng):
                    for op in ops:
                        if op.eng != engname:
                            continue
                        for kind, j in op.waits:
                            oj = ops[j]
                            if kind == "d":
                                eng.wait_ge(dsem[oj.dsem], oj.dval)
                            else:
                                eng.wait_ge(csem[(oj.eng, oj.sigval[0])], oj.sigval[1])
                        if op.fn is None:
                            assert not op.signal and not op.dma
                            continue
                        ins = op.fn(eng)
                        if op.dma:
                            ins.then_inc(dsem[op.dsem], 16)
                        elif op.signal:
                            ins.then_inc(csem[(op.eng, op.sigval[0])], 1)
                return body

            block.tensor(run("pe"))
            block.vector(run("dve"))
            block.scalar(run("act"))
            block.gpsimd(run("pool"))
            block.sync(run("sp"))


S = 2048
D = 2048
NT = 16
NBLK = 63
NSLOT = NBLK * 128
SM_SCALE = 128 ** -0.5


def host_consts():
    c = {}
    c["ident_f"] = np.eye(128, dtype=np.float32)
    c["ident_b"] = np.eye(128, dtype=np.float32).astype(ml_dtypes.bfloat16)
    q = np.arange(128)[:, None]; s = np.arange(128)[None, :]
    c["causal_neg"] = np.where(s <= q, 0.0, -1e30).astype(np.float32)
    c["ltri"] = (q < s).astype(np.float32).astype(ml_dtypes.bfloat16)
    c["ones_b"] = np.ones((128, 128), np.float32).astype(ml_dtypes.bfloat16)
    c["ones_f"] = np.ones((128, 128), np.float32)
    c["anti_f"] = np.eye(128, dtype=np.float32)[::-1].copy()
    corr = np.ones((4, 16), np.float32)
    for g, w in enumerate((2, 4, 8, 16)):
        for t in range(16):
            corr[g, t] = w / min(t + 1, w)
    c["corr"] = np.broadcast_to(corr[None], (128, 4, 16)).copy()
    oh = np.zeros((32, 384), np.float32)
    for m in range(383):
        r = max(m - 127, 0)
        if r < 16:
            b = r
        else:
            rf = np.float32(max(r, 1))
            b = 16 + int(np.float32(np.log(rf / np.float32(16)) / np.float32(np.log(8.0)) * np.float32(16)))
            b = min(b, 31)
        oh[b, m] = 1.0
    c["bucket_oh"] = oh
    e1 = np.arange(32)[:, None]; e2 = np.arange(32)[None, :]
    c["u_strict"] = (e1 < e2).astype(np.float32)
    c["u_incl"] = (e1 <= e2).astype(np.float32)
    c["thr16"] = np.broadcast_to((np.arange(16) * 128).astype(np.float32)[None], (32, 16)).copy()
    bg = np.broadcast_to(np.arange(NBLK, dtype=np.float32)[None, :, None], (128, NBLK, 32)).copy()
    c["bgrid"] = bg
    return c


CONST_SPECS = [("ident_f", [128, 128], F32), ("ident_b", [128, 128], BF16), ("causal_neg", [128, 128], F32),
               ("ltri", [128, 128], BF16), ("ones_b", [128, 128], BF16), ("ones_f", [128, 128], F32), ("anti_f", [128, 128], F32),
               ("corr", [128, 4, 16], F32), ("bucket_oh", [32, 384], F32), ("u_strict", [32, 32], F32),
               ("u_incl", [32, 32], F32), ("thr16", [32, 16], F32), ("bgrid", [128, NBLK, 32], F32)]


def w_in_view_(w_in, c0, n):
    return w_in[:, c0:c0 + n].rearrange("(kc p) n -> p kc n", p=128)


def build(stop=None):
    nc = bass.Bass("TRN2", target_bir_lowering=False)

    def din(name, shape, dt=F32):
        return nc.dram_tensor(name, list(shape), dt, kind="ExternalInput").ap()

    def dscratch(name, shape, dt=F32):
        return nc.dram_tensor(name, list(shape), dt, kind="Internal").ap()

    x = din("x", [S, D]); cT = din("cT", [128, 16]); w_ada = din("w_ada", [D, 6 * D]); b_ada = din("b_ada", [1, 6 * D])
    w_kw = din("w_kw", [D, 80])
    g_mixT = din("g_mixT", [128, 16]); w_in = din("w_in", [D, 3664]); w_pool = din("w_pool", [4, 256, 256])
    pool_scaleT = din("pool_scaleT", [128, 8]); rel_bias = din("rel_bias", [32, 8]); w_out = din("w_out", [D, D])
    g_ffn = din("g_ffn", [1, D]); wr = din("wr", [128, 16, 64]); br = din("br", [128, 36])
    NE_DECL = 32 if stop in (None, "E", "F") else 1
    w_gate = [din("w_gate_%d" % j, [NE_DECL * 128, 2048]) for j in range(4)]
    w_up = [din("w_up_%d" % j, [NE_DECL * 128, 2048]) for j in range(4)]
    w_down = [din("w_down_%d" % j, [NE_DECL * 128, 2048]) for j in range(4)]
    g_final = din("g_final", [1, D])
    cst = {n: din("c_" + n, shp, dt) for n, shp, dt in CONST_SPECS}
    out = nc.dram_tensor("out", [S, D], F32, kind="ExternalOutput").ap()
    dbg = {}

    def dbg_out(name, shape, dt=F32):
        dbg[name] = nc.dram_tensor("dbg_" + name, list(shape), dt, kind="ExternalOutput").ap()
        return dbg[name]

    modd = dscratch("modd", [1, 6 * D])
    fextd = dscratch("fextd", [8, 384])
    x1d = dscratch("x1d", [S, D])
    h2d = dscratch("h2d", [S, D], BF16)
    Xs = dscratch("Xs", [NSLOT, D], BF16)
    Ys = dscratch("Ys", [NSLOT, D])

    P = Prog(nc)
    bar_t = P.sb([1, 16], F32, "bar_t")
    P.add("pool", lambda e: e.memset(bar_t[:], 0.0), writes=["bar_t"])
    P.bar_src = bar_t[:]
    P.bar_dst = dscratch("bar_d", [1, 16])
    from contextlib import ExitStack
    st = ExitStack()
    ps = [st.enter_context(nc.psum_tensor("ps%d" % k, [128, 512], F32)) for k in range(6)]
    pb = [st.enter_context(nc.psum_tensor("pb%d" % k, [128, 1024], BF16)) for k in range(2)]
    PS = ["ps%d" % k for k in range(6)]
    PB = ["pb0", "pb1"]

    ident_f = P.sb([128, 128], F32, "ident_f"); ident_b = P.sb([128, 128], BF16, "ident_b")
    causal_neg = P.sb([128, 128], F32, "causal_neg"); ltri = P.sb([128, 128], BF16, "ltri")
    ones_b = P.sb([128, 128], BF16, "ones_b"); ones_f = P.sb([128, 128], F32, "ones_f")
    corr = P.sb([128, 4, 16], F32, "corr")
    for n, t in (("ident_f", ident_f), ("ident_b", ident_b), ("causal_neg", causal_neg), ("ltri", ltri),
                 ("ones_b", ones_b), ("ones_f", ones_f), ("corr", corr)):
        P.dma("sp", t[:], cst[n], writes=[n])
    modT = P.sb([128, 96], F32, "modT")
    s1T = P.sb([128, 16], F32, "s1T")
    gmT = P.sb([128, 16], F32, "gmT")
    pscT = P.sb([128, 8], F32, "pscT")
    eps_t = P.sb([128, 1], F32, "eps")
    P.dma("sp", gmT[:], g_mixT, writes=["gmT"])
    P.dma("sp", pscT[:], pool_scaleT, writes=["pscT"])
    P.add("pool", lambda e: e.memset(eps_t[:], 1e-6), writes=["eps"])
    base_mark = P.off

    scT = P.sb([128, 16], F32, "scT")
    wa = [P.sb([128, 16, 512], F32, "wa%d" % k) for k in range(2)]
    mod_row = P.sb([1, 6 * D], F32, "mod_row")
    ba_row = P.sb([1, 6 * D], F32, "ba_row")
    gf_row = P.sb([1, D], F32, "gf_row")
    P.dma("sp", scT[:], cT, writes=["scT"])
    P.dma("sp", ba_row[:], b_ada, writes=["ba_row"])
    P.dma("sp", gf_row[:], g_ffn, writes=["gf_row"])
    P.add("act", lambda e: e.activation(out=scT[:], in_=scT[:], func=AF.Silu), reads=["scT"], writes=["scT"])
    zt = P.sb([128, D], BF16, "zt")
    P.add("pool", lambda e: e.memset(zt[:], 0.0), writes=["zt"])
    for b in range(NBLK):
        P.dma("pool", Xs[b * 128:(b + 1) * 128, :], zt[:], reads=["zt"], writes=["Xs_zero_%d" % b])
    for n in range(24):
        wt = wa[n % 2]; wn = "wa%d" % (n % 2)
        P.dma("sp" if n % 2 == 0 else "act", wt[:], w_ada[:, n * 512:(n + 1) * 512].rearrange("(kc p) n -> p kc n", p=128), writes=[wn])
        pt = ps[n % 2]; pn = PS[n % 2]
        for kc in range(16):
            P.add("pe", lambda e, pt=pt, wt=wt, kc=kc: e.matmul(pt[0:1, :], lhsT=scT[:, kc:kc + 1], rhs=wt[:, kc, :], start=(kc == 0), stop=(kc == 15)),
                  reads=["scT", wn], writes=[pn])
        P.add("dve", lambda e, pt=pt, n=n: e.tensor_tensor(out=mod_row[0:1, n * 512:(n + 1) * 512], in0=pt[0:1, :], in1=ba_row[0:1, n * 512:(n + 1) * 512], op=ALU.add),
              reads=[pn, "ba_row"], writes=["mod_row"])
    P.add("dve", lambda e: e.scalar_tensor_tensor(out=mod_row[0:1, 4 * D:5 * D], in0=mod_row[0:1, 4 * D:5 * D], scalar=1.0, in1=gf_row[0:1, :], op0=ALU.add, op1=ALU.mult),
          reads=["mod_row", "gf_row"], writes=["mod_row"])
    P.dma("sp", modd, mod_row[:], reads=["mod_row"], writes=["modd"])
    for j in range(96):
        P.add("pe", lambda e, j=j: e.matmul(ps[2][:, j:j + 1], lhsT=mod_row[0:1, j * 128:(j + 1) * 128], rhs=ones_f[0:1, 0:1], start=True, stop=True),
              reads=["mod_row", "ones_f"], writes=[PS[2]])
    P.add("dve", lambda e: e.tensor_copy(out=modT[:], in_=ps[2][:, 0:96]), reads=[PS[2]], writes=["modT"])
    P.add("dve", lambda e: e.scalar_tensor_tensor(out=s1T[:], in0=modT[:, 16:32], scalar=1.0, in1=gmT[:], op0=ALU.add, op1=ALU.mult),
          reads=["modT", "gmT"], writes=["s1T"])
    if stop == "A":
        d = dbg_out("modT", [128, 96])
        P.dma("sp", d, modT[:], reads=["modT"], writes=["dbg"])
        return nc, P, st, dbg
    P.barrier()
    P.off = base_mark

    hT = P.sb([128, 16, S], BF16, "hT")
    proj_mark = P.off
    xs = [P.sb([128, D], F32, "xs%d" % k) for k in range(2)]
    xn = [P.sb([128, D], BF16, "xn%d" % k) for k in range(2)]
    junk = P.sb([128, D], BF16, "junk")
    ss = P.sb([128, 16], F32, "ss"); rt = P.sb([128, 16], F32, "rt"); rstd = P.sb([128, 16], F32, "rstd")
    P.add("pool", lambda e: e.memset(ss[:], 0.0), writes=["ss"])
    for i in range(NT):
        xt = xs[i % 2]; xtn = "xs%d" % (i % 2); xb = xn[i % 2]; xbn = "xn%d" % (i % 2)
        P.dma("sp", xt[:], x[i * 128:(i + 1) * 128, :], writes=[xtn])
        P.add("act", lambda e, xt=xt, i=i: e.activation(out=junk[:], in_=xt[:], func=AF.Square, accum_out=ss[:, i:i + 1]),
              reads=[xtn, "ss"], writes=["junk", "ss"])
        P.add("act", lambda e, i=i: e.activation(out=rt[:, i:i + 1], in_=ss[:, i:i + 1], func=AF.Sqrt, bias=eps_t[:, 0:1], scale=1.0 / D),
              reads=["ss", "eps"], writes=["rt"])
        P.add("dve", lambda e, i=i: e.reciprocal(out=rstd[:, i:i + 1], in_=rt[:, i:i + 1]), reads=["rt"], writes=["rstd"])
        P.add("dve", lambda e, xt=xt, xb=xb, i=i: e.tensor_scalar(out=xb[:], in0=xt[:], scalar1=rstd[:, i:i + 1], scalar2=None, op0=ALU.mult),
              reads=[xtn, "rstd"], writes=[xbn])
        for c in range(16):
            pbt = pb[c // 8]; pbn = PB[c // 8]
            P.add("pe", lambda e, xb=xb, c=c, pbt=pbt: e.transpose(out=pbt[:, (c % 8) * 128:(c % 8 + 1) * 128], in_=xb[:, c * 128:(c + 1) * 128], identity=ident_b[:]),
                  reads=[xbn, "ident_b"], writes=[pbn])
        for c in range(16):
            pbt = pb[c // 8]; pbn = PB[c // 8]
            P.add("act", lambda e, c=c, i=i, pbt=pbt: e.activation(out=hT[:, c, i * 128:(i + 1) * 128], in_=pbt[:, (c % 8) * 128:(c % 8 + 1) * 128], func=AF.Identity,
                                                                   bias=modT[:, c:c + 1], scale=s1T[:, c:c + 1]),
                  reads=[pbn, "modT", "s1T"], writes=["hT_%d" % i])
    HT = ["hT_%d" % i for i in range(NT)]
    if stop == "B1":
        d = dbg_out("hT", [128, 16, S], BF16)
        P.dma("sp", d, hT[:], reads=HT, writes=["dbg"])
        return nc, P, st, dbg
    P.barrier()
    P.off = proj_mark

    uT = P.sb([128, 8, S], BF16, "uT"); qT = P.sb([128, 8, S], BF16, "qT"); qiT = P.sb([128, 8, S], BF16, "qiT")
    kT = P.sb([128, 2, S], BF16, "kT"); vtok = P.sb([128, NT, 256], BF16, "vtok"); kiT2 = P.sb([128, S], BF16, "kiT2")
    wi_all = P.sb([128, NT, 16], F32, "wi_all")
    after_proj_mark = P.off
    wst = [P.sb([128, 16, 272], BF16, "wst%d" % k) for k in range(2)]
    kw_st = P.sb([128, 16, 80], F32, "kw_st")
    P.dma("sp", kw_st[:], w_kw.rearrange("(kc p) n -> p kc n", p=128), writes=["kw_st"])
    evac_n = [0]

    def evac(dst, src, reads, writes, eng=None):
        k = evac_n[0]; evac_n[0] += 1
        if eng == "act" or (eng is None and k % 2 == 0):
            P.add("act", lambda e: e.copy(out=dst, in_=src), reads=reads, writes=writes)
        else:
            P.add("dve", lambda e: e.tensor_copy(out=dst, in_=src), reads=reads, writes=writes)

    def w_in_view(c0, n):
        return w_in_view_(w_in, c0, n)

    groups = []
    for g in range(4):
        groups.append((g * 256, "fm", [(uT, 2 * g), (uT, 2 * g + 1)]))
    for g in range(4):
        groups.append((1024 + g * 256, "fm", [(qT, 2 * g), (qT, 2 * g + 1)]))
    groups.append((2048, "fm", [(kT, 0), (kT, 1)]))
    for g in range(4):
        groups.append((2560 + g * 256, "fm", [(qiT, 2 * g), (qiT, 2 * g + 1)]))
    groups.append((3584, "ki", None))
    groups.append((2304, "tm", None))
    gi = 0
    pk = 0
    for c0, kind, dests in groups:
        if stop == "B2a" and gi == 1:
            d = dbg_out("uT01", [128, 2, S], BF16)
            P.dma("sp", d, uT[:, 0:2, :], reads=[uT.name + "_0", uT.name + "_1"], writes=["dbg_uT01"])
            return nc, P, st, dbg
        if stop == "B2c" and kind == "tm":
            d = dbg_out("kiT2", [128, S], BF16)
            P.dma("sp", d, kiT2[:], reads=["kiT2"], writes=["dbg_kiT2"])
            return nc, P, st, dbg
        if stop == "B2b" and kind == "ki":
            d = dbg_out("qiT", [128, 8, S], BF16)
            P.dma("sp", d, qiT[:], reads=[qiT.name + "_%d" % c for c in range(8)], writes=["dbg_qiT"])
            return nc, P, st, dbg
        wt = wst[gi % 2]; wn = "wst%d" % (gi % 2); gi += 1
        if kind == "fm":
            P.dma("pool", wt[:, :, 0:256], w_in_view(c0, 256), writes=[wn])
            for sub, (dt_, ch) in enumerate(dests):
                for tc in range(4):
                    pt = ps[pk % 4]; pn = PS[pk % 4]; pk += 1
                    for kc in range(16):
                        P.add("pe", lambda e, pt=pt, wt=wt, kc=kc, sub=sub, tc=tc: e.matmul(pt[:], lhsT=wt[:, kc, sub * 128:(sub + 1) * 128], rhs=hT[:, kc, tc * 512:(tc + 1) * 512], start=(kc == 0), stop=(kc == 15)),
                              reads=[wn] + HT[tc * 4:tc * 4 + 4], writes=[pn])
                    evac(dt_[:, ch, tc * 512:(tc + 1) * 512], pt[:], [pn], [dt_.name + "_%d" % ch])
        elif kind == "ki":
            P.add("dve", lambda e, wt=wt: e.tensor_copy(out=wt[:, :, 0:64], in_=kw_st[:, :, 0:64]), reads=["kw_st"], writes=[wn])
            P.add("dve", lambda e, wt=wt: e.tensor_copy(out=wt[:, :, 64:128], in_=kw_st[:, :, 0:64]), reads=["kw_st"], writes=[wn])
            for tc in range(4):
                pt = ps[pk % 4]; pn = PS[pk % 4]; pk += 1
                for kc in range(16):
                    P.add("pe", lambda e, pt=pt, wt=wt, kc=kc, tc=tc: e.matmul(pt[:], lhsT=wt[:, kc, 0:128], rhs=hT[:, kc, tc * 512:(tc + 1) * 512], start=(kc == 0), stop=(kc == 15)),
                          reads=[wn] + HT[tc * 4:tc * 4 + 4], writes=[pn])
                evac(kiT2[:, tc * 512:(tc + 1) * 512], pt[:], [pn], ["kiT2"])
        else:
            P.dma("pool", wt[:, :, 0:256], w_in_view(2304, 256), writes=[wn])
            P.add("dve", lambda e, wt=wt: e.tensor_copy(out=wt[:, :, 256:272], in_=kw_st[:, :, 64:80]), reads=["kw_st"], writes=[wn])
            for i in range(NT):
                pt = ps[pk % 4]; pn = PS[pk % 4]; pk += 1
                for kc in range(16):
                    P.add("pe", lambda e, pt=pt, wt=wt, kc=kc, i=i: e.matmul(pt[:, 0:256], lhsT=hT[:, kc, i * 128:(i + 1) * 128], rhs=wt[:, kc, 0:256], start=(kc == 0), stop=(kc == 15)),
                          reads=[wn, HT[i]], writes=[pn])
                P.add("act", lambda e, pt=pt, i=i: e.copy(out=vtok[:, i, :], in_=pt[:, 0:256]), reads=[pn], writes=["vtok"])
                pt2 = ps[4 + i % 2]; pn2 = PS[4 + i % 2]
                for kc in range(16):
                    P.add("pe", lambda e, pt2=pt2, wt=wt, kc=kc, i=i: e.matmul(pt2[:, 0:16], lhsT=hT[:, kc, i * 128:(i + 1) * 128], rhs=wt[:, kc, 256:272], start=(kc == 0), stop=(kc == 15)),
                          reads=[wn, HT[i]], writes=[pn2])
                P.add("dve", lambda e, pt2=pt2, i=i: e.tensor_copy(out=wi_all[:, i, :], in_=pt2[:, 0:16]), reads=[pn2], writes=["wi_all"])
    UT = [uT.name + "_%d" % c for c in range(8)]; QT = [qT.name + "_%d" % c for c in range(8)]
    QIT = [qiT.name + "_%d" % c for c in range(8)]; KT = [kT.name + "_%d" % c for c in range(2)]
    if stop == "B2":
        for nm, t, toks, shp, dt_ in (("uT", uT, UT, [128, 8, S], BF16), ("qT", qT, QT, [128, 8, S], BF16), ("qiT", qiT, QIT, [128, 8, S], BF16),
                                      ("kT", kT, KT, [128, 2, S], BF16), ("vtok", vtok, ["vtok"], [128, NT, 256], BF16),
                                      ("kiT2", kiT2, ["kiT2"], [128, S], BF16), ("wi_all", wi_all, ["wi_all"], [128, NT, 16], F32)):
            d = dbg_out(nm, shp, dt_)
            P.dma("sp", d, t[:], reads=toks, writes=["dbg_" + nm])
        return nc, P, st, dbg
    P.barrier()
    P.off = base_mark
    pool_outT = P.sb([128, 8, S], BF16, "pool_outT"); attn_outT = P.sb([128, 8, S], BF16, "attn_outT")
    POT = ["pool_outT_%d" % c for c in range(8)]; AOT = ["attn_outT_%d" % i for i in range(NT)]
    P.off = after_proj_mark
    A_ = P.sb([128, S], F32, "poolA"); B_ = P.sb([128, S], F32, "poolB"); wp = P.sb([128, 4, 2, 256], BF16, "wp")
    P.dma("pool", wp[:], w_pool.rearrange("g (cc p) d -> p g cc d", p=128), writes=["wp"])
    for c in range(8):
        g = c // 2; w = 2 << g
        u = uT[:, c, :]; un = UT[c]
        P.add("dve", lambda e, u=u: e.tensor_tensor(out=A_[:, 1:S], in0=u[:, 1:S], in1=u[:, 0:S - 1], op=ALU.add), reads=[un], writes=["poolA"])
        P.add("dve", lambda e, u=u: e.tensor_copy(out=A_[:, 0:1], in_=u[:, 0:1]), reads=[un], writes=["poolA"])
        cur, curn, oth, othn = A_, "poolA", B_, "poolB"
        d = 2
        while d < w:
            P.add("dve", lambda e, cur=cur, oth=oth, d=d: e.tensor_tensor(out=oth[:, d:S], in0=cur[:, d:S], in1=cur[:, 0:S - d], op=ALU.add), reads=[curn], writes=[othn])
            P.add("dve", lambda e, cur=cur, oth=oth, d=d: e.tensor_copy(out=oth[:, 0:d], in_=cur[:, 0:d]), reads=[curn], writes=[othn])
            cur, curn, oth, othn = oth, othn, cur, curn
            d *= 2
        P.add("dve", lambda e, cur=cur, g=g: e.tensor_tensor(out=cur[:, 0:16], in0=cur[:, 0:16], in1=corr[:, g, :], op=ALU.mult), reads=[curn, "corr"], writes=[curn])
        P.add("dve", lambda e, cur=cur, u=u, w=w: e.scalar_tensor_tensor(out=u, in0=cur[:], scalar=1.0 / w, in1=u, op0=ALU.mult, op1=ALU.subtract), reads=[curn, un], writes=[un])
        if c % 2 == 1:
            for dch in range(2):
                for tc in range(4):
                    pt = ps[pk % 4]; pn = PS[pk % 4]; pk += 1
                    for cc in range(2):
                        P.add("pe", lambda e, pt=pt, g=g, cc=cc, dch=dch, tc=tc: e.matmul(pt[:], lhsT=wp[:, g, cc, dch * 128:(dch + 1) * 128], rhs=uT[:, 2 * g + cc, tc * 512:(tc + 1) * 512], start=(cc == 0), stop=(cc == 1)),
                              reads=["wp", UT[2 * g], UT[2 * g + 1]], writes=[pn])
                    P.add("act", lambda e, pt=pt, g=g, dch=dch, tc=tc: e.activation(out=pool_outT[:, 2 * g + dch, tc * 512:(tc + 1) * 512], in_=pt[:], func=AF.Identity, scale=pscT[:, 2 * g + dch:2 * g + dch + 1]),
                          reads=[pn, "pscT"], writes=[POT[2 * g + dch]])
    if stop == "B3":
        d = dbg_out("pool_outT", [128, 8, S], BF16)
        P.dma("sp", d, pool_outT[:], reads=POT, writes=["dbg_pool_outT"])
        return nc, P, st, dbg
    P.barrier()

    P.off = proj_mark
    acc = P.sb([128, S], F32, "acc"); maskq = P.sb([128, S], BF16, "maskq"); junkb = P.sb([128, S], BF16, "junkb")
    maskT = P.sb([128, NT, 128], BF16, "maskT"); EB3 = P.sb([128, 8, 384], F32, "EB3")
    assert P.off == proj_mark + 32768
    P.off = after_proj_mark
    rtile = [P.sb([128, 512], F32, "rtile%d" % k) for k in range(2)]
    Et = [P.sb([128, 512], BF16, "Et%d" % k) for k in range(2)]
    P1 = [P.sb([128, 512], BF16, "P1%d" % k) for k in range(2)]
    Pt = [P.sb([128, 512], BF16, "Pt%d" % k) for k in range(2)]
    rinv = P.sb([128, 512], F32, "rinv")
    lo = P.sb([128, 1], F32, "lo"); hi = P.sb([128, 1], F32, "hi"); mid = P.sb([128, 1], F32, "mid"); cnt = P.sb([128, 1], F32, "cnt")
    ge = P.sb([128, 1], U32, "ge"); lt = P.sb([128, 1], U32, "lt")
    relb = P.sb([32, 8], F32, "relb"); boh = P.sb([32, 384], F32, "boh"); fext = P.sb([8, 384], F32, "fext")
    hank = P.sb([128, 256], F32, "hank"); anti = P.sb([128, 128], F32, "anti")
    P.dma("sp", relb[:], rel_bias, writes=["relb"]); P.dma("sp", boh[:], cst["bucket_oh"], writes=["boh"])
    P.dma("sp", anti[:], cst["anti_f"], writes=["anti"])
    P.add("pe", lambda e: e.matmul(ps[0][0:8, 0:384], lhsT=relb[:], rhs=boh[:], start=True, stop=True), reads=["relb", "boh"], writes=[PS[0]])
    P.add("dve", lambda e: e.tensor_copy(out=fext[:], in_=ps[0][0:8, 0:384]), reads=[PS[0]], writes=["fext"])
    P.dma("sp", fextd, fext[:], reads=["fext"], writes=["fextd"])
    for h in range(8):
        P.dma("sp", hank[:], bass.AP(fextd.tensor, h * 384, [[1, 128], [1, 256]]), reads=["fextd"], writes=["hank"])
        P.add("pe", lambda e: e.matmul(ps[1][:, 0:256], lhsT=anti[:], rhs=hank[:], start=True, stop=True), reads=["anti", "hank"], writes=[PS[1]])
        P.add("act", lambda e, h=h: e.activation(out=EB3[:, h, 0:256], in_=ps[1][:, 0:256], func=AF.Exp), reads=[PS[1]], writes=["EB3"])
        P.add("act", lambda e, h=h: e.activation(out=EB3[:, h, 256:384], in_=ps[1][:, 255:256].to_broadcast([128, 128]), func=AF.Exp), reads=[PS[1]], writes=["EB3"])
    NIT = 17
    ek = 0
    for i in range(NT):
        nk = (i + 1) * 128
        nch = (nk + 511) // 512
        for h in range(16):
            pr, hh = h // 2, h % 2
            b0 = 64 * hh
            for kc in range(nch):
                wd = min(512, nk - kc * 512)
                pt = ps[ek % 2]; pn = PS[ek % 2]; rtl = rtile[ek % 2]; rn = "rtile%d" % (ek % 2); ek += 1
                P.add("pe", lambda e, pt=pt, pr=pr, b0=b0, i=i, kc=kc, wd=wd: e.matmul(pt[:, 0:wd], lhsT=qiT[b0:b0 + 64, pr, i * 128:(i + 1) * 128], rhs=kiT2[b0:b0 + 64, kc * 512:kc * 512 + wd], start=True, stop=True),
                      reads=[QIT[pr], "kiT2"], writes=[pn])
                P.add("act", lambda e, pt=pt, rtl=rtl, wd=wd: e.activation(out=rtl[:, 0:wd], in_=pt[:, 0:wd], func=AF.Relu), reads=[pn], writes=[rn])
                if h == 0:
                    P.add("dve", lambda e, rtl=rtl, kc=kc, wd=wd, i=i, h=h: e.tensor_scalar(out=acc[:, kc * 512:kc * 512 + wd], in0=rtl[:, 0:wd], scalar1=wi_all[:, i, h:h + 1], scalar2=None, op0=ALU.mult),
                          reads=[rn, "wi_all"], writes=["acc"])
                else:
                    P.add("dve", lambda e, rtl=rtl, kc=kc, wd=wd, i=i, h=h: e.scalar_tensor_tensor(out=acc[:, kc * 512:kc * 512 + wd], in0=rtl[:, 0:wd], scalar=wi_all[:, i, h:h + 1], in1=acc[:, kc * 512:kc * 512 + wd], op0=ALU.mult, op1=ALU.add),
                          reads=[rn, "wi_all", "acc"], writes=["acc"])
        P.add("dve", lambda e, i=i: e.tensor_tensor(out=acc[:, i * 128:(i + 1) * 128], in0=acc[:, i * 128:(i + 1) * 128], in1=causal_neg[:], op=ALU.add), reads=["acc", "causal_neg"], writes=["acc"])
        if i >= 2:
            P.add("dve", lambda e, nk=nk: e.tensor_reduce(out=hi[:], in_=acc[:, 0:nk], axis=AX.X, op=ALU.max), reads=["acc"], writes=["hi"])
            P.add("dve", lambda e, i=i: e.tensor_reduce(out=lo[:], in_=acc[:, 0:i * 128], axis=AX.X, op=ALU.min), reads=["acc"], writes=["lo"])
            P.add("dve", lambda e: e.tensor_tensor(out=hi[:], in0=hi[:], in1=lo[:], op=ALU.subtract), reads=["hi", "lo"], writes=["hi"])
            for it in range(NIT):
                P.add("dve", lambda e, it=it: e.scalar_tensor_tensor(out=mid[:], in0=hi[:], scalar=0.5 ** (it + 1), in1=lo[:], op0=ALU.mult, op1=ALU.add), reads=["lo", "hi"], writes=["mid"])
                P.add("dve", lambda e, nk=nk: e.tensor_scalar(out=junkb[:, 0:nk], in0=acc[:, 0:nk], scalar1=mid[:, 0:1], scalar2=None, op0=ALU.is_ge, op1=ALU.add, accum_out=cnt[:, 0:1]),
                      reads=["acc", "mid"], writes=["junkb", "cnt"])
                P.add("dve", lambda e: e.tensor_scalar(out=ge[:], in0=cnt[:], scalar1=255.5, scalar2=None, op0=ALU.is_ge), reads=["cnt"], writes=["ge"])
                P.add("dve", lambda e: e.copy_predicated(out=lo[:], mask=ge[:], data=mid[:]), reads=["ge", "mid", "lo"], writes=["lo"])
            P.add("dve", lambda e, nk=nk: e.tensor_scalar(out=maskq[:, 0:nk], in0=acc[:, 0:nk], scalar1=lo[:, 0:1], scalar2=None, op0=ALU.is_ge), reads=["acc", "lo"], writes=["maskq"])
        else:
            P.add("dve", lambda e, nk=nk: e.tensor_scalar(out=maskq[:, 0:nk], in0=acc[:, 0:nk], scalar1=-1e29, scalar2=None, op0=ALU.is_ge), reads=["acc"], writes=["maskq"])
        for j0 in range(0, i + 1, 8):
            jj = list(range(j0, min(i + 1, j0 + 8)))
            pbt = pb[(j0 // 8) % 2]; pbn = PB[(j0 // 8) % 2]
            for j in jj:
                P.add("pe", lambda e, j=j, pbt=pbt: e.transpose(out=pbt[:, (j % 8) * 128:(j % 8 + 1) * 128], in_=maskq[:, j * 128:(j + 1) * 128], identity=ident_b[:]), reads=["maskq", "ident_b"], writes=[pbn])
            for j in jj:
                P.add("act", lambda e, j=j, pbt=pbt: e.copy(out=maskT[:, j, :], in_=pbt[:, (j % 8) * 128:(j % 8 + 1) * 128]), reads=[pbn], writes=["maskT"])
        for kv in range(2):
            for j in range(i + 1):
                off = 0 if j == i else (128 if j == i - 1 else 256)
                pS = ps[2 + ek % 2]; pSn = PS[2 + ek % 2]
                et = Et[ek % 2]; etn = "Et%d" % (ek % 2); p1 = P1[ek % 2]; p1n = "P1%d" % (ek % 2); ptt = Pt[ek % 2]; ptn = "Pt%d" % (ek % 2); ek += 1
                P.add("pe", lambda e, pS=pS, kv=kv, j=j, i=i: e.matmul(pS[:], lhsT=kT[:, kv, j * 128:(j + 1) * 128], rhs=qT[:, 4 * kv:4 * kv + 4, i * 128:(i + 1) * 128], start=True, stop=True),
                      reads=[KT[kv]] + QT[4 * kv:4 * kv + 4], writes=[pSn])
                P.add("act", lambda e, pS=pS, et=et: e.activation(out=et[:], in_=pS[:], func=AF.Exp, scale=SM_SCALE), reads=[pSn], writes=[etn])
                P.add("pool", lambda e, et=et, p1=p1, j=j: e.tensor_tensor(out=p1[:].rearrange("p (h q) -> p h q", h=4), in0=et[:].rearrange("p (h q) -> p h q", h=4), in1=maskT[:, j:j + 1, :].to_broadcast([128, 4, 128]), op=ALU.mult),
                      reads=[etn, "maskT"], writes=[p1n])
                P.add("dve", lambda e, p1=p1, ptt=ptt, kv=kv, off=off: e.tensor_tensor(out=ptt[:].rearrange("p (h q) -> p h q", h=4), in0=p1[:].rearrange("p (h q) -> p h q", h=4), in1=EB3[:, 4 * kv:4 * kv + 4, off:off + 128], op=ALU.mult),
                      reads=[p1n, "EB3"], writes=[ptn])
                P.add("pe", lambda e, ptt=ptt, kv=kv, j=j, i=i: e.matmul(ps[4][:], lhsT=vtok[:, j, kv * 128:(kv + 1) * 128], rhs=ptt[:], start=(j == 0), stop=(j == i)), reads=["vtok", ptn], writes=[PS[4]])
                P.add("pe", lambda e, ptt=ptt, j=j, i=i: e.matmul(ps[5][:], lhsT=ones_b[:], rhs=ptt[:], start=(j == 0), stop=(j == i)), reads=["ones_b", ptn], writes=[PS[5]])
            P.add("dve", lambda e: e.reciprocal(out=rinv[:], in_=ps[5][:]), reads=[PS[5]], writes=["rinv"])
            P.add("dve", lambda e, kv=kv, i=i: e.tensor_tensor(out=attn_outT[:, 4 * kv:4 * kv + 4, i * 128:(i + 1) * 128], in0=ps[4][:].rearrange("p (h q) -> p h q", h=4), in1=rinv[:].rearrange("p (h q) -> p h q", h=4), op=ALU.mult),
                  reads=[PS[4], "rinv"], writes=[AOT[i]])
        if stop == "B4a" and i == 3:
            d = dbg_out("acc", [128, 512]); P.dma("sp", d, acc[:, 0:512], reads=["acc"], writes=["dbg_acc"])
            d = dbg_out("maskq", [128, 512], BF16); P.dma("sp", d, maskq[:, 0:512], reads=["maskq"], writes=["dbg_maskq"])
            d = dbg_out("thr", [128, 1]); P.dma("sp", d, lo[:], reads=["lo"], writes=["dbg_thr"])
            d = dbg_out("EB3", [128, 8, 384]); P.dma("sp", d, EB3[:], reads=["EB3"], writes=["dbg_EB3"])
            d = dbg_out("attn_outT", [128, 8, 512], BF16); P.dma("sp", d, attn_outT[:, :, 0:512], reads=AOT[0:4], writes=["dbg_attn_outT"])
            return nc, P, st, dbg
    if stop == "B4":
        d = dbg_out("attn_outT", [128, 8, S], BF16)
        P.dma("sp", d, attn_outT[:], reads=AOT, writes=["dbg_attn_outT"])
        return nc, P, st, dbg
    P.barrier()
    P.off = proj_mark
    O1 = P.sb([128, NT, 32], BF16, "O1"); O2 = P.sb([128, NT, 32], BF16, "O2"); Osum = P.sb([128, NT, 32], BF16, "Osum")
    g1 = P.sb([128, NT], F32, "g1"); g2 = P.sb([128, NT], F32, "g2")
    dest_i = P.sb([128, NT, 2], U32, "dest_i")
    blk_i = P.sb([128, NBLK], I32, "blk_i"); act_i = P.sb([128, NBLK], I32, "act_i"); widx = P.sb([128, NBLK], U32, "widx")
    route_mark = P.off
    wo = P.sb([128, 16, D], BF16, "wo")
    xt2 = [P.sb([128, D], F32, "xt2_%d" % k) for k in range(2)]
    gt1_bc = P.sb([128, D], F32, "gt1_bc"); s2_bc = P.sb([128, D], F32, "s2_bc"); sh2_bc = P.sb([128, D], F32, "sh2_bc")
    h2 = P.sb([128, D], F32, "h2"); h2b = P.sb([128, D], BF16, "h2b"); h2T = P.sb([128, 16, 128], F32, "h2T")
    wr_sb = P.sb([128, 16, 64], F32, "wr_sb"); br_bc = P.sb([128, 36], F32, "br_bc")
    ss2 = P.sb([128, NT], F32, "ss2"); rt2 = P.sb([128, NT], F32, "rt2"); rstd2 = P.sb([128, NT], F32, "rstd2")
    L_all = P.sb([128, NT, 36], F32, "L_all")
    for n in range(4):
        P.dma("pool", wo[:, :, n * 512:(n + 1) * 512], w_out[:, n * 512:(n + 1) * 512].rearrange("(kc p) n -> p kc n", p=128), writes=["wo"])
    P.dma("sp", gt1_bc[:], modd[0:1, 2 * D:3 * D].to_broadcast([128, D]), reads=["modd"], writes=["gt1_bc"])
    P.dma("sp", sh2_bc[:], modd[0:1, 3 * D:4 * D].to_broadcast([128, D]), reads=["modd"], writes=["sh2_bc"])
    P.dma("sp", s2_bc[:], modd[0:1, 4 * D:5 * D].to_broadcast([128, D]), reads=["modd"], writes=["s2_bc"])
    P.dma("sp", wr_sb[:], wr, writes=["wr_sb"])
    P.dma("sp", br_bc[:], br, writes=["br_bc"])
    P.add("pool", lambda e: e.memset(ss2[:], 0.0), writes=["ss2"])

    for i in range(NT):
        xt = xt2[i % 2]; xtn = "xt2_%d" % (i % 2)
        P.dma("sp", xt[:], x[i * 128:(i + 1) * 128, :], writes=[xtn])
        for n in range(4):
            for kc in range(16):
                src = pool_outT if kc < 8 else attn_outT
                srcn = POT[kc] if kc < 8 else AOT[i]
                P.add("pe", lambda e, n=n, kc=kc, src=src, i=i: e.matmul(ps[n][:], lhsT=src[:, kc % 8, i * 128:(i + 1) * 128], rhs=wo[:, kc, n * 512:(n + 1) * 512], start=(kc == 0), stop=(kc == 15)),
                      reads=[srcn, "wo"], writes=[PS[n]])
            P.add("dve", lambda e, n=n: e.tensor_tensor(out=h2[:, n * 512:(n + 1) * 512], in0=ps[n][:], in1=gt1_bc[:, n * 512:(n + 1) * 512], op=ALU.mult), reads=[PS[n], "gt1_bc"], writes=["h2"])
            P.add("pool", lambda e, n=n, xt=xt: e.tensor_tensor(out=xt[:, n * 512:(n + 1) * 512], in0=xt[:, n * 512:(n + 1) * 512], in1=h2[:, n * 512:(n + 1) * 512], op=ALU.add), reads=[xtn, "h2"], writes=[xtn])
        P.dma("sp", x1d[i * 128:(i + 1) * 128, :], xt[:], reads=[xtn], writes=["x1d_%d" % i])
        P.add("act", lambda e, xt=xt, i=i: e.activation(out=h2b[:], in_=xt[:], func=AF.Square, accum_out=ss2[:, i:i + 1]), reads=[xtn, "ss2"], writes=["h2b", "ss2"])
        P.add("act", lambda e, i=i: e.activation(out=rt2[:, i:i + 1], in_=ss2[:, i:i + 1], func=AF.Sqrt, bias=eps_t[:, 0:1], scale=1.0 / D), reads=["ss2", "eps"], writes=["rt2"])
        P.add("dve", lambda e, i=i: e.reciprocal(out=rstd2[:, i:i + 1], in_=rt2[:, i:i + 1]), reads=["rt2"], writes=["rstd2"])
        P.add("dve", lambda e, xt=xt, i=i: e.scalar_tensor_tensor(out=h2[:], in0=xt[:], scalar=rstd2[:, i:i + 1], in1=s2_bc[:], op0=ALU.mult, op1=ALU.mult), reads=[xtn, "rstd2", "s2_bc", "h2"], writes=["h2"])
        P.add("pool", lambda e: e.tensor_tensor(out=h2[:], in0=h2[:], in1=sh2_bc[:], op=ALU.add), reads=["h2", "sh2_bc"], writes=["h2"])
        P.add("act", lambda e: e.copy(out=h2b[:], in_=h2[:]), reads=["h2"], writes=["h2b"])
        P.dma("sp", h2d[i * 128:(i + 1) * 128, :], h2b[:], reads=["h2b"], writes=["h2d_%d" % i])
        for c0 in range(0, 16, 4):
            for c in range(c0, c0 + 4):
                P.add("pe", lambda e, c=c: e.transpose(out=ps[4][:, (c % 4) * 128:(c % 4 + 1) * 128], in_=h2[:, c * 128:(c + 1) * 128], identity=ident_f[:]), reads=["h2", "ident_f"], writes=[PS[4]])
            for c in range(c0, c0 + 4):
                evac(h2T[:, c, :], ps[4][:, (c % 4) * 128:(c % 4 + 1) * 128], [PS[4]], ["h2T"], eng=("act" if (c0 // 4) % 2 == 0 else "dve"))
        for c in range(16):
            P.add("pe", lambda e, c=c: e.matmul(ps[5][:, 0:64], lhsT=h2T[:, c, :], rhs=wr_sb[:, c, :], start=(c == 0), stop=(c == 15)), reads=["h2T", "wr_sb"], writes=[PS[5]])
        P.add("dve", lambda e, i=i: e.tensor_tensor(out=L_all[:, i, :], in0=ps[5][:, 0:36], in1=br_bc[:], op=ALU.add), reads=[PS[5], "br_bc"], writes=["L_all"])
    def bc(t, k):
        return t[:].unsqueeze(2).to_broadcast([128, NT, k])
    R = {n: P.sb([128, NT], F32, "R_" + n) for n in ("gmax", "gsum", "pg", "m1", "m2", "dd", "e2", "den", "rden")}
    R3 = {n: P.sb([128, NT, k], F32, "R3_" + n) for n, k in (("og", 4), ("eg", 4), ("es", 8), ("tmp", 8), ("o1", 8), ("es2", 8), ("o2", 8))}
    Lg = L_all[:, :, 0:4]
    def rop(eng, fn, reads, writes):
        P.add(eng, fn, reads=reads, writes=writes)
    rop("dve", lambda e: e.tensor_reduce(out=R["gmax"][:], in_=Lg, axis=AX.X, op=ALU.max), ["L_all"], ["gmax"])
    rop("dve", lambda e: e.tensor_tensor(out=R3["og"][:], in0=Lg, in1=bc(R["gmax"], 4), op=ALU.is_ge), ["L_all", "gmax"], ["og"])
    rop("dve", lambda e: e.tensor_tensor(out=R3["eg"][:], in0=Lg, in1=bc(R["gmax"], 4), op=ALU.subtract), ["L_all", "gmax"], ["eg"])
    rop("act", lambda e: e.activation(out=R3["eg"][:], in_=R3["eg"][:], func=AF.Exp), ["eg"], ["eg"])
    rop("dve", lambda e: e.tensor_reduce(out=R["gsum"][:], in_=R3["eg"][:], axis=AX.X, op=ALU.add), ["eg"], ["gsum"])
    rop("dve", lambda e: e.reciprocal(out=R["pg"][:], in_=R["gsum"][:]), ["gsum"], ["pg"])
    rop("dve", lambda e: e.tensor_tensor(out=R3["es"][:], in0=L_all[:, :, 4:12], in1=R3["og"][:, :, 0:1].to_broadcast([128, NT, 8]), op=ALU.mult), ["L_all", "og"], ["es"])
    for g in range(1, 4):
        rop("dve", lambda e, g=g: e.tensor_tensor(out=R3["tmp"][:], in0=L_all[:, :, 4 + 8 * g:12 + 8 * g], in1=R3["og"][:, :, g:g + 1].to_broadcast([128, NT, 8]), op=ALU.mult), ["L_all", "og"], ["tmp"])
        rop("dve", lambda e: e.tensor_tensor(out=R3["es"][:], in0=R3["es"][:], in1=R3["tmp"][:], op=ALU.add), ["es", "tmp"], ["es"])
    rop("dve", lambda e: e.tensor_reduce(out=R["m1"][:], in_=R3["es"][:], axis=AX.X, op=ALU.max), ["es"], ["m1"])
    rop("dve", lambda e: e.tensor_tensor(out=R3["o1"][:], in0=R3["es"][:], in1=bc(R["m1"], 8), op=ALU.is_ge), ["es", "m1"], ["o1"])
    rop("dve", lambda e: e.scalar_tensor_tensor(out=R3["es2"][:], in0=R3["o1"][:], scalar=-1e30, in1=R3["es"][:], op0=ALU.mult, op1=ALU.add), ["o1", "es"], ["es2"])
    rop("dve", lambda e: e.tensor_reduce(out=R["m2"][:], in_=R3["es2"][:], axis=AX.X, op=ALU.max), ["es2"], ["m2"])
    rop("dve", lambda e: e.tensor_tensor(out=R3["o2"][:], in0=R3["es2"][:], in1=bc(R["m2"], 8), op=ALU.is_ge), ["es2", "m2"], ["o2"])
    rop("dve", lambda e: e.tensor_tensor(out=R["dd"][:], in0=R["m2"][:], in1=R["m1"][:], op=ALU.subtract), ["m1", "m2"], ["dd"])
    rop("act", lambda e: e.activation(out=R["e2"][:], in_=R["dd"][:], func=AF.Exp), ["dd"], ["e2"])
    rop("dve", lambda e: e.tensor_scalar(out=R["den"][:], in0=R["e2"][:], scalar1=1.0, scalar2=None, op0=ALU.add), ["e2"], ["den"])
    rop("dve", lambda e: e.reciprocal(out=R["rden"][:], in_=R["den"][:]), ["den"], ["rden"])
    rop("dve", lambda e: e.tensor_tensor(out=g1[:], in0=R["pg"][:], in1=R["rden"][:], op=ALU.mult), ["pg", "rden"], ["g1"])
    rop("dve", lambda e: e.tensor_tensor(out=g2[:], in0=g1[:], in1=R["e2"][:], op=ALU.mult), ["g1", "e2"], ["g2"])
    for g in range(4):
        rop("dve", lambda e, g=g: e.tensor_tensor(out=O1[:, :, 8 * g:8 * g + 8], in0=R3["o1"][:], in1=R3["og"][:, :, g:g + 1].to_broadcast([128, NT, 8]), op=ALU.mult), ["o1", "og"], ["O1"])
        rop("dve", lambda e, g=g: e.tensor_tensor(out=O2[:, :, 8 * g:8 * g + 8], in0=R3["o2"][:], in1=R3["og"][:, :, g:g + 1].to_broadcast([128, NT, 8]), op=ALU.mult), ["o2", "og"], ["O2"])
    rop("dve", lambda e: e.tensor_tensor(out=Osum[:], in0=O1[:], in1=O2[:], op=ALU.add), ["O1", "O2"], ["Osum"])
    X1D = ["x1d_%d" % i for i in range(NT)]; H2D = ["h2d_%d" % i for i in range(NT)]
    if stop == "C":
        for nm, t, shp, dt_ in (("O1", O1, [128, NT, 32], BF16), ("O2", O2, [128, NT, 32], BF16), ("g1", g1, [128, NT], F32), ("g2", g2, [128, NT], F32)):
            d = dbg_out(nm, shp, dt_); P.dma("sp", d, t[:], reads=[nm], writes=["dbg_" + nm])
        d = dbg_out("x1", [S, D]); P.dma("sp", d, x1d, reads=X1D, writes=["dbg_x1"])
        d = dbg_out("h2", [S, D], BF16); P.dma("sp", d, h2d, reads=H2D, writes=["dbg_h2"])
        return nc, P, st, dbg
    P.barrier()
    P.off = route_mark
    us_sb = P.sb([32, 32], F32, "us_sb"); ui_sb = P.sb([32, 32], F32, "ui_sb"); thr16 = P.sb([32, 16], F32, "thr16")
    cmp16 = P.sb([32, 16], F32, "cmp16"); nblkT = P.sb([32, 1], F32, "nblkT"); nb_bc = P.sb([32, 128], F32, "nb_bc")
    pstart_s = P.sb([128, 32], F32, "pstart_s"); pend_sb = P.sb([128, 32], F32, "pend_sb")
    bgrid = P.sb([128, NBLK, 32], F32, "bgrid"); cmpb = P.sb([128, NBLK, 32], F32, "cmpb")
    blkf = P.sb([128, NBLK], F32, "blkf"); actf = P.sb([128, NBLK], F32, "actf")
    pidx = P.sb([128, 1], I32, "pidx"); pidxf = P.sb([128, 1], F32, "pidxf"); widxf = P.sb([128, NBLK], F32, "widxf")
    destf = P.sb([128, 32], F32, "destf"); dtmp = P.sb([128, 32], F32, "dtmp"); d12 = P.sb([128, 2], F32, "d12")
    h2bt = [P.sb([128, D], BF16, "h2bt%d" % k) for k in range(2)]
    P.dma("sp", us_sb[:], cst["u_strict"], writes=["us_sb"]); P.dma("sp", ui_sb[:], cst["u_incl"], writes=["ui_sb"])
    P.dma("sp", thr16[:], cst["thr16"], writes=["thr16"]); P.dma("sp", bgrid[:], cst["bgrid"], writes=["bgrid"])
    for i in range(NT):
        P.add("pe", lambda e, i=i: e.matmul(ps[0][0:32, 0:128], lhsT=Osum[:, i, :], rhs=ones_b[:], start=(i == 0), stop=(i == NT - 1)), reads=["Osum", "ones_b"], writes=[PS[0]])
    P.add("dve", lambda e: e.tensor_tensor(out=cmp16[:], in0=ps[0][0:32, 0:1].to_broadcast([32, 16]), in1=thr16[:], op=ALU.is_gt), reads=[PS[0], "thr16"], writes=["cmp16"])
    P.add("dve", lambda e: e.tensor_reduce(out=nblkT[:], in_=cmp16[:], axis=AX.X, op=ALU.add), reads=["cmp16"], writes=["nblkT"])
    P.add("dve", lambda e: e.tensor_scalar(out=nb_bc[:], in0=ones_f[0:32, :], scalar1=nblkT[:, 0:1], scalar2=None, op0=ALU.mult), reads=["nblkT", "ones_f"], writes=["nb_bc"])
    P.add("pe", lambda e: e.matmul(ps[1][:, 0:32], lhsT=nb_bc[:], rhs=us_sb[:], start=True, stop=True), reads=["nb_bc", "us_sb"], writes=[PS[1]])
    P.add("pe", lambda e: e.matmul(ps[1][:, 32:64], lhsT=nb_bc[:], rhs=ui_sb[:], start=True, stop=True), reads=["nb_bc", "ui_sb"], writes=[PS[1]])
    P.add("dve", lambda e: e.tensor_scalar(out=pstart_s[:], in0=ps[1][:, 0:32], scalar1=128.0, scalar2=None, op0=ALU.mult), reads=[PS[1]], writes=["pstart_s"])
    P.add("dve", lambda e: e.tensor_copy(out=pend_sb[:], in_=ps[1][:, 32:64]), reads=[PS[1]], writes=["pend_sb"])
    P.add("dve", lambda e: e.tensor_tensor(out=cmpb[:], in0=pend_sb[:].unsqueeze(1).to_broadcast([128, NBLK, 32]), in1=bgrid[:], op=ALU.is_le), reads=["pend_sb", "bgrid"], writes=["cmpb"])
    P.add("dve", lambda e: e.tensor_reduce(out=blkf[:], in_=cmpb[:], axis=AX.X, op=ALU.add), reads=["cmpb"], writes=["blkf"])
    P.add("dve", lambda e: e.tensor_scalar(out=blkf[:], in0=blkf[:], scalar1=31.0, scalar2=None, op0=ALU.min), reads=["blkf"], writes=["blkf"])
    P.add("dve", lambda e: e.tensor_copy(out=blk_i[:], in_=blkf[:]), reads=["blkf"], writes=["blk_i"])
    P.add("dve", lambda e: e.tensor_scalar(out=actf[:], in0=bgrid[:, :, 0], scalar1=pend_sb[:, 31:32], scalar2=None, op0=ALU.is_lt), reads=["bgrid", "pend_sb"], writes=["actf"])
    P.add("dve", lambda e: e.tensor_copy(out=act_i[:], in_=actf[:]), reads=["actf"], writes=["act_i"])
    P.add("pool", lambda e: e.iota(pidx[:], pattern=[[0, 1]], base=0, channel_multiplier=1), writes=["pidx"])
    P.add("dve", lambda e: e.tensor_copy(out=pidxf[:], in_=pidx[:]), reads=["pidx"], writes=["pidxf"])
    P.add("dve", lambda e: e.tensor_scalar(out=widxf[:], in0=blkf[:], scalar1=128.0, scalar2=pidxf[:, 0:1], op0=ALU.mult, op1=ALU.add), reads=["blkf", "pidxf"], writes=["widxf"])
    P.add("dve", lambda e: e.tensor_scalar(out=actf[:], in0=actf[:], scalar1=-1.0, scalar2=-8192.0, op0=ALU.add, op1=ALU.mult), reads=["actf"], writes=["actf"])
    P.add("dve", lambda e: e.tensor_tensor(out=widxf[:], in0=widxf[:], in1=actf[:], op=ALU.add), reads=["widxf", "actf"], writes=["widxf"])
    P.add("dve", lambda e: e.tensor_copy(out=widx[:], in_=widxf[:]), reads=["widxf"], writes=["widx"])
    for i in range(NT):
        pt = ps[2 + i % 2]; pn = PS[2 + i % 2]
        for j in range(i + 1):
            P.add("pe", lambda e, pt=pt, i=i, j=j: e.matmul(pt[:, 0:32], lhsT=(ones_b if j < i else ltri)[:], rhs=Osum[:, j, :], start=(j == 0), stop=(j == i)), reads=["Osum", "ones_b", "ltri"], writes=[pn])
        P.add("dve", lambda e, pt=pt: e.tensor_tensor(out=destf[:], in0=pt[:, 0:32], in1=pstart_s[:], op=ALU.add), reads=[pn, "pstart_s"], writes=["destf"])
        for a, Oa, on in ((0, O1, "O1"), (1, O2, "O2")):
            P.add("dve", lambda e, Oa=Oa, i=i: e.tensor_tensor(out=dtmp[:], in0=destf[:], in1=Oa[:, i, :], op=ALU.mult), reads=["destf", on], writes=["dtmp"])
            P.add("dve", lambda e, a=a: e.tensor_reduce(out=d12[:, a:a + 1], in_=dtmp[:], axis=AX.X, op=ALU.add), reads=["dtmp"], writes=["d12"])
        P.add("dve", lambda e, i=i: e.tensor_copy(out=dest_i[:, i, :], in_=d12[:]), reads=["d12"], writes=["dest_i_%d" % i])
        hb = h2bt[i % 2]; hbn = "h2bt%d" % (i % 2)
        P.dma("sp", hb[:], h2d[i * 128:(i + 1) * 128, :], reads=[H2D[i]], writes=[hbn])
        for a in range(2):
            P.add("pool", lambda e, hb=hb, i=i, a=a: e.indirect_dma_start(out=Xs, out_offset=bass.IndirectOffsetOnAxis(ap=dest_i[:, i, a:a + 1], axis=0), in_=hb[:], in_offset=None),
                  reads=[hbn, "dest_i_%d" % i] + ["Xs_zero_%d" % bb for bb in range(NBLK)], writes=["Xs_s_%d_%d" % (i, a)], dma=True)
    DEST = ["dest_i_%d" % i for i in range(NT)]
    XS = ["Xs_s_%d_%d" % (i, a) for i in range(NT) for a in range(2)]
    if stop == "D":
        d = dbg_out("dest", [128, NT, 2], U32); P.dma("sp", d, dest_i[:], reads=DEST, writes=["dbg_dest"])
        d = dbg_out("blk", [128, NBLK], I32); P.dma("sp", d, blk_i[:], reads=["blk_i"], writes=["dbg_blk"])
        d = dbg_out("act", [128, NBLK], I32); P.dma("sp", d, act_i[:], reads=["act_i"], writes=["dbg_act"])
        d = dbg_out("Xs", [NSLOT, D], BF16); P.dma("sp", d, Xs, reads=XS, writes=["dbg_Xs"])
        return nc, P, st, dbg
    P.barrier()

    P.off = route_mark
    wg = [P.sb([128, 16, 512], BF16, "wg%d" % k) for k in range(2)]
    wu = [P.sb([128, 16, 512], BF16, "wu%d" % k) for k in range(2)]
    wd = [P.sb([128, 4, D], BF16, "wd%d" % k) for k in range(2)]
    xe = [P.sb([128, D], BF16, "xe%d" % k) for k in range(2)]
    xeT = [P.sb([128, 16, 128], BF16, "xeT%d" % k) for k in range(2)]
    sg = P.sb([128, 512], F32, "sg"); a_bf = P.sb([128, 512], BF16, "a_bf"); aT = P.sb([128, 4, 128], BF16, "aT")
    yb = [P.sb([128, D], F32, "yb%d" % k) for k in range(2)]
    ET = mybir.EngineType
    bcreg = {}

    def mk_bc(e):
        bcreg["r"] = e.alloc_register("wbound")
        return e.reg_mov(bcreg["r"], 32 * 128 - 1)
    P.add("pool", mk_bc)
    for b in range(NBLK):
        k = b % 2
        for wt_, wsrc, wn_, is_down in ((wg[k], w_gate, "wg%d" % k, False), (wu[k], w_up, "wu%d" % k, False), (wd[k], w_down, "wd%d" % k, True)):
            for j in range(4):
                dst = wt_[:, j, :] if is_down else wt_[:, 4 * j:4 * j + 4, :].rearrange("p c n -> p (c n)")
                P.add("pool", lambda e, dst=dst, src=wsrc[j], b=b: e.indirect_dma_start(out=dst, out_offset=None, in_=src, in_offset=bass.IndirectOffsetOnAxis(ap=widx[:, b:b + 1], axis=0),
                                                                                     bounds_check=bcreg["r"], oob_is_err=False),
                      reads=["widx"], writes=[wn_ + "_%d" % j], dma=True)
        P.dma("sp", xe[k][:], Xs[b * 128:(b + 1) * 128, :], reads=XS, writes=["xe%d" % k])
        for c in range(16):
            sl = c % 8; pbt = pb[c // 8]
            P.add("pe", lambda e, c=c, sl=sl, pbt=pbt, k=k: e.transpose(out=pbt[:, sl * 128:(sl + 1) * 128], in_=xe[k][:, c * 128:(c + 1) * 128], identity=ident_b[:]), reads=["xe%d" % k, "ident_b"], writes=[PB[c // 8]])
        for c in range(16):
            sl = c % 8; pbt = pb[c // 8]
            evac(xeT[k][:, c, :], pbt[:, sl * 128:(sl + 1) * 128], [PB[c // 8]], ["xeT%d" % k], eng=("act" if c < 8 else "dve"))
        for c in range(16):
            P.add("pe", lambda e, c=c, k=k: e.matmul(ps[0][:], lhsT=xeT[k][:, c, :], rhs=wg[k][:, c, :], start=(c == 0), stop=(c == 15)), reads=["xeT%d" % k, "wg%d_%d" % (k, c // 4)], writes=[PS[0]])
        for c in range(16):
            P.add("pe", lambda e, c=c, k=k: e.matmul(ps[1][:], lhsT=xeT[k][:, c, :], rhs=wu[k][:, c, :], start=(c == 0), stop=(c == 15)), reads=["xeT%d" % k, "wu%d_%d" % (k, c // 4)], writes=[PS[1]])
        P.add("act", lambda e: e.activation(out=sg[:], in_=ps[0][:], func=AF.Silu), reads=[PS[0]], writes=["sg"])
        P.add("dve", lambda e: e.tensor_tensor(out=a_bf[:], in0=sg[:], in1=ps[1][:], op=ALU.mult), reads=["sg", PS[1]], writes=["a_bf"])
        for f_ in range(4):
            P.add("pe", lambda e, f_=f_: e.transpose(out=pb[0][:, f_ * 128:(f_ + 1) * 128], in_=a_bf[:, f_ * 128:(f_ + 1) * 128], identity=ident_b[:]), reads=["a_bf", "ident_b"], writes=[PB[0]])
        for f_ in range(4):
            evac(aT[:, f_, :], pb[0][:, f_ * 128:(f_ + 1) * 128], [PB[0]], ["aT"], eng="dve")
        for n in range(4):
            for f_ in range(4):
                P.add("pe", lambda e, n=n, f_=f_, k=k: e.matmul(ps[2 + n][:], lhsT=aT[:, f_, :], rhs=wd[k][:, f_, n * 512:(n + 1) * 512], start=(f_ == 0), stop=(f_ == 3)), reads=["aT", "wd%d_%d" % (k, f_)], writes=[PS[2 + n]])
            evac(yb[k][:, n * 512:(n + 1) * 512], ps[2 + n][:], [PS[2 + n]], ["yb%d" % k])
        P.dma("sp", Ys[b * 128:(b + 1) * 128, :], yb[k][:], reads=["yb%d" % k], writes=["Ys"])
    if stop == "E":
        for bi, b in enumerate((0, 10, 30)):
            P.dma("sp", yb[bi % 2][:], Ys[b * 128:(b + 1) * 128, :], reads=["Ys", "yb%d" % (bi % 2)], writes=["yb%d" % (bi % 2)])
            d = dbg_out("Ys%d" % b, [128, D]); P.dma("sp", d, yb[bi % 2][:], reads=["yb%d" % (bi % 2)], writes=["dbg_Ys%d" % b])
        return nc, P, st, dbg
    P.barrier()

    P.off = route_mark
    x1t = [P.sb([128, D], F32, "x1t%d" % k) for k in range(2)]
    y1 = [P.sb([128, D], F32, "y1_%d" % k) for k in range(2)]
    y2 = [P.sb([128, D], F32, "y2_%d" % k) for k in range(2)]
    ob = [P.sb([128, D], F32, "ob%d" % k) for k in range(2)]
    gt2_bc = P.sb([128, D], F32, "gt2_bc"); gfin_bc = P.sb([128, D], F32, "gfin_bc")
    junk3 = P.sb([128, D], BF16, "junk3")
    ss3 = P.sb([128, NT], F32, "ss3"); rt3 = P.sb([128, NT], F32, "rt3"); rstd3 = P.sb([128, NT], F32, "rstd3")
    P.dma("sp", gt2_bc[:], modd[0:1, 5 * D:6 * D].to_broadcast([128, D]), reads=["modd"], writes=["gt2_bc"])
    P.dma("sp", gfin_bc[:], g_final[0:1, :].to_broadcast([128, D]), writes=["gfin_bc"])
    P.add("pool", lambda e: e.memset(ss3[:], 0.0), writes=["ss3"])
    for i in range(NT):
        k = i % 2
        P.dma("sp", x1t[k][:], x1d[i * 128:(i + 1) * 128, :], reads=[X1D[i]], writes=["x1t%d" % k])
        for a, yt, yn in ((0, y1[k], "y1_%d" % k), (1, y2[k], "y2_%d" % k)):
            P.add("pool", lambda e, yt=yt, i=i, a=a: e.indirect_dma_start(out=yt[:], out_offset=None, in_=Ys, in_offset=bass.IndirectOffsetOnAxis(ap=dest_i[:, i, a:a + 1], axis=0)),
                  reads=["Ys", DEST[i]], writes=[yn], dma=True)
        P.add("dve", lambda e, k=k, i=i: e.tensor_scalar(out=y1[k][:], in0=y1[k][:], scalar1=g1[:, i:i + 1], scalar2=None, op0=ALU.mult), reads=["y1_%d" % k, "g1"], writes=["y1_%d" % k])
        P.add("dve", lambda e, k=k, i=i: e.scalar_tensor_tensor(out=y1[k][:], in0=y2[k][:], scalar=g2[:, i:i + 1], in1=y1[k][:], op0=ALU.mult, op1=ALU.add), reads=["y1_%d" % k, "y2_%d" % k, "g2"], writes=["y1_%d" % k])
        P.add("pool", lambda e, k=k: e.tensor_tensor(out=y1[k][:], in0=y1[k][:], in1=gt2_bc[:], op=ALU.mult), reads=["y1_%d" % k, "gt2_bc"], writes=["y1_%d" % k])
        P.add("pool", lambda e, k=k: e.tensor_tensor(out=x1t[k][:], in0=x1t[k][:], in1=y1[k][:], op=ALU.add), reads=["y1_%d" % k, "x1t%d" % k], writes=["x1t%d" % k])
        P.add("act", lambda e, k=k, i=i: e.activation(out=junk3[:], in_=x1t[k][:], func=AF.Square, accum_out=ss3[:, i:i + 1]), reads=["x1t%d" % k, "ss3"], writes=["junk3", "ss3"])
        P.add("act", lambda e, i=i: e.activation(out=rt3[:, i:i + 1], in_=ss3[:, i:i + 1], func=AF.Sqrt, bias=eps_t[:, 0:1], scale=1.0 / D), reads=["ss3", "eps"], writes=["rt3"])
        P.add("dve", lambda e, i=i: e.reciprocal(out=rstd3[:, i:i + 1], in_=rt3[:, i:i + 1]), reads=["rt3"], writes=["rstd3"])
        P.add("dve", lambda e, k=k, i=i: e.scalar_tensor_tensor(out=ob[k][:], in0=x1t[k][:], scalar=rstd3[:, i:i + 1], in1=gfin_bc[:], op0=ALU.mult, op1=ALU.mult), reads=["x1t%d" % k, "rstd3", "gfin_bc"], writes=["ob%d" % k])
        P.dma("sp", out[i * 128:(i + 1) * 128, :], ob[k][:], reads=["ob%d" % k], writes=["out_%d" % i])
    dbg["__final__"] = ["out_%d" % i for i in range(NT)]
    return nc, P, st, dbg


def finish(nc, P, st, out_tokens):
    P.add("sp", None, reads=list(out_tokens))
    P.emit()
    st.close()


def make_shared(inputs, consts):
    f = np.ascontiguousarray
    m = {
        "w_ada": f(inputs["w_ada"][0]),
        "b_ada": f(inputs["b_ada"][0].reshape(1, -1)),
        "g_mixT": f(inputs["g_mix"][0].reshape(16, 128).T),
        "w_in": f(inputs["w_in"][0]),
        "w_kw": f(inputs["w_in"][0][:, 3584:3664]),
        "w_pool": f(inputs["w_pool"][0]),
        "pool_scaleT": f(inputs["pool_scale"][0].reshape(8, 128).T),
        "rel_bias": f(inputs["rel_bias"]),
        "w_out": f(inputs["w_out"][0]),
        "g_ffn": f(inputs["g_ffn"][0].reshape(1, -1)),
        "wr": f(np.concatenate([inputs["w_group"][0], inputs["w_router"][0], np.zeros((D, 28), np.float32)], axis=1).reshape(16, 128, 64).transpose(1, 0, 2)),
        "br": f(np.broadcast_to(np.concatenate([inputs["b_group"][0], inputs["b_router"][0]]).reshape(1, -1), (128, 36))),
        "g_final": f(inputs["g_final"].reshape(1, -1)),
    }
    wgl = inputs["w_gate"][0].reshape(32, 16, 128, 512).transpose(0, 2, 1, 3).reshape(32 * 128, 16, 512)
    wul = inputs["w_up"][0].reshape(32, 16, 128, 512).transpose(0, 2, 1, 3).reshape(32 * 128, 16, 512)
    wdl = inputs["w_down"][0].reshape(32, 4, 128, D).transpose(0, 2, 1, 3).reshape(32 * 128, 4, D)
    for j in range(4):
        m["w_gate_%d" % j] = f(wgl[:, 4 * j:4 * j + 4, :].reshape(32 * 128, 2048))
        m["w_up_%d" % j] = f(wul[:, 4 * j:4 * j + 4, :].reshape(32 * 128, 2048))
        m["w_down_%d" % j] = f(wdl[:, j, :])
    for k, v in consts.items():
        m["c_" + k] = v
    return m


def make_in_map(inputs, b, consts, shared=None):
    if shared is None:
        shared = make_shared(inputs, consts)
    m = dict(shared)
    m["x"] = np.ascontiguousarray(inputs["x"][b])
    m["cT"] = np.ascontiguousarray(inputs["c"][b].reshape(16, 128).T)
    return m


_CACHE = {}


def kernel(**inputs):
    inputs = {k: np.asarray(v) for k, v in inputs.items()}
    if "nc" not in _CACHE:
        nc, P, st, dbg = build()
        finish(nc, P, st, dbg.pop("__final__"))
        _CACHE["nc"] = nc
    nc = _CACHE["nc"]
    consts = host_consts()
    shared = make_shared(inputs, consts)
    in_maps = [make_in_map(inputs, b, consts, shared) for b in range(8)]
    res = run_bass_kernel_spmd(nc, in_maps, core_ids=list(range(8)))
    return np.stack([np.asarray(r["out"], dtype=np.float32) for r in res.results], axis=0)
```

```python
import numpy as np
import ml_dtypes
import concourse.bass as bass
import concourse.mybir as mybir
from concourse.bass_utils import run_bass_kernel_spmd

F32 = mybir.dt.float32
BF16 = mybir.dt.bfloat16
I32 = mybir.dt.int32
U32 = mybir.dt.uint32
ALU = mybir.AluOpType
AF = mybir.ActivationFunctionType
AX = mybir.AxisListType


class _Op:
    __slots__ = ("eng", "fn", "reads", "writes", "dma", "deps", "signal", "waits",
                 "sigval", "dsem", "dval", "after", "is_bar")

    def __init__(self, eng, fn, reads, writes, dma, after):
        self.eng = eng; self.fn = fn; self.reads = reads; self.writes = writes
        self.dma = dma; self.deps = (); self.signal = False; self.waits = []
        self.sigval = 0; self.dsem = None; self.dval = 0; self.after = after


class Prog:
    ENGS = ("pe", "dve", "act", "pool", "sp")

    def __init__(self, nc):
        self.nc = nc
        self.ops = []
        self.off = 16512
        self.nalloc = 0

    def sb(self, shape, dtype, name=None):
        esz = {F32: 4, BF16: 2, I32: 4, U32: 4}[dtype]
        n = 1
        for s in shape[1:]:
            n *= s
        nbytes = (n * esz + 63) // 64 * 64
        self.nalloc += 1
        name = "%s_%d" % (name or "t", self.nalloc)
        t = self.nc.alloc_sbuf_tensor_at(name, list(shape), dtype, offset=self.off)
        self.off += nbytes
        assert self.off <= 229376, ("sbuf overflow", self.off)
        return t

    def add(self, eng, fn, reads=(), writes=(), dma=False, after=()):
        self.ops.append(_Op(eng, fn, tuple(reads), tuple(writes), dma, tuple(after)))
        return len(self.ops) - 1

    def dma(self, q, out, in_, reads=(), writes=(), **kw):
        return self.add(q, lambda e: e.dma_start(out=out, in_=in_, **kw), reads, writes, dma=True)

    def barrier(self):
        deps = [i for i, o in enumerate(self.ops) if o.dma]
        for e in ("pe", "dve", "act", "pool"):
            for i in range(len(self.ops) - 1, -1, -1):
                o = self.ops[i]
                if o.eng == e and not o.dma and o.fn is not None and not getattr(o, "is_bar", False):
                    deps.append(i)
                    break
        src, dst = self.bar_src, self.bar_dst
        b = self.add("sp", lambda en: en.dma_start(out=dst, in_=src), dma=True, after=deps)
        for e in ("pe", "dve", "act", "pool"):
            self.add(e, None, after=[b])

    def emit(self):
        nc = self.nc
        ops = self.ops
        last_w = {}
        readers = {}
        for i, op in enumerate(ops):
            deps = set(op.after)
            for t in op.reads:
                if t in last_w:
                    deps.add(last_w[t])
            for t in op.writes:
                if t in last_w:
                    deps.add(last_w[t])
                deps.update(readers.get(t, ()))
            for t in op.reads:
                readers.setdefault(t, []).append(i)
            for t in op.writes:
                last_w[t] = i
                readers[t] = []
            deps.discard(i)
            op.deps = deps
        seen = {e: {e2: -1 for e2 in self.ENGS} for e in self.ENGS}
        dma_seen = {e: set() for e in self.ENGS}
        NQ = {"sp": 20, "act": 8, "pool": 16}
        NOUT = {"sp": 20, "act": 8, "pool": 10}
        dma_count = {q: 0 for q in NQ}
        dma_hist = {q: [] for q in NQ}
        for i, op in enumerate(ops):
            E = op.eng
            waits = []
            if op.dma:
                k = dma_count[E]
                dma_count[E] += 1
                dma_hist[E].append(i)
                if k >= NOUT[E]:
                    prev = dma_hist[E][k - NOUT[E]]
                    if prev not in dma_seen[E]:
                        waits.append(("d", prev))
                        dma_seen[E].add(prev)
                op.dsem = (E, k % NQ[E])
                op.dval = 16 * (k // NQ[E] + 1)
            for j in sorted(op.deps):
                oj = ops[j]
                if oj.dma:
                    if j not in dma_seen[E]:
                        dma_seen[E].add(j)
                        waits.append(("d", j))
                else:
                    E2 = oj.eng
                    if E2 == E and E == "pe" and not op.dma:
                        continue
                    if seen[E][E2] >= j:
                        continue
                    seen[E][E2] = j
                    oj.signal = True
                    waits.append(("c", j))
            op.waits = waits
        CH = 1000
        cnt = {e: 0 for e in self.ENGS}
        for op in ops:
            if op.signal:
                op.sigval = (cnt[op.eng] // CH, cnt[op.eng] % CH + 1)
                cnt[op.eng] += 1
        self.stats = dict(cnt)
        self.stats["nops"] = len(ops)
        from contextlib import ExitStack
        with ExitStack() as st:
            csem = {}
            for e in self.ENGS:
                for c in range(cnt[e] // CH + 1):
                    csem[(e, c)] = st.enter_context(nc.semaphore("c_%s_%d" % (e, c)))
            dsem = {}
            for q, n in NQ.items():
                for k in range(n):
                    dsem[(q, k)] = st.enter_context(nc.semaphore("d_%s_%d" % (q, k)))
            block = st.enter_context(nc.Block())

            def run(engname):
                def body(eng):
                    for op in ops:
                        if op.eng != engname:
                            continue
                        for kind, j in op.waits:
                            oj = ops[j]
                            if kind == "d":
                                eng.wait_ge(dsem[oj.dsem], oj.dval)
                            else:
                                eng.wait_ge(csem[(oj.eng, oj.sigval[0])], oj.sigval[1])
                        if op.fn is None:
                            assert not op.signal and not op.dma
                            continue
                        ins = op.fn(eng)
                        if op.dma:
                            ins.then_inc(dsem[op.dsem], 16)
                        elif op.signal:
                            ins.then_inc(csem[(op.eng, op.sigval[0])], 1)
                return body

            block.tensor(run("pe"))
            block.vector(run("dve"))
            block.scalar(run("act"))
            block.gpsimd(run("pool"))
            block.sync(run("sp"))


S = 2048
D = 2048
NT = 16
NBLK = 63
NSLOT = NBLK * 128
SM_SCALE = 128 ** -0.5


def host_consts():
    c = {}
    c["ident_f"] = np.eye(128, dtype=np.float32)
    c["ident_b"] = np.eye(128, dtype=np.float32).astype(ml_dtypes.bfloat16)
    q = np.arange(128)[:, None]; s = np.arange(128)[None, :]
    c["causal_neg"] = np.where(s <= q, 0.0, -1e30).astype(np.float32)
    c["ltri"] = (q < s).astype(np.float32).astype(ml_dtypes.bfloat16)
    c["ones_b"] = np.ones((128, 128), np.float32).astype(ml_dtypes.bfloat16)
    c["ones_f"] = np.ones((128, 128), np.float32)
    c["anti_f"] = np.eye(128, dtype=np.float32)[::-1].copy()
    corr = np.ones((4, 16), np.float32)
    for g, w in enumerate((2, 4, 8, 16)):
        for t in range(16):
            corr[g, t] = w / min(t + 1, w)
    c["corr"] = np.broadcast_to(corr[None], (128, 4, 16)).copy()
    oh = np.zeros((32, 384), np.float32)
    for m in range(383):
        r = max(m - 127, 0)
        if r < 16:
            b = r
        else:
            rf = np.float32(max(r, 1))
            b = 16 + int(np.float32(np.log(rf / np.float32(16)) / np.float32(np.log(8.0)) * np.float32(16)))
            b = min(b, 31)
        oh[b, m] = 1.0
    c["bucket_oh"] = oh
    e1 = np.arange(32)[:, None]; e2 = np.arange(32)[None, :]
    c["u_strict"] = (e1 < e2).astype(np.float32)
    c["u_incl"] = (e1 <= e2).astype(np.float32)
    c["thr16"] = np.broadcast_to((np.arange(16) * 128).astype(np.float32)[None], (32, 16)).copy()
    bg = np.broadcast_to(np.arange(NBLK, dtype=np.float32)[None, :, None], (128, NBLK, 32)).copy()
    c["bgrid"] = bg
    return c


CONST_SPECS = [("ident_f", [128, 128], F32), ("ident_b", [128, 128], BF16), ("causal_neg", [128, 128], F32),
               ("ltri", [128, 128], BF16), ("ones_b", [128, 128], BF16), ("ones_f", [128, 128], F32), ("anti_f", [128, 128], F32),
               ("corr", [128, 4, 16], F32), ("bucket_oh", [32, 384], F32), ("u_strict", [32, 32], F32),
               ("u_incl", [32, 32], F32), ("thr16", [32, 16], F32), ("bgrid", [128, NBLK, 32], F32)]


def w_in_view_(w_in, c0, n):
    return w_in[:, c0:c0 + n].rearrange("(kc p) n -> p kc n", p=128)


def build(stop=None):
    nc = bass.Bass("TRN2", target_bir_lowering=False)

    def din(name, shape, dt=F32):
        return nc.dram_tensor(name, list(shape), dt, kind="ExternalInput").ap()

    def dscratch(name, shape, dt=F32):
        return nc.dram_tensor(name, list(shape), dt, kind="Internal").ap()

    x = din("x", [S, D]); cT = din("cT", [128, 16]); w_ada = din("w_ada", [D, 6 * D]); b_ada = din("b_ada", [1, 6 * D])
    w_kw = din("w_kw", [D, 80])
    g_mixT = din("g_mixT", [128, 16]); w_in = din("w_in", [D, 3664]); w_pool = din("w_pool", [4, 256, 256])
    pool_scaleT = din("pool_scaleT", [128, 8]); rel_bias = din("rel_bias", [32, 8]); w_out = din("w_out", [D, D])
    g_ffn = din("g_ffn", [1, D]); wr = din("wr", [128, 16, 64]); br = din("br", [128, 36])
    NE_DECL = 32 if stop in (None, "E", "F") else 1
    w_gate = [din("w_gate_%d" % j, [NE_DECL * 128, 2048]) for j in range(4)]
    w_up = [din("w_up_%d" % j, [NE_DECL * 128, 2048]) for j in range(4)]
    w_down = [din("w_down_%d" % j, [NE_DECL * 128, 2048]) for j in range(4)]
    g_final = din("g_final", [1, D])
    cst = {n: din("c_" + n, shp, dt) for n, shp, dt in CONST_SPECS}
    out = nc.dram_tensor("out", [S, D], F32, kind="ExternalOutput").ap()
    dbg = {}

    def dbg_out(name, shape, dt=F32):
        dbg[name] = nc.dram_tensor("dbg_" + name, list(shape), dt, kind="ExternalOutput").ap()
        return dbg[name]

    modd = dscratch("modd", [1, 6 * D])
    fextd = dscratch("fextd", [8, 384])
    x1d = dscratch("x1d", [S, D])
    h2d = dscratch("h2d", [S, D], BF16)
    Xs = dscratch("Xs", [NSLOT, D], BF16)
    Ys = dscratch("Ys", [NSLOT, D])

    P = Prog(nc)
    bar_t = P.sb([1, 16], F32, "bar_t")
    P.add("pool", lambda e: e.memset(bar_t[:], 0.0), writes=["bar_t"])
    P.bar_src = bar_t[:]
    P.bar_dst = dscratch("bar_d", [1, 16])
    from contextlib import ExitStack
    st = ExitStack()
    ps = [st.enter_context(nc.psum_tensor("ps%d" % k, [128, 512], F32)) for k in range(6)]
    pb = [st.enter_context(nc.psum_tensor("pb%d" % k, [128, 1024], BF16)) for k in range(2)]
    PS = ["ps%d" % k for k in range(6)]
    PB = ["pb0", "pb1"]

    ident_f = P.sb([128, 128], F32, "ident_f"); ident_b = P.sb([128, 128], BF16, "ident_b")
    causal_neg = P.sb([128, 128], F32, "causal_neg"); ltri = P.sb([128, 128], BF16, "ltri")
    ones_b = P.sb([128, 128], BF16, "ones_b"); ones_f = P.sb([128, 128], F32, "ones_f")
    corr = P.sb([128, 4, 16], F32, "corr")
    for n, t in (("ident_f", ident_f), ("ident_b", ident_b), ("causal_neg", causal_neg), ("ltri", ltri),
                 ("ones_b", ones_b), ("ones_f", ones_f), ("corr", corr)):
        P.dma("sp", t[:], cst[n], writes=[n])
    modT = P.sb([128, 96], F32, "modT")
    s1T = P.sb([128, 16], F32, "s1T")
    gmT = P.sb([128, 16], F32, "gmT")
    pscT = P.sb([128, 8], F32, "pscT")
    eps_t = P.sb([128, 1], F32, "eps")
    P.dma("sp", gmT[:], g_mixT, writes=["gmT"])
    P.dma("sp", pscT[:], pool_scaleT, writes=["pscT"])
    P.add("pool", lambda e: e.memset(eps_t[:], 1e-6), writes=["eps"])
    base_mark = P.off

    scT = P.sb([128, 16], F32, "scT")
    wa = [P.sb([128, 16, 512], F32, "wa%d" % k) for k in range(2)]
    mod_row = P.sb([1, 6 * D], F32, "mod_row")
    ba_row = P.sb([1, 6 * D], F32, "ba_row")
    gf_row = P.sb([1, D], F32, "gf_row")
    P.dma("sp", scT[:], cT, writes=["scT"])
    P.dma("sp", ba_row[:], b_ada, writes=["ba_row"])
    P.dma("sp", gf_row[:], g_ffn, writes=["gf_row"])
    P.add("act", lambda e: e.activation(out=scT[:], in_=scT[:], func=AF.Silu), reads=["scT"], writes=["scT"])
    zt = P.sb([128, D], BF16, "zt")
    P.add("pool", lambda e: e.memset(zt[:], 0.0), writes=["zt"])
    for b in range(NBLK):
        P.dma("pool", Xs[b * 128:(b + 1) * 128, :], zt[:], reads=["zt"], writes=["Xs_zero_%d" % b])
    for n in range(24):
        wt = wa[n % 2]; wn = "wa%d" % (n % 2)
        P.dma("sp" if n % 2 == 0 else "act", wt[:], w_ada[:, n * 512:(n + 1) * 512].rearrange("(kc p) n -> p kc n", p=128), writes=[wn])
        pt = ps[n % 2]; pn = PS[n % 2]
        for kc in range(16):
            P.add("pe", lambda e, pt=pt, wt=wt, kc=kc: e.matmul(pt[0:1, :], lhsT=scT[:, kc:kc + 1], rhs=wt[:, kc, :], start=(kc == 0), stop=(kc == 15)),
                  reads=["scT", wn], writes=[pn])
        P.add("dve", lambda e, pt=pt, n=n: e.tensor_tensor(out=mod_row[0:1, n * 512:(n + 1) * 512], in0=pt[0:1, :], in1=ba_row[0:1, n * 512:(n + 1) * 512], op=ALU.add),
              reads=[pn, "ba_row"], writes=["mod_row"])
    P.add("dve", lambda e: e.scalar_tensor_tensor(out=mod_row[0:1, 4 * D:5 * D], in0=mod_row[0:1, 4 * D:5 * D], scalar=1.0, in1=gf_row[0:1, :], op0=ALU.add, op1=ALU.mult),
          reads=["mod_row", "gf_row"], writes=["mod_row"])
    P.dma("sp", modd, mod_row[:], reads=["mod_row"], writes=["modd"])
    for j in range(96):
        P.add("pe", lambda e, j=j: e.matmul(ps[2][:, j:j + 1], lhsT=mod_row[0:1, j * 128:(j + 1) * 128], rhs=ones_f[0:1, 0:1], start=True, stop=True),
              reads=["mod_row", "ones_f"], writes=[PS[2]])
    P.add("dve", lambda e: e.tensor_copy(out=modT[:], in_=ps[2][:, 0:96]), reads=[PS[2]], writes=["modT"])
    P.add("dve", lambda e: e.scalar_tensor_tensor(out=s1T[:], in0=modT[:, 16:32], scalar=1.0, in1=gmT[:], op0=ALU.add, op1=ALU.mult),
          reads=["modT", "gmT"], writes=["s1T"])
    if stop == "A":
        d = dbg_out("modT", [128, 96])
        P.dma("sp", d, modT[:], reads=["modT"], writes=["dbg"])
        return nc, P, st, dbg
    P.barrier()
    P.off = base_mark

    hT = P.sb([128, 16, S], BF16, "hT")
    proj_mark = P.off
    xs = [P.sb([128, D], F32, "xs%d" % k) for k in range(2)]
    xn = [P.sb([128, D], BF16, "xn%d" % k) for k in range(2)]
    junk = P.sb([128, D], BF16, "junk")
    ss = P.sb([128, 16], F32, "ss"); rt = P.sb([128, 16], F32, "rt"); rstd = P.sb([128, 16], F32, "rstd")
    P.add("pool", lambda e: e.memset(ss[:], 0.0), writes=["ss"])
    for i in range(NT):
        xt = xs[i % 2]; xtn = "xs%d" % (i % 2); xb = xn[i % 2]; xbn = "xn%d" % (i % 2)
        P.dma("sp", xt[:], x[i * 128:(i + 1) * 128, :], writes=[xtn])
        P.add("act", lambda e, xt=xt, i=i: e.activation(out=junk[:], in_=xt[:], func=AF.Square, accum_out=ss[:, i:i + 1]),
              reads=[xtn, "ss"], writes=["junk", "ss"])
        P.add("act", lambda e, i=i: e.activation(out=rt[:, i:i + 1], in_=ss[:, i:i + 1], func=AF.Sqrt, bias=eps_t[:, 0:1], scale=1.0 / D),
              reads=["ss", "eps"], writes=["rt"])
        P.add("dve", lambda e, i=i: e.reciprocal(out=rstd[:, i:i + 1], in_=rt[:, i:i + 1]), reads=["rt"], writes=["rstd"])
        P.add("dve", lambda e, xt=xt, xb=xb, i=i: e.tensor_scalar(out=xb[:], in0=xt[:], scalar1=rstd[:, i:i + 1], scalar2=None, op0=ALU.mult),
              reads=[xtn, "rstd"], writes=[xbn])
        for c in range(16):
            pbt = pb[c // 8]; pbn = PB[c // 8]
            P.add("pe", lambda e, xb=xb, c=c, pbt=pbt: e.transpose(out=pbt[:, (c % 8) * 128:(c % 8 + 1) * 128], in_=xb[:, c * 128:(c + 1) * 128], identity=ident_b[:]),
                  reads=[xbn, "ident_b"], writes=[pbn])
        for c in range(16):
            pbt = pb[c // 8]; pbn = PB[c // 8]
            P.add("act", lambda e, c=c, i=i, pbt=pbt: e.activation(out=hT[:, c, i * 128:(i + 1) * 128], in_=pbt[:, (c % 8) * 128:(c % 8 + 1) * 128], func=AF.Identity,
                                                                   bias=modT[:, c:c + 1], scale=s1T[:, c:c + 1]),
                  reads=[pbn, "modT", "s1T"], writes=["hT_%d" % i])
    HT = ["hT_%d" % i for i in range(NT)]
    if stop == "B1":
        d = dbg_out("hT", [128, 16, S], BF16)
        P.dma("sp", d, hT[:], reads=HT, writes=["dbg"])
        return nc, P, st, dbg
    P.barrier()
    P.off = proj_mark

    uT = P.sb([128, 8, S], BF16, "uT"); qT = P.sb([128, 8, S], BF16, "qT"); qiT = P.sb([128, 8, S], BF16, "qiT")
    kT = P.sb([128, 2, S], BF16, "kT"); vtok = P.sb([128, NT, 256], BF16, "vtok"); kiT2 = P.sb([128, S], BF16, "kiT2")
    wi_all = P.sb([128, NT, 16], F32, "wi_all")
    after_proj_mark = P.off
    wst = [P.sb([128, 16, 272], BF16, "wst%d" % k) for k in range(2)]
    kw_st = P.sb([128, 16, 80], F32, "kw_st")
    P.dma("sp", kw_st[:], w_kw.rearrange("(kc p) n -> p kc n", p=128), writes=["kw_st"])
    evac_n = [0]

    def evac(dst, src, reads, writes, eng=None):
        k = evac_n[0]; evac_n[0] += 1
        if eng == "act" or (eng is None and k % 2 == 0):
            P.add("act", lambda e: e.copy(out=dst, in_=src), reads=reads, writes=writes)
        else:
            P.add("dve", lambda e: e.tensor_copy(out=dst, in_=src), reads=reads, writes=writes)

    def w_in_view(c0, n):
        return w_in_view_(w_in, c0, n)

    groups = []
    for g in range(4):
        groups.append((g * 256, "fm", [(uT, 2 * g), (uT, 2 * g + 1)]))
    for g in range(4):
        groups.append((1024 + g * 256, "fm", [(qT, 2 * g), (qT, 2 * g + 1)]))
    groups.append((2048, "fm", [(kT, 0), (kT, 1)]))
    for g in range(4):
        groups.append((2560 + g * 256, "fm", [(qiT, 2 * g), (qiT, 2 * g + 1)]))
    groups.append((3584, "ki", None))
    groups.append((2304, "tm", None))
    gi = 0
    pk = 0
    for c0, kind, dests in groups:
        if stop == "B2a" and gi == 1:
            d = dbg_out("uT01", [128, 2, S], BF16)
            P.dma("sp", d, uT[:, 0:2, :], reads=[uT.name + "_0", uT.name + "_1"], writes=["dbg_uT01"])
            return nc, P, st, dbg
        if stop == "B2c" and kind == "tm":
            d = dbg_out("kiT2", [128, S], BF16)
            P.dma("sp", d, kiT2[:], reads=["kiT2"], writes=["dbg_kiT2"])
            return nc, P, st, dbg
        if stop == "B2b" and kind == "ki":
            d = dbg_out("qiT", [128, 8, S], BF16)
            P.dma("sp", d, qiT[:], reads=[qiT.name + "_%d" % c for c in range(8)], writes=["dbg_qiT"])
            return nc, P, st, dbg
        wt = wst[gi % 2]; wn = "wst%d" % (gi % 2); gi += 1
        if kind == "fm":
            P.dma("pool", wt[:, :, 0:256], w_in_view(c0, 256), writes=[wn])
            for sub, (dt_, ch) in enumerate(dests):
                for tc in range(4):
                    pt = ps[pk % 4]; pn = PS[pk % 4]; pk += 1
                    for kc in range(16):
                        P.add("pe", lambda e, pt=pt, wt=wt, kc=kc, sub=sub, tc=tc: e.matmul(pt[:], lhsT=wt[:, kc, sub * 128:(sub + 1) * 128], rhs=hT[:, kc, tc * 512:(tc + 1) * 512], start=(kc == 0), stop=(kc == 15)),
                              reads=[wn] + HT[tc * 4:tc * 4 + 4], writes=[pn])
                    evac(dt_[:, ch, tc * 512:(tc + 1) * 512], pt[:], [pn], [dt_.name + "_%d" % ch])
        elif kind == "ki":
            P.add("dve", lambda e, wt=wt: e.tensor_copy(out=wt[:, :, 0:64], in_=kw_st[:, :, 0:64]), reads=["kw_st"], writes=[wn])
            P.add("dve", lambda e, wt=wt: e.tensor_copy(out=wt[:, :, 64:128], in_=kw_st[:, :, 0:64]), reads=["kw_st"], writes=[wn])
            for tc in range(4):
                pt = ps[pk % 4]; pn = PS[pk % 4]; pk += 1
                for kc in range(16):
                    P.add("pe", lambda e, pt=pt, wt=wt, kc=kc, tc=tc: e.matmul(pt[:], lhsT=wt[:, kc, 0:128], rhs=hT[:, kc, tc * 512:(tc + 1) * 512], start=(kc == 0), stop=(kc == 15)),
                          reads=[wn] + HT[tc * 4:tc * 4 + 4], writes=[pn])
                evac(kiT2[:, tc * 512:(tc + 1) * 512], pt[:], [pn], ["kiT2"])
        else:
            P.dma("pool", wt[:, :, 0:256], w_in_view(2304, 256), writes=[wn])
            P.add("dve", lambda e, wt=wt: e.tensor_copy(out=wt[:, :, 256:272], in_=kw_st[:, :, 64:80]), reads=["kw_st"], writes=[wn])
            for i in range(NT):
                pt = ps[pk % 4]; pn = PS[pk % 4]; pk += 1
                for kc in range(16):
                    P.add("pe", lambda e, pt=pt, wt=wt, kc=kc, i=i: e.matmul(pt[:, 0:256], lhsT=hT[:, kc, i * 128:(i + 1) * 128], rhs=wt[:, kc, 0:256], start=(kc == 0), stop=(kc == 15)),
                          reads=[wn, HT[i]], writes=[pn])
                P.add("act", lambda e, pt=pt, i=i: e.copy(out=vtok[:, i, :], in_=pt[:, 0:256]), reads=[pn], writes=["vtok"])
                pt2 = ps[4 + i % 2]; pn2 = PS[4 + i % 2]
                for kc in range(16):
                    P.add("pe", lambda e, pt2=pt2, wt=wt, kc=kc, i=i: e.matmul(pt2[:, 0:16], lhsT=hT[:, kc, i * 128:(i + 1) * 128], rhs=wt[:, kc, 256:272], start=(kc == 0), stop=(kc == 15)),
                          reads=[wn, HT[i]], writes=[pn2])
                P.add("dve", lambda e, pt2=pt2, i=i: e.tensor_copy(out=wi_all[:, i, :], in_=pt2[:, 0:16]), reads=[pn2], writes=["wi_all"])
    UT = [uT.name + "_%d" % c for c in range(8)]; QT = [qT.name + "_%d" % c for c in range(8)]
    QIT = [qiT.name + "_%d" % c for c in range(8)]; KT = [kT.name + "_%d" % c for c in range(2)]
    if stop == "B2":
        for nm, t, toks, shp, dt_ in (("uT", uT, UT, [128, 8, S], BF16), ("qT", qT, QT, [128, 8, S], BF16), ("qiT", qiT, QIT, [128, 8, S], BF16),
                                      ("kT", kT, KT, [128, 2, S], BF16), ("vtok", vtok, ["vtok"], [128, NT, 256], BF16),
                                      ("kiT2", kiT2, ["kiT2"], [128, S], BF16), ("wi_all", wi_all, ["wi_all"], [128, NT, 16], F32)):
            d = dbg_out(nm, shp, dt_)
            P.dma("sp", d, t[:], reads=toks, writes=["dbg_" + nm])
        return nc, P, st, dbg
    P.barrier()
    P.off = base_mark
    pool_outT = P.sb([128, 8, S], BF16, "pool_outT"); attn_outT = P.sb([128, 8, S], BF16, "attn_outT")
    POT = ["pool_outT_%d" % c for c in range(8)]; AOT = ["attn_outT_%d" % i for i in range(NT)]
    P.off = after_proj_mark
    A_ = P.sb([128, S], F32, "poolA"); B_ = P.sb([128, S], F32, "poolB"); wp = P.sb([128, 4, 2, 256], BF16, "wp")
    P.dma("pool", wp[:], w_pool.rearrange("g (cc p) d -> p g cc d", p=128), writes=["wp"])
    for c in range(8):
        g = c // 2; w = 2 << g
        u = uT[:, c, :]; un = UT[c]
        P.add("dve", lambda e, u=u: e.tensor_tensor(out=A_[:, 1:S], in0=u[:, 1:S], in1=u[:, 0:S - 1], op=ALU.add), reads=[un], writes=["poolA"])
        P.add("dve", lambda e, u=u: e.tensor_copy(out=A_[:, 0:1], in_=u[:, 0:1]), reads=[un], writes=["poolA"])
        cur, curn, oth, othn = A_, "poolA", B_, "poolB"
        d = 2
        while d < w:
            P.add("dve", lambda e, cur=cur, oth=oth, d=d: e.tensor_tensor(out=oth[:, d:S], in0=cur[:, d:S], in1=cur[:, 0:S - d], op=ALU.add), reads=[curn], writes=[othn])
            P.add("dve", lambda e, cur=cur, oth=oth, d=d: e.tensor_copy(out=oth[:, 0:d], in_=cur[:, 0:d]), reads=[curn], writes=[othn])
            cur, curn, oth, othn = oth, othn, cur, curn
            d *= 2
        P.add("dve", lambda e, cur=cur, g=g: e.tensor_tensor(out=cur[:, 0:16], in0=cur[:, 0:16], in1=corr[:, g, :], op=ALU.mult), reads=[curn, "corr"], writes=[curn])
        P.add("dve", lambda e, cur=cur, u=u, w=w: e.scalar_tensor_tensor(out=u, in0=cur[:], scalar=1.0 / w, in1=u, op0=ALU.mult, op1=ALU.subtract), reads=[curn, un], writes=[un])
        if c % 2 == 1:
            for dch in range(2):
                for tc in range(4):
                    pt = ps[pk % 4]; pn = PS[pk % 4]; pk += 1
                    for cc in range(2):
                        P.add("pe", lambda e, pt=pt, g=g, cc=cc, dch=dch, tc=tc: e.matmul(pt[:], lhsT=wp[:, g, cc, dch * 128:(dch + 1) * 128], rhs=uT[:, 2 * g + cc, tc * 512:(tc + 1) * 512], start=(cc == 0), stop=(cc == 1)),
                              reads=["wp", UT[2 * g], UT[2 * g + 1]], writes=[pn])
                    P.add("act", lambda e, pt=pt, g=g, dch=dch, tc=tc: e.activation(out=pool_outT[:, 2 * g + dch, tc * 512:(tc + 1) * 512], in_=pt[:], func=AF.Identity, scale=pscT[:, 2 * g + dch:2 * g + dch + 1]),
                          reads=[pn, "pscT"], writes=[POT[2 * g + dch]])
    if stop == "B3":
        d = dbg_out("pool_outT", [128, 8, S], BF16)
        P.dma("sp", d, pool_outT[:], reads=POT, writes=["dbg_pool_outT"])
        return nc, P, st, dbg
    P.barrier()

    P.off = proj_mark
    acc = P.sb([128, S], F32, "acc"); maskq = P.sb([128, S], BF16, "maskq"); junkb = P.sb([128, S], BF16, "junkb")
    maskT = P.sb([128, NT, 128], BF16, "maskT"); EB3 = P.sb([128, 8, 384], F32, "EB3")
    assert P.off == proj_mark + 32768
    P.off = after_proj_mark
    rtile = [P.sb([128, 512], F32, "rtile%d" % k) for k in range(2)]
    Et = [P.sb([128, 512], BF16, "Et%d" % k) for k in range(2)]
    P1 = [P.sb([128, 512], BF16, "P1%d" % k) for k in range(2)]
    Pt = [P.sb([128, 512], BF16, "Pt%d" % k) for k in range(2)]
    rinv = P.sb([128, 512], F32, "rinv")
    lo = P.sb([128, 1], F32, "lo"); hi = P.sb([128, 1], F32, "hi"); mid = P.sb([128, 1], F32, "mid"); cnt = P.sb([128, 1], F32, "cnt")
    ge = P.sb([128, 1], U32, "ge"); lt = P.sb([128, 1], U32, "lt")
    relb = P.sb([32, 8], F32, "relb"); boh = P.sb([32, 384], F32, "boh"); fext = P.sb([8, 384], F32, "fext")
    hank = P.sb([128, 256], F32, "hank"); anti = P.sb([128, 128], F32, "anti")
    P.dma("sp", relb[:], rel_bias, writes=["relb"]); P.dma("sp", boh[:], cst["bucket_oh"], writes=["boh"])
    P.dma("sp", anti[:], cst["anti_f"], writes=["anti"])
    P.add("pe", lambda e: e.matmul(ps[0][0:8, 0:384], lhsT=relb[:], rhs=boh[:], start=True, stop=True), reads=["relb", "boh"], writes=[PS[0]])
    P.add("dve", lambda e: e.tensor_copy(out=fext[:], in_=ps[0][0:8, 0:384]), reads=[PS[0]], writes=["fext"])
    P.dma("sp", fextd, fext[:], reads=["fext"], writes=["fextd"])
    for h in range(8):
        P.dma("sp", hank[:], bass.AP(fextd.tensor, h * 384, [[1, 128], [1, 256]]), reads=["fextd"], writes=["hank"])
        P.add("pe", lambda e: e.matmul(ps[1][:, 0:256], lhsT=anti[:], rhs=hank[:], start=True, stop=True), reads=["anti", "hank"], writes=[PS[1]])
        P.add("act", lambda e, h=h: e.activation(out=EB3[:, h, 0:256], in_=ps[1][:, 0:256], func=AF.Exp), reads=[PS[1]], writes=["EB3"])
        P.add("act", lambda e, h=h: e.activation(out=EB3[:, h, 256:384], in_=ps[1][:, 255:256].to_broadcast([128, 128]), func=AF.Exp), reads=[PS[1]], writes=["EB3"])
    NIT = 15
    pow2 = P.sb([128, NIT], F32, "pow2"); wk_all = P.sb([128, NIT], F32, "wk_all"); sgh = P.sb([128, 1], F32, "sgh")
    for it in range(NIT):
        P.add("pool", lambda e, it=it: e.memset(pow2[:, it:it + 1], 0.5 ** (it + 1)), writes=["pow2"])
    ek = 0
    for i in range(NT):
        nk = (i + 1) * 128
        nch = (nk + 511) // 512
        for h in range(16):
            pr, hh = h // 2, h % 2
            b0 = 64 * hh
            for kc in range(nch):
                wd = min(512, nk - kc * 512)
                pt = ps[ek % 2]; pn = PS[ek % 2]; rtl = rtile[ek % 2]; rn = "rtile%d" % (ek % 2); ek += 1
                P.add("pe", lambda e, pt=pt, pr=pr, b0=b0, i=i, kc=kc, wd=wd: e.matmul(pt[:, 0:wd], lhsT=qiT[b0:b0 + 64, pr, i * 128:(i + 1) * 128], rhs=kiT2[b0:b0 + 64, kc * 512:kc * 512 + wd], start=True, stop=True),
                      reads=[QIT[pr], "kiT2"], writes=[pn])
                P.add("act", lambda e, pt=pt, rtl=rtl, wd=wd: e.activation(out=rtl[:, 0:wd], in_=pt[:, 0:wd], func=AF.Relu), reads=[pn], writes=[rn])
                if h == 0:
                    P.add("dve", lambda e, rtl=rtl, kc=kc, wd=wd, i=i, h=h: e.tensor_scalar(out=acc[:, kc * 512:kc * 512 + wd], in0=rtl[:, 0:wd], scalar1=wi_all[:, i, h:h + 1], scalar2=None, op0=ALU.mult),
                          reads=[rn, "wi_all"], writes=["acc"])
                else:
                    P.add("dve", lambda e, rtl=rtl, kc=kc, wd=wd, i=i, h=h: e.scalar_tensor_tensor(out=acc[:, kc * 512:kc * 512 + wd], in0=rtl[:, 0:wd], scalar=wi_all[:, i, h:h + 1], in1=acc[:, kc * 512:kc * 512 + wd], op0=ALU.mult, op1=ALU.add),
                          reads=[rn, "wi_all", "acc"], writes=["acc"])
        P.add("dve", lambda e, i=i: e.tensor_tensor(out=acc[:, i * 128:(i + 1) * 128], in0=acc[:, i * 128:(i + 1) * 128], in1=causal_neg[:], op=ALU.add), reads=["acc", "causal_neg"], writes=["acc"])
        if i >= 2:
            P.add("dve", lambda e, nk=nk: e.tensor_reduce(out=hi[:], in_=acc[:, 0:nk], axis=AX.X, op=ALU.max), reads=["acc"], writes=["hi"])
            P.add("dve", lambda e, i=i: e.tensor_reduce(out=lo[:], in_=acc[:, 0:i * 128], axis=AX.X, op=ALU.min), reads=["acc"], writes=["lo"])
            P.add("dve", lambda e: e.tensor_tensor(out=hi[:], in0=hi[:], in1=lo[:], op=ALU.subtract), reads=["hi", "lo"], writes=["hi"])
            P.add("dve", lambda e: e.tensor_scalar(out=wk_all[:], in0=pow2[:], scalar1=hi[:, 0:1], scalar2=None, op0=ALU.mult), reads=["hi", "pow2"], writes=["wk_all"])
            P.add("dve", lambda e: e.scalar_tensor_tensor(out=mid[:], in0=hi[:], scalar=0.5, in1=lo[:], op0=ALU.mult, op1=ALU.add), reads=["lo", "hi"], writes=["mid"])
            for it in range(NIT):
                P.add("dve", lambda e, nk=nk: e.tensor_scalar(out=junkb[:, 0:nk], in0=acc[:, 0:nk], scalar1=mid[:, 0:1], scalar2=None, op0=ALU.is_ge, op1=ALU.add, accum_out=cnt[:, 0:1]),
                      reads=["acc", "mid"], writes=["junkb", "cnt"])
                P.add("dve", lambda e: e.tensor_scalar(out=sgh[:], in0=cnt[:], scalar1=255.5, scalar2=0.5, op0=ALU.is_ge, op1=ALU.subtract), reads=["cnt"], writes=["sgh"])
                P.add("dve", lambda e, it=it: e.scalar_tensor_tensor(out=mid[:], in0=sgh[:], scalar=wk_all[:, it:it + 1], in1=mid[:], op0=ALU.mult, op1=ALU.add), reads=["sgh", "wk_all", "mid"], writes=["mid"])
            P.add("dve", lambda e: e.scalar_tensor_tensor(out=lo[:], in0=hi[:], scalar=-(0.5 ** (NIT + 1)), in1=mid[:], op0=ALU.mult, op1=ALU.add), reads=["hi", "mid"], writes=["lo"])
            P.add("dve", lambda e, nk=nk: e.tensor_scalar(out=maskq[:, 0:nk], in0=acc[:, 0:nk], scalar1=lo[:, 0:1], scalar2=None, op0=ALU.is_ge), reads=["acc", "lo"], writes=["maskq"])
        else:
            P.add("dve", lambda e, nk=nk: e.tensor_scalar(out=maskq[:, 0:nk], in0=acc[:, 0:nk], scalar1=-1e29, scalar2=None, op0=ALU.is_ge), reads=["acc"], writes=["maskq"])
        for j0 in range(0, i + 1, 8):
            jj = list(range(j0, min(i + 1, j0 + 8)))
            pbt = pb[(j0 // 8) % 2]; pbn = PB[(j0 // 8) % 2]
            for j in jj:
                P.add("pe", lambda e, j=j, pbt=pbt: e.transpose(out=pbt[:, (j % 8) * 128:(j % 8 + 1) * 128], in_=maskq[:, j * 128:(j + 1) * 128], identity=ident_b[:]), reads=["maskq", "ident_b"], writes=[pbn])
            for j in jj:
                P.add("act", lambda e, j=j, pbt=pbt: e.copy(out=maskT[:, j, :], in_=pbt[:, (j % 8) * 128:(j % 8 + 1) * 128]), reads=[pbn], writes=["maskT"])
        for kv in range(2):
            for j in range(i + 1):
                off = 0 if j == i else (128 if j == i - 1 else 256)
                pS = ps[2 + ek % 2]; pSn = PS[2 + ek % 2]
                et = Et[ek % 2]; etn = "Et%d" % (ek % 2); p1 = P1[ek % 2]; p1n = "P1%d" % (ek % 2); ptt = Pt[ek % 2]; ptn = "Pt%d" % (ek % 2); ek += 1
                P.add("pe", lambda e, pS=pS, kv=kv, j=j, i=i: e.matmul(pS[:], lhsT=kT[:, kv, j * 128:(j + 1) * 128], rhs=qT[:, 4 * kv:4 * kv + 4, i * 128:(i + 1) * 128], start=True, stop=True),
                      reads=[KT[kv]] + QT[4 * kv:4 * kv + 4], writes=[pSn])
                P.add("act", lambda e, pS=pS, et=et: e.activation(out=et[:], in_=pS[:], func=AF.Exp, scale=SM_SCALE), reads=[pSn], writes=[etn])
                P.add("pool", lambda e, et=et, p1=p1, j=j: e.tensor_tensor(out=p1[:].rearrange("p (h q) -> p h q", h=4), in0=et[:].rearrange("p (h q) -> p h q", h=4), in1=maskT[:, j:j + 1, :].to_broadcast([128, 4, 128]), op=ALU.mult),
                      reads=[etn, "maskT"], writes=[p1n])
                P.add("dve", lambda e, p1=p1, ptt=ptt, kv=kv, off=off: e.tensor_tensor(out=ptt[:].rearrange("p (h q) -> p h q", h=4), in0=p1[:].rearrange("p (h q) -> p h q", h=4), in1=EB3[:, 4 * kv:4 * kv + 4, off:off + 128], op=ALU.mult),
                      reads=[p1n, "EB3"], writes=[ptn])
                P.add("pe", lambda e, ptt=ptt, kv=kv, j=j, i=i: e.matmul(ps[4][:], lhsT=vtok[:, j, kv * 128:(kv + 1) * 128], rhs=ptt[:], start=(j == 0), stop=(j == i)), reads=["vtok", ptn], writes=[PS[4]])
                P.add("pe", lambda e, ptt=ptt, j=j, i=i: e.matmul(ps[5][:], lhsT=ones_b[:], rhs=ptt[:], start=(j == 0), stop=(j == i)), reads=["ones_b", ptn], writes=[PS[5]])
            P.add("dve", lambda e: e.reciprocal(out=rinv[:], in_=ps[5][:]), reads=[PS[5]], writes=["rinv"])
            P.add("dve", lambda e, kv=kv, i=i: e.tensor_tensor(out=attn_outT[:, 4 * kv:4 * kv + 4, i * 128:(i + 1) * 128], in0=ps[4][:].rearrange("p (h q) -> p h q", h=4), in1=rinv[:].rearrange("p (h q) -> p h q", h=4), op=ALU.mult),
                  reads=[PS[4], "rinv"], writes=[AOT[i]])
        if stop == "B4a" and i == 3:
            d = dbg_out("acc", [128, 512]); P.dma("sp", d, acc[:, 0:512], reads=["acc"], writes=["dbg_acc"])
            d = dbg_out("maskq", [128, 512], BF16); P.dma("sp", d, maskq[:, 0:512], reads=["maskq"], writes=["dbg_maskq"])
            d = dbg_out("thr", [128, 1]); P.dma("sp", d, lo[:], reads=["lo"], writes=["dbg_thr"])
            d = dbg_out("EB3", [128, 8, 384]); P.dma("sp", d, EB3[:], reads=["EB3"], writes=["dbg_EB3"])
            d = dbg_out("attn_outT", [128, 8, 512], BF16); P.dma("sp", d, attn_outT[:, :, 0:512], reads=AOT[0:4], writes=["dbg_attn_outT"])
            return nc, P, st, dbg
    if stop == "B4":
        d = dbg_out("attn_outT", [128, 8, S], BF16)
        P.dma("sp", d, attn_outT[:], reads=AOT, writes=["dbg_attn_outT"])
        return nc, P, st, dbg
    P.barrier()
    P.off = proj_mark
    O1 = P.sb([128, NT, 32], BF16, "O1"); O2 = P.sb([128, NT, 32], BF16, "O2"); Osum = P.sb([128, NT, 32], BF16, "Osum")
    g1 = P.sb([128, NT], F32, "g1"); g2 = P.sb([128, NT], F32, "g2")
    dest_i = P.sb([128, NT, 2], U32, "dest_i")
    blk_i = P.sb([128, NBLK], I32, "blk_i"); act_i = P.sb([128, NBLK], I32, "act_i"); widx = P.sb([128, NBLK], U32, "widx")
    route_mark = P.off
    wo = P.sb([128, 16, D], BF16, "wo")
    xt2 = [P.sb([128, D], F32, "xt2_%d" % k) for k in range(2)]
    gt1_bc = P.sb([128, D], F32, "gt1_bc"); s2_bc = P.sb([128, D], F32, "s2_bc"); sh2_bc = P.sb([128, D], F32, "sh2_bc")
    h2 = P.sb([128, D], F32, "h2"); h2b = P.sb([128, D], BF16, "h2b"); h2T = P.sb([128, 16, 128], F32, "h2T")
    wr_sb = P.sb([128, 16, 64], F32, "wr_sb"); br_bc = P.sb([128, 36], F32, "br_bc")
    ss2 = P.sb([128, NT], F32, "ss2"); rt2 = P.sb([128, NT], F32, "rt2"); rstd2 = P.sb([128, NT], F32, "rstd2")
    L_all = P.sb([128, NT, 36], F32, "L_all")
    for n in range(4):
        P.dma("pool", wo[:, :, n * 512:(n + 1) * 512], w_out[:, n * 512:(n + 1) * 512].rearrange("(kc p) n -> p kc n", p=128), writes=["wo"])
    P.dma("sp", gt1_bc[:], modd[0:1, 2 * D:3 * D].to_broadcast([128, D]), reads=["modd"], writes=["gt1_bc"])
    P.dma("sp", sh2_bc[:], modd[0:1, 3 * D:4 * D].to_broadcast([128, D]), reads=["modd"], writes=["sh2_bc"])
    P.dma("sp", s2_bc[:], modd[0:1, 4 * D:5 * D].to_broadcast([128, D]), reads=["modd"], writes=["s2_bc"])
    P.dma("sp", wr_sb[:], wr, writes=["wr_sb"])
    P.dma("sp", br_bc[:], br, writes=["br_bc"])
    P.add("pool", lambda e: e.memset(ss2[:], 0.0), writes=["ss2"])

    for i in range(NT):
        xt = xt2[i % 2]; xtn = "xt2_%d" % (i % 2)
        P.dma("sp", xt[:], x[i * 128:(i + 1) * 128, :], writes=[xtn])
        for n in range(4):
            for kc in range(16):
                src = pool_outT if kc < 8 else attn_outT
                srcn = POT[kc] if kc < 8 else AOT[i]
                P.add("pe", lambda e, n=n, kc=kc, src=src, i=i: e.matmul(ps[n][:], lhsT=src[:, kc % 8, i * 128:(i + 1) * 128], rhs=wo[:, kc, n * 512:(n + 1) * 512], start=(kc == 0), stop=(kc == 15)),
                      reads=[srcn, "wo"], writes=[PS[n]])
            P.add("dve", lambda e, n=n: e.tensor_tensor(out=h2[:, n * 512:(n + 1) * 512], in0=ps[n][:], in1=gt1_bc[:, n * 512:(n + 1) * 512], op=ALU.mult), reads=[PS[n], "gt1_bc"], writes=["h2"])
            P.add("pool", lambda e, n=n, xt=xt: e.tensor_tensor(out=xt[:, n * 512:(n + 1) * 512], in0=xt[:, n * 512:(n + 1) * 512], in1=h2[:, n * 512:(n + 1) * 512], op=ALU.add), reads=[xtn, "h2"], writes=[xtn])
        P.dma("sp", x1d[i * 128:(i + 1) * 128, :], xt[:], reads=[xtn], writes=["x1d_%d" % i])
        P.add("act", lambda e, xt=xt, i=i: e.activation(out=h2b[:], in_=xt[:], func=AF.Square, accum_out=ss2[:, i:i + 1]), reads=[xtn, "ss2"], writes=["h2b", "ss2"])
        P.add("act", lambda e, i=i: e.activation(out=rt2[:, i:i + 1], in_=ss2[:, i:i + 1], func=AF.Sqrt, bias=eps_t[:, 0:1], scale=1.0 / D), reads=["ss2", "eps"], writes=["rt2"])
        P.add("dve", lambda e, i=i: e.reciprocal(out=rstd2[:, i:i + 1], in_=rt2[:, i:i + 1]), reads=["rt2"], writes=["rstd2"])
        P.add("dve", lambda e, xt=xt, i=i: e.scalar_tensor_tensor(out=h2[:], in0=xt[:], scalar=rstd2[:, i:i + 1], in1=s2_bc[:], op0=ALU.mult, op1=ALU.mult), reads=[xtn, "rstd2", "s2_bc", "h2"], writes=["h2"])
        P.add("pool", lambda e: e.tensor_tensor(out=h2[:], in0=h2[:], in1=sh2_bc[:], op=ALU.add), reads=["h2", "sh2_bc"], writes=["h2"])
        P.add("act", lambda e: e.copy(out=h2b[:], in_=h2[:]), reads=["h2"], writes=["h2b"])
        P.dma("sp", h2d[i * 128:(i + 1) * 128, :], h2b[:], reads=["h2b"], writes=["h2d_%d" % i])
        for c0 in range(0, 16, 4):
            for c in range(c0, c0 + 4):
                P.add("pe", lambda e, c=c: e.transpose(out=ps[4][:, (c % 4) * 128:(c % 4 + 1) * 128], in_=h2[:, c * 128:(c + 1) * 128], identity=ident_f[:]), reads=["h2", "ident_f"], writes=[PS[4]])
            for c in range(c0, c0 + 4):
                evac(h2T[:, c, :], ps[4][:, (c % 4) * 128:(c % 4 + 1) * 128], [PS[4]], ["h2T"], eng=("act" if (c0 // 4) % 2 == 0 else "dve"))
        for c in range(16):
            P.add("pe", lambda e, c=c: e.matmul(ps[5][:, 0:64], lhsT=h2T[:, c, :], rhs=wr_sb[:, c, :], start=(c == 0), stop=(c == 15)), reads=["h2T", "wr_sb"], writes=[PS[5]])
        P.add("dve", lambda e, i=i: e.tensor_tensor(out=L_all[:, i, :], in0=ps[5][:, 0:36], in1=br_bc[:], op=ALU.add), reads=[PS[5], "br_bc"], writes=["L_all"])
    def bc(t, k):
        return t[:].unsqueeze(2).to_broadcast([128, NT, k])
    R = {n: P.sb([128, NT], F32, "R_" + n) for n in ("gmax", "gsum", "pg", "m1", "m2", "dd", "e2", "den", "rden")}
    R3 = {n: P.sb([128, NT, k], F32, "R3_" + n) for n, k in (("og", 4), ("eg", 4), ("es", 8), ("tmp", 8), ("o1", 8), ("es2", 8), ("o2", 8))}
    Lg = L_all[:, :, 0:4]
    def rop(eng, fn, reads, writes):
        P.add(eng, fn, reads=reads, writes=writes)
    rop("dve", lambda e: e.tensor_reduce(out=R["gmax"][:], in_=Lg, axis=AX.X, op=ALU.max), ["L_all"], ["gmax"])
    rop("dve", lambda e: e.tensor_tensor(out=R3["og"][:], in0=Lg, in1=bc(R["gmax"], 4), op=ALU.is_ge), ["L_all", "gmax"], ["og"])
    rop("dve", lambda e: e.tensor_tensor(out=R3["eg"][:], in0=Lg, in1=bc(R["gmax"], 4), op=ALU.subtract), ["L_all", "gmax"], ["eg"])
    rop("act", lambda e: e.activation(out=R3["eg"][:], in_=R3["eg"][:], func=AF.Exp), ["eg"], ["eg"])
    rop("dve", lambda e: e.tensor_reduce(out=R["gsum"][:], in_=R3["eg"][:], axis=AX.X, op=ALU.add), ["eg"], ["gsum"])
    rop("dve", lambda e: e.reciprocal(out=R["pg"][:], in_=R["gsum"][:]), ["gsum"], ["pg"])
    rop("dve", lambda e: e.tensor_tensor(out=R3["es"][:], in0=L_all[:, :, 4:12], in1=R3["og"][:, :, 0:1].to_broadcast([128, NT, 8]), op=ALU.mult), ["L_all", "og"], ["es"])
    for g in range(1, 4):
        rop("dve", lambda e, g=g: e.tensor_tensor(out=R3["tmp"][:], in0=L_all[:, :, 4 + 8 * g:12 + 8 * g], in1=R3["og"][:, :, g:g + 1].to_broadcast([128, NT, 8]), op=ALU.mult), ["L_all", "og"], ["tmp"])
        rop("dve", lambda e: e.tensor_tensor(out=R3["es"][:], in0=R3["es"][:], in1=R3["tmp"][:], op=ALU.add), ["es", "tmp"], ["es"])
    rop("dve", lambda e: e.tensor_reduce(out=R["m1"][:], in_=R3["es"][:], axis=AX.X, op=ALU.max), ["es"], ["m1"])
    rop("dve", lambda e: e.tensor_tensor(out=R3["o1"][:], in0=R3["es"][:], in1=bc(R["m1"], 8), op=ALU.is_ge), ["es", "m1"], ["o1"])
    rop("dve", lambda e: e.scalar_tensor_tensor(out=R3["es2"][:], in0=R3["o1"][:], scalar=-1e30, in1=R3["es"][:], op0=ALU.mult, op1=ALU.add), ["o1", "es"], ["es2"])
    rop("dve", lambda e: e.tensor_reduce(out=R["m2"][:], in_=R3["es2"][:], axis=AX.X, op=ALU.max), ["es2"], ["m2"])
    rop("dve", lambda e: e.tensor_tensor(out=R3["o2"][:], in0=R3["es2"][:], in1=bc(R["m2"], 8), op=ALU.is_ge), ["es2", "m2"], ["o2"])
    rop("dve", lambda e: e.tensor_tensor(out=R["dd"][:], in0=R["m2"][:], in1=R["m1"][:], op=ALU.subtract), ["m1", "m2"], ["dd"])
    rop("act", lambda e: e.activation(out=R["e2"][:], in_=R["dd"][:], func=AF.Exp), ["dd"], ["e2"])
    rop("dve", lambda e: e.tensor_scalar(out=R["den"][:], in0=R["e2"][:], scalar1=1.0, scalar2=None, op0=ALU.add), ["e2"], ["den"])
    rop("dve", lambda e: e.reciprocal(out=R["rden"][:], in_=R["den"][:]), ["den"], ["rden"])
    rop("dve", lambda e: e.tensor_tensor(out=g1[:], in0=R["pg"][:], in1=R["rden"][:], op=ALU.mult), ["pg", "rden"], ["g1"])
    rop("dve", lambda e: e.tensor_tensor(out=g2[:], in0=g1[:], in1=R["e2"][:], op=ALU.mult), ["g1", "e2"], ["g2"])
    for g in range(4):
        rop("dve", lambda e, g=g: e.tensor_tensor(out=O1[:, :, 8 * g:8 * g + 8], in0=R3["o1"][:], in1=R3["og"][:, :, g:g + 1].to_broadcast([128, NT, 8]), op=ALU.mult), ["o1", "og"], ["O1"])
        rop("dve", lambda e, g=g: e.tensor_tensor(out=O2[:, :, 8 * g:8 * g + 8], in0=R3["o2"][:], in1=R3["og"][:, :, g:g + 1].to_broadcast([128, NT, 8]), op=ALU.mult), ["o2", "og"], ["O2"])
    rop("dve", lambda e: e.tensor_tensor(out=Osum[:], in0=O1[:], in1=O2[:], op=ALU.add), ["O1", "O2"], ["Osum"])
    X1D = ["x1d_%d" % i for i in range(NT)]; H2D = ["h2d_%d" % i for i in range(NT)]
    if stop == "C":
        for nm, t, shp, dt_ in (("O1", O1, [128, NT, 32], BF16), ("O2", O2, [128, NT, 32], BF16), ("g1", g1, [128, NT], F32), ("g2", g2, [128, NT], F32)):
            d = dbg_out(nm, shp, dt_); P.dma("sp", d, t[:], reads=[nm], writes=["dbg_" + nm])
        d = dbg_out("x1", [S, D]); P.dma("sp", d, x1d, reads=X1D, writes=["dbg_x1"])
        d = dbg_out("h2", [S, D], BF16); P.dma("sp", d, h2d, reads=H2D, writes=["dbg_h2"])
        return nc, P, st, dbg
    P.barrier()
    P.off = route_mark
    us_sb = P.sb([32, 32], F32, "us_sb"); ui_sb = P.sb([32, 32], F32, "ui_sb"); thr16 = P.sb([32, 16], F32, "thr16")
    cmp16 = P.sb([32, 16], F32, "cmp16"); nblkT = P.sb([32, 1], F32, "nblkT"); nb_bc = P.sb([32, 128], F32, "nb_bc")
    pstart_s = P.sb([128, 32], F32, "pstart_s"); pend_sb = P.sb([128, 32], F32, "pend_sb")
    bgrid = P.sb([128, NBLK, 32], F32, "bgrid"); cmpb = P.sb([128, NBLK, 32], F32, "cmpb")
    blkf = P.sb([128, NBLK], F32, "blkf"); actf = P.sb([128, NBLK], F32, "actf")
    pidx = P.sb([128, 1], I32, "pidx"); pidxf = P.sb([128, 1], F32, "pidxf"); widxf = P.sb([128, NBLK], F32, "widxf")
    destf = P.sb([128, 32], F32, "destf"); dtmp = P.sb([128, 32], F32, "dtmp"); d12 = P.sb([128, 2], F32, "d12")
    h2bt = [P.sb([128, D], BF16, "h2bt%d" % k) for k in range(2)]
    P.dma("sp", us_sb[:], cst["u_strict"], writes=["us_sb"]); P.dma("sp", ui_sb[:], cst["u_incl"], writes=["ui_sb"])
    P.dma("sp", thr16[:], cst["thr16"], writes=["thr16"]); P.dma("sp", bgrid[:], cst["bgrid"], writes=["bgrid"])
    for i in range(NT):
        P.add("pe", lambda e, i=i: e.matmul(ps[0][0:32, 0:128], lhsT=Osum[:, i, :], rhs=ones_b[:], start=(i == 0), stop=(i == NT - 1)), reads=["Osum", "ones_b"], writes=[PS[0]])
    P.add("dve", lambda e: e.tensor_tensor(out=cmp16[:], in0=ps[0][0:32, 0:1].to_broadcast([32, 16]), in1=thr16[:], op=ALU.is_gt), reads=[PS[0], "thr16"], writes=["cmp16"])
    P.add("dve", lambda e: e.tensor_reduce(out=nblkT[:], in_=cmp16[:], axis=AX.X, op=ALU.add), reads=["cmp16"], writes=["nblkT"])
    P.add("dve", lambda e: e.tensor_scalar(out=nb_bc[:], in0=ones_f[0:32, :], scalar1=nblkT[:, 0:1], scalar2=None, op0=ALU.mult), reads=["nblkT", "ones_f"], writes=["nb_bc"])
    P.add("pe", lambda e: e.matmul(ps[1][:, 0:32], lhsT=nb_bc[:], rhs=us_sb[:], start=True, stop=True), reads=["nb_bc", "us_sb"], writes=[PS[1]])
    P.add("pe", lambda e: e.matmul(ps[1][:, 32:64], lhsT=nb_bc[:], rhs=ui_sb[:], start=True, stop=True), reads=["nb_bc", "ui_sb"], writes=[PS[1]])
    P.add("dve", lambda e: e.tensor_scalar(out=pstart_s[:], in0=ps[1][:, 0:32], scalar1=128.0, scalar2=None, op0=ALU.mult), reads=[PS[1]], writes=["pstart_s"])
    P.add("dve", lambda e: e.tensor_copy(out=pend_sb[:], in_=ps[1][:, 32:64]), reads=[PS[1]], writes=["pend_sb"])
    P.add("dve", lambda e: e.tensor_tensor(out=cmpb[:], in0=pend_sb[:].unsqueeze(1).to_broadcast([128, NBLK, 32]), in1=bgrid[:], op=ALU.is_le), reads=["pend_sb", "bgrid"], writes=["cmpb"])
    P.add("dve", lambda e: e.tensor_reduce(out=blkf[:], in_=cmpb[:], axis=AX.X, op=ALU.add), reads=["cmpb"], writes=["blkf"])
    P.add("dve", lambda e: e.tensor_scalar(out=blkf[:], in0=blkf[:], scalar1=31.0, scalar2=None, op0=ALU.min), reads=["blkf"], writes=["blkf"])
    P.add("dve", lambda e: e.tensor_copy(out=blk_i[:], in_=blkf[:]), reads=["blkf"], writes=["blk_i"])
    P.add("dve", lambda e: e.tensor_scalar(out=actf[:], in0=bgrid[:, :, 0], scalar1=pend_sb[:, 31:32], scalar2=None, op0=ALU.is_lt), reads=["bgrid", "pend_sb"], writes=["actf"])
    P.add("dve", lambda e: e.tensor_copy(out=act_i[:], in_=actf[:]), reads=["actf"], writes=["act_i"])
    P.add("pool", lambda e: e.iota(pidx[:], pattern=[[0, 1]], base=0, channel_multiplier=1), writes=["pidx"])
    P.add("dve", lambda e: e.tensor_copy(out=pidxf[:], in_=pidx[:]), reads=["pidx"], writes=["pidxf"])
    P.add("dve", lambda e: e.tensor_scalar(out=widxf[:], in0=blkf[:], scalar1=128.0, scalar2=pidxf[:, 0:1], op0=ALU.mult, op1=ALU.add), reads=["blkf", "pidxf"], writes=["widxf"])
    P.add("dve", lambda e: e.tensor_scalar(out=actf[:], in0=actf[:], scalar1=-1.0, scalar2=-8192.0, op0=ALU.add, op1=ALU.mult), reads=["actf"], writes=["actf"])
    P.add("dve", lambda e: e.tensor_tensor(out=widxf[:], in0=widxf[:], in1=actf[:], op=ALU.add), reads=["widxf", "actf"], writes=["widxf"])
    P.add("dve", lambda e: e.tensor_copy(out=widx[:], in_=widxf[:]), reads=["widxf"], writes=["widx"])
    for i in range(NT):
        pt = ps[2 + i % 2]; pn = PS[2 + i % 2]
        for j in range(i + 1):
            P.add("pe", lambda e, pt=pt, i=i, j=j: e.matmul(pt[:, 0:32], lhsT=(ones_b if j < i else ltri)[:], rhs=Osum[:, j, :], start=(j == 0), stop=(j == i)), reads=["Osum", "ones_b", "ltri"], writes=[pn])
        P.add("dve", lambda e, pt=pt: e.tensor_tensor(out=destf[:], in0=pt[:, 0:32], in1=pstart_s[:], op=ALU.add), reads=[pn, "pstart_s"], writes=["destf"])
        for a, Oa, on in ((0, O1, "O1"), (1, O2, "O2")):
            P.add("dve", lambda e, Oa=Oa, i=i: e.tensor_tensor(out=dtmp[:], in0=destf[:], in1=Oa[:, i, :], op=ALU.mult), reads=["destf", on], writes=["dtmp"])
            P.add("dve", lambda e, a=a: e.tensor_reduce(out=d12[:, a:a + 1], in_=dtmp[:], axis=AX.X, op=ALU.add), reads=["dtmp"], writes=["d12"])
        P.add("dve", lambda e, i=i: e.tensor_copy(out=dest_i[:, i, :], in_=d12[:]), reads=["d12"], writes=["dest_i_%d" % i])
        hb = h2bt[i % 2]; hbn = "h2bt%d" % (i % 2)
        P.dma("sp", hb[:], h2d[i * 128:(i + 1) * 128, :], reads=[H2D[i]], writes=[hbn])
        for a in range(2):
            P.add("pool", lambda e, hb=hb, i=i, a=a: e.indirect_dma_start(out=Xs, out_offset=bass.IndirectOffsetOnAxis(ap=dest_i[:, i, a:a + 1], axis=0), in_=hb[:], in_offset=None),
                  reads=[hbn, "dest_i_%d" % i] + ["Xs_zero_%d" % bb for bb in range(NBLK)], writes=["Xs_s_%d_%d" % (i, a)], dma=True)
    DEST = ["dest_i_%d" % i for i in range(NT)]
    XS = ["Xs_s_%d_%d" % (i, a) for i in range(NT) for a in range(2)]
    if stop == "D":
        d = dbg_out("dest", [128, NT, 2], U32); P.dma("sp", d, dest_i[:], reads=DEST, writes=["dbg_dest"])
        d = dbg_out("blk", [128, NBLK], I32); P.dma("sp", d, blk_i[:], reads=["blk_i"], writes=["dbg_blk"])
        d = dbg_out("act", [128, NBLK], I32); P.dma("sp", d, act_i[:], reads=["act_i"], writes=["dbg_act"])
        d = dbg_out("Xs", [NSLOT, D], BF16); P.dma("sp", d, Xs, reads=XS, writes=["dbg_Xs"])
        return nc, P, st, dbg
    P.barrier()

    P.off = route_mark
    wg = [P.sb([128, 16, 512], BF16, "wg%d" % k) for k in range(2)]
    wu = [P.sb([128, 16, 512], BF16, "wu%d" % k) for k in range(2)]
    wd = [P.sb([128, 4, D], BF16, "wd%d" % k) for k in range(2)]
    xe = [P.sb([128, D], BF16, "xe%d" % k) for k in range(2)]
    xeT = [P.sb([128, 16, 128], BF16, "xeT%d" % k) for k in range(2)]
    sg = P.sb([128, 512], F32, "sg"); a_bf = P.sb([128, 512], BF16, "a_bf"); aT = P.sb([128, 4, 128], BF16, "aT")
    yb = [P.sb([128, D], F32, "yb%d" % k) for k in range(2)]
    ET = mybir.EngineType
    bcreg = {}

    def mk_bc(e):
        bcreg["r"] = e.alloc_register("wbound")
        return e.reg_mov(bcreg["r"], 32 * 128 - 1)
    P.add("pool", mk_bc)
    for b in range(NBLK):
        k = b % 2
        for wt_, wsrc, wn_, is_down in ((wg[k], w_gate, "wg%d" % k, False), (wu[k], w_up, "wu%d" % k, False), (wd[k], w_down, "wd%d" % k, True)):
            for j in range(4):
                dst = wt_[:, j, :] if is_down else wt_[:, 4 * j:4 * j + 4, :].rearrange("p c n -> p (c n)")
                P.add("pool", lambda e, dst=dst, src=wsrc[j], b=b: e.indirect_dma_start(out=dst, out_offset=None, in_=src, in_offset=bass.IndirectOffsetOnAxis(ap=widx[:, b:b + 1], axis=0),
                                                                                     bounds_check=bcreg["r"], oob_is_err=False),
                      reads=["widx"], writes=[wn_ + "_%d" % j], dma=True)
        P.dma("sp", xe[k][:], Xs[b * 128:(b + 1) * 128, :], reads=XS, writes=["xe%d" % k])
        for c in range(16):
            sl = c % 8; pbt = pb[c // 8]
            P.add("pe", lambda e, c=c, sl=sl, pbt=pbt, k=k: e.transpose(out=pbt[:, sl * 128:(sl + 1) * 128], in_=xe[k][:, c * 128:(c + 1) * 128], identity=ident_b[:]), reads=["xe%d" % k, "ident_b"], writes=[PB[c // 8]])
        for c in range(16):
            sl = c % 8; pbt = pb[c // 8]
            evac(xeT[k][:, c, :], pbt[:, sl * 128:(sl + 1) * 128], [PB[c // 8]], ["xeT%d" % k], eng=("act" if c < 8 else "dve"))
        for c in range(16):
            P.add("pe", lambda e, c=c, k=k: e.matmul(ps[0][:], lhsT=xeT[k][:, c, :], rhs=wg[k][:, c, :], start=(c == 0), stop=(c == 15)), reads=["xeT%d" % k, "wg%d_%d" % (k, c // 4)], writes=[PS[0]])
        for c in range(16):
            P.add("pe", lambda e, c=c, k=k: e.matmul(ps[1][:], lhsT=xeT[k][:, c, :], rhs=wu[k][:, c, :], start=(c == 0), stop=(c == 15)), reads=["xeT%d" % k, "wu%d_%d" % (k, c // 4)], writes=[PS[1]])
        P.add("act", lambda e: e.activation(out=sg[:], in_=ps[0][:], func=AF.Silu), reads=[PS[0]], writes=["sg"])
        P.add("dve", lambda e: e.tensor_tensor(out=a_bf[:], in0=sg[:], in1=ps[1][:], op=ALU.mult), reads=["sg", PS[1]], writes=["a_bf"])
        for f_ in range(4):
            P.add("pe", lambda e, f_=f_: e.transpose(out=pb[0][:, f_ * 128:(f_ + 1) * 128], in_=a_bf[:, f_ * 128:(f_ + 1) * 128], identity=ident_b[:]), reads=["a_bf", "ident_b"], writes=[PB[0]])
        for f_ in range(4):
            evac(aT[:, f_, :], pb[0][:, f_ * 128:(f_ + 1) * 128], [PB[0]], ["aT"], eng="dve")
        for n in range(4):
            for f_ in range(4):
                P.add("pe", lambda e, n=n, f_=f_, k=k: e.matmul(ps[2 + n][:], lhsT=aT[:, f_, :], rhs=wd[k][:, f_, n * 512:(n + 1) * 512], start=(f_ == 0), stop=(f_ == 3)), reads=["aT", "wd%d_%d" % (k, f_)], writes=[PS[2 + n]])
            evac(yb[k][:, n * 512:(n + 1) * 512], ps[2 + n][:], [PS[2 + n]], ["yb%d" % k])
        P.dma("sp", Ys[b * 128:(b + 1) * 128, :], yb[k][:], reads=["yb%d" % k], writes=["Ys"])
    if stop == "E":
        for bi, b in enumerate((0, 10, 30)):
            P.dma("sp", yb[bi % 2][:], Ys[b * 128:(b + 1) * 128, :], reads=["Ys", "yb%d" % (bi % 2)], writes=["yb%d" % (bi % 2)])
            d = dbg_out("Ys%d" % b, [128, D]); P.dma("sp", d, yb[bi % 2][:], reads=["yb%d" % (bi % 2)], writes=["dbg_Ys%d" % b])
        return nc, P, st, dbg
    P.barrier()

    P.off = route_mark
    x1t = [P.sb([128, D], F32, "x1t%d" % k) for k in range(2)]
    y1 = [P.sb([128, D], F32, "y1_%d" % k) for k in range(2)]
    y2 = [P.sb([128, D], F32, "y2_%d" % k) for k in range(2)]
    ob = [P.sb([128, D], F32, "ob%d" % k) for k in range(2)]
    gt2_bc = P.sb([128, D], F32, "gt2_bc"); gfin_bc = P.sb([128, D], F32, "gfin_bc")
    junk3 = P.sb([128, D], BF16, "junk3")
    ss3 = P.sb([128, NT], F32, "ss3"); rt3 = P.sb([128, NT], F32, "rt3"); rstd3 = P.sb([128, NT], F32, "rstd3")
    P.dma("sp", gt2_bc[:], modd[0:1, 5 * D:6 * D].to_broadcast([128, D]), reads=["modd"], writes=["gt2_bc"])
    P.dma("sp", gfin_bc[:], g_final[0:1, :].to_broadcast([128, D]), writes=["gfin_bc"])
    P.add("pool", lambda e: e.memset(ss3[:], 0.0), writes=["ss3"])
    for i in range(NT):
        k = i % 2
        P.dma("sp", x1t[k][:], x1d[i * 128:(i + 1) * 128, :], reads=[X1D[i]], writes=["x1t%d" % k])
        for a, yt, yn in ((0, y1[k], "y1_%d" % k), (1, y2[k], "y2_%d" % k)):
            P.add("pool", lambda e, yt=yt, i=i, a=a: e.indirect_dma_start(out=yt[:], out_offset=None, in_=Ys, in_offset=bass.IndirectOffsetOnAxis(ap=dest_i[:, i, a:a + 1], axis=0)),
                  reads=["Ys", DEST[i]], writes=[yn], dma=True)
        P.add("dve", lambda e, k=k, i=i: e.tensor_scalar(out=y1[k][:], in0=y1[k][:], scalar1=g1[:, i:i + 1], scalar2=None, op0=ALU.mult), reads=["y1_%d" % k, "g1"], writes=["y1_%d" % k])
        P.add("dve", lambda e, k=k, i=i: e.scalar_tensor_tensor(out=y1[k][:], in0=y2[k][:], scalar=g2[:, i:i + 1], in1=y1[k][:], op0=ALU.mult, op1=ALU.add), reads=["y1_%d" % k, "y2_%d" % k, "g2"], writes=["y1_%d" % k])
        P.add("pool", lambda e, k=k: e.tensor_tensor(out=y1[k][:], in0=y1[k][:], in1=gt2_bc[:], op=ALU.mult), reads=["y1_%d" % k, "gt2_bc"], writes=["y1_%d" % k])
        P.add("pool", lambda e, k=k: e.tensor_tensor(out=x1t[k][:], in0=x1t[k][:], in1=y1[k][:], op=ALU.add), reads=["y1_%d" % k, "x1t%d" % k], writes=["x1t%d" % k])
        P.add("act", lambda e, k=k, i=i: e.activation(out=junk3[:], in_=x1t[k][:], func=AF.Square, accum_out=ss3[:, i:i + 1]), reads=["x1t%d" % k, "ss3"], writes=["junk3", "ss3"])
        P.add("act", lambda e, i=i: e.activation(out=rt3[:, i:i + 1], in_=ss3[:, i:i + 1], func=AF.Sqrt, bias=eps_t[:, 0:1], scale=1.0 / D), reads=["ss3", "eps"], writes=["rt3"])
        P.add("dve", lambda e, i=i: e.reciprocal(out=rstd3[:, i:i + 1], in_=rt3[:, i:i + 1]), reads=["rt3"], writes=["rstd3"])
        P.add("dve", lambda e, k=k, i=i: e.scalar_tensor_tensor(out=ob[k][:], in0=x1t[k][:], scalar=rstd3[:, i:i + 1], in1=gfin_bc[:], op0=ALU.mult, op1=ALU.mult), reads=["x1t%d" % k, "rstd3", "gfin_bc"], writes=["ob%d" % k])
        P.dma("sp", out[i * 128:(i + 1) * 128, :], ob[k][:], reads=["ob%d" % k], writes=["out_%d" % i])
    dbg["__final__"] = ["out_%d" % i for i in range(NT)]
    return nc, P, st, dbg


def finish(nc, P, st, out_tokens):
    P.add("sp", None, reads=list(out_tokens))
    P.emit()
    st.close()


def make_shared(inputs, consts):
    f = np.ascontiguousarray
    m = {
        "w_ada": f(inputs["w_ada"][0]),
        "b_ada": f(inputs["b_ada"][0].reshape(1, -1)),
        "g_mixT": f(inputs["g_mix"][0].reshape(16, 128).T),
        "w_in": f(inputs["w_in"][0]),
        "w_kw": f(inputs["w_in"][0][:, 3584:3664]),
        "w_pool": f(inputs["w_pool"][0]),
        "pool_scaleT": f(inputs["pool_scale"][0].reshape(8, 128).T),
        "rel_bias": f(inputs["rel_bias"]),
        "w_out": f(inputs["w_out"][0]),
        "g_ffn": f(inputs["g_ffn"][0].reshape(1, -1)),
        "wr": f(np.concatenate([inputs["w_group"][0], inputs["w_router"][0], np.zeros((D, 28), np.float32)], axis=1).reshape(16, 128, 64).transpose(1, 0, 2)),
        "br": f(np.broadcast_to(np.concatenate([inputs["b_group"][0], inputs["b_router"][0]]).reshape(1, -1), (128, 36))),
        "g_final": f(inputs["g_final"].reshape(1, -1)),
    }
    wgl = inputs["w_gate"][0].reshape(32, 16, 128, 512).transpose(0, 2, 1, 3).reshape(32 * 128, 16, 512)
    wul = inputs["w_up"][0].reshape(32, 16, 128, 512).transpose(0, 2, 1, 3).reshape(32 * 128, 16, 512)
    wdl = inputs["w_down"][0].reshape(32, 4, 128, D).transpose(0, 2, 1, 3).reshape(32 * 128, 4, D)
    for j in range(4):
        m["w_gate_%d" % j] = f(wgl[:, 4 * j:4 * j + 4, :].reshape(32 * 128, 2048))
        m["w_up_%d" % j] = f(wul[:, 4 * j:4 * j + 4, :].reshape(32 * 128, 2048))
        m["w_down_%d" % j] = f(wdl[:, j, :])
    for k, v in consts.items():
        m["c_" + k] = v
    return m


def make_in_map(inputs, b, consts, shared=None):
    if shared is None:
        shared = make_shared(inputs, consts)
    m = dict(shared)
    m["x"] = np.ascontiguousarray(inputs["x"][b])
    m["cT"] = np.ascontiguousarray(inputs["c"][b].reshape(16, 128).T)
    return m


_CACHE = {}


def kernel(**inputs):
    inputs = {k: np.asarray(v) for k, v in inputs.items()}
    if "nc" not in _CACHE:
        nc, P, st, dbg = build()
        finish(nc, P, st, dbg.pop("__final__"))
        _CACHE["nc"] = nc
    nc = _CACHE["nc"]
    consts = host_consts()
    shared = make_shared(inputs, consts)
    in_maps = [make_in_map(inputs, b, consts, shared) for b in range(8)]
    res = run_bass_kernel_spmd(nc, in_maps, core_ids=list(range(8)))
    return np.stack([np.asarray(r["out"], dtype=np.float32) for r in res.results], axis=0)
```

```python
import numpy as np
import ml_dtypes
import concourse.bass as bass
import concourse.mybir as mybir
from concourse.bass_utils import run_bass_kernel_spmd

F32 = mybir.dt.float32
BF16 = mybir.dt.bfloat16
I32 = mybir.dt.int32
U32 = mybir.dt.uint32
ALU = mybir.AluOpType
AF = mybir.ActivationFunctionType
AX = mybir.AxisListType


class _Op:
    __slots__ = ("eng", "fn", "reads", "writes", "dma", "deps", "signal", "waits",
                 "sigval", "dsem", "dval", "after", "is_bar")

    def __init__(self, eng, fn, reads, writes, dma, after):
        self.eng = eng; self.fn = fn; self.reads = reads; self.writes = writes
        self.dma = dma; self.deps = (); self.signal = False; self.waits = []
        self.sigval = 0; self.dsem = None; self.dval = 0; self.after = after


class Prog:
    ENGS = ("pe", "dve", "act", "pool", "sp")

    def __init__(self, nc):
        self.nc = nc
        self.ops = []
        self.off = 16512
        self.nalloc = 0

    def sb(self, shape, dtype, name=None):
        esz = {F32: 4, BF16: 2, I32: 4, U32: 4}[dtype]
        n = 1
        for s in shape[1:]:
            n *= s
        nbytes = (n * esz + 63) // 64 * 64
        self.nalloc += 1
        name = "%s_%d" % (name or "t", self.nalloc)
        t = self.nc.alloc_sbuf_tensor_at(name, list(shape), dtype, offset=self.off)
        self.off += nbytes
        assert self.off <= 229376, ("sbuf overflow", self.off)
        return t

    def add(self, eng, fn, reads=(), writes=(), dma=False, after=()):
        self.ops.append(_Op(eng, fn, tuple(reads), tuple(writes), dma, tuple(after)))
        return len(self.ops) - 1

    def dma(self, q, out, in_, reads=(), writes=(), **kw):
        return self.add(q, lambda e: e.dma_start(out=out, in_=in_, **kw), reads, writes, dma=True)

    def barrier(self):
        deps = [i for i, o in enumerate(self.ops) if o.dma]
        for e in ("pe", "dve", "act", "pool"):
            for i in range(len(self.ops) - 1, -1, -1):
                o = self.ops[i]
                if o.eng == e and not o.dma and o.fn is not None and not getattr(o, "is_bar", False):
                    deps.append(i)
                    break
        src, dst = self.bar_src, self.bar_dst
        b = self.add("sp", lambda en: en.dma_start(out=dst, in_=src), dma=True, after=deps)
        for e in ("pe", "dve", "act", "pool"):
            self.add(e, None, after=[b])

    def emit(self):
        nc = self.nc
        ops = self.ops
        last_w = {}
        readers = {}
        for i, op in enumerate(ops):
            deps = set(op.after)
            for t in op.reads:
                if t in last_w:
                    deps.add(last_w[t])
            for t in op.writes:
                if t in last_w:
                    deps.add(last_w[t])
                deps.update(readers.get(t, ()))
            for t in op.reads:
                readers.setdefault(t, []).append(i)
            for t in op.writes:
                last_w[t] = i
                readers[t] = []
            deps.discard(i)
            op.deps = deps
        seen = {e: {e2: -1 for e2 in self.ENGS} for e in self.ENGS}
        dma_seen = {e: set() for e in self.ENGS}
        NQ = {"sp": 20, "act": 8, "pool": 16}
        NOUT = {"sp": 20, "act": 8, "pool": 10}
        dma_count = {q: 0 for q in NQ}
        dma_hist = {q: [] for q in NQ}
        for i, op in enumerate(ops):
            E = op.eng
            waits = []
            if op.dma:
                k = dma_count[E]
                dma_count[E] += 1
                dma_hist[E].append(i)
                if k >= NOUT[E]:
                    prev = dma_hist[E][k - NOUT[E]]
                    if prev not in dma_seen[E]:
                        waits.append(("d", prev))
                        dma_seen[E].add(prev)
                op.dsem = (E, k % NQ[E])
                op.dval = 16 * (k // NQ[E] + 1)
            for j in sorted(op.deps):
                oj = ops[j]
                if oj.dma:
                    if j not in dma_seen[E]:
                        dma_seen[E].add(j)
                        waits.append(("d", j))
                else:
                    E2 = oj.eng
                    if E2 == E and E == "pe" and not op.dma:
                        continue
                    if seen[E][E2] >= j:
                        continue
                    seen[E][E2] = j
                    oj.signal = True
                    waits.append(("c", j))
            op.waits = waits
        CH = 1000
        cnt = {e: 0 for e in self.ENGS}
        for op in ops:
            if op.signal:
                op.sigval = (cnt[op.eng] // CH, cnt[op.eng] % CH + 1)
                cnt[op.eng] += 1
        self.stats = dict(cnt)
        self.stats["nops"] = len(ops)
        from contextlib import ExitStack
        with ExitStack() as st:
            csem = {}
            for e in self.ENGS:
                for c in range(cnt[e] // CH + 1):
                    csem[(e, c)] = st.enter_context(nc.semaphore("c_%s_%d" % (e, c)))
            dsem = {}
            for q, n in NQ.items():
                for k in range(n):
                    dsem[(q, k)] = st.enter_context(nc.semaphore("d_%s_%d" % (q, k)))
            block = st.enter_context(nc.Block())

            def run(engname):
                def body(eng):
                    for op in ops:
                        if op.eng != engname:
                            continue
                        for kind, j in op.waits:
                            oj = ops[j]
                            if kind == "d":
                                eng.wait_ge(dsem[oj.dsem], oj.dval)
                            else:
                                eng.wait_ge(csem[(oj.eng, oj.sigval[0])], oj.sigval[1])
                        if op.fn is None:
                            assert not op.signal and not op.dma
                            continue
                        ins = op.fn(eng)
                        if op.dma:
                            ins.then_inc(dsem[op.dsem], 16)
                        elif op.signal:
                            ins.then_inc(csem[(op.eng, op.sigval[0])], 1)
                return body

            block.tensor(run("pe"))
            block.vector(run("dve"))
            block.scalar(run("act"))
            block.gpsimd(run("pool"))
            block.sync(run("sp"))


S = 2048
D = 2048
NT = 16
NBLK = 63
NSLOT = NBLK * 128
SM_SCALE = 128 ** -0.5


def host_consts():
    c = {}
    c["ident_f"] = np.eye(128, dtype=np.float32)
    c["ident_b"] = np.eye(128, dtype=np.float32).astype(ml_dtypes.bfloat16)
    q = np.arange(128)[:, None]; s = np.arange(128)[None, :]
    c["causal_neg"] = np.where(s <= q, 0.0, -1e30).astype(np.float32)
    c["ltri"] = (q < s).astype(np.float32).astype(ml_dtypes.bfloat16)
    c["ones_b"] = np.ones((128, 128), np.float32).astype(ml_dtypes.bfloat16)
    c["ones_f"] = np.ones((128, 128), np.float32)
    c["anti_f"] = np.eye(128, dtype=np.float32)[::-1].copy()
    corr = np.ones((4, 16), np.float32)
    for g, w in enumerate((2, 4, 8, 16)):
        for t in range(16):
            corr[g, t] = w / min(t + 1, w)
    c["corr"] = np.broadcast_to(corr[None], (128, 4, 16)).copy()
    oh = np.zeros((32, 384), np.float32)
    for m in range(383):
        r = max(m - 127, 0)
        if r < 16:
            b = r
        else:
            rf = np.float32(max(r, 1))
            b = 16 + int(np.float32(np.log(rf / np.float32(16)) / np.float32(np.log(8.0)) * np.float32(16)))
            b = min(b, 31)
        oh[b, m] = 1.0
    c["bucket_oh"] = oh
    e1 = np.arange(32)[:, None]; e2 = np.arange(32)[None, :]
    c["u_strict"] = (e1 < e2).astype(np.float32)
    c["u_incl"] = (e1 <= e2).astype(np.float32)
    c["thr16"] = np.broadcast_to((np.arange(16) * 128).astype(np.float32)[None], (32, 16)).copy()
    bg = np.broadcast_to(np.arange(NBLK, dtype=np.float32)[None, :, None], (128, NBLK, 32)).copy()
    c["bgrid"] = bg
    return c


CONST_SPECS = [("ident_f", [128, 128], F32), ("ident_b", [128, 128], BF16), ("causal_neg", [128, 128], F32),
               ("ltri", [128, 128], BF16), ("ones_b", [128, 128], BF16), ("ones_f", [128, 128], F32), ("anti_f", [128, 128], F32),
               ("corr", [128, 4, 16], F32), ("bucket_oh", [32, 384], F32), ("u_strict", [32, 32], F32),
               ("u_incl", [32, 32], F32), ("thr16", [32, 16], F32), ("bgrid", [128, NBLK, 32], F32)]


def w_in_view_(w_in, c0, n):
    return w_in[:, c0:c0 + n].rearrange("(kc p) n -> p kc n", p=128)


def build(stop=None):
    nc = bass.Bass("TRN2", target_bir_lowering=False)

    def din(name, shape, dt=F32):
        return nc.dram_tensor(name, list(shape), dt, kind="ExternalInput").ap()

    def dscratch(name, shape, dt=F32):
        return nc.dram_tensor(name, list(shape), dt, kind="Internal").ap()

    x = din("x", [S, D]); cT = din("cT", [128, 16]); w_ada = din("w_ada", [D, 6 * D]); b_ada = din("b_ada", [1, 6 * D])
    w_kw = din("w_kw", [D, 80])
    g_mixT = din("g_mixT", [128, 16]); w_in = din("w_in", [D, 3664]); w_pool = din("w_pool", [4, 256, 256])
    pool_scaleT = din("pool_scaleT", [128, 8]); rel_bias = din("rel_bias", [32, 8]); w_out = din("w_out", [D, D])
    g_ffn = din("g_ffn", [1, D]); wr = din("wr", [128, 16, 64]); br = din("br", [128, 36])
    NE_DECL = 32 if stop in (None, "E", "F") else 1
    w_gate = [din("w_gate_%d" % j, [NE_DECL * 128, 2048]) for j in range(4)]
    w_up = [din("w_up_%d" % j, [NE_DECL * 128, 2048]) for j in range(4)]
    w_down = [din("w_down_%d" % j, [NE_DECL * 128, 2048]) for j in range(4)]
    g_final = din("g_final", [1, D])
    cst = {n: din("c_" + n, shp, dt) for n, shp, dt in CONST_SPECS}
    out = nc.dram_tensor("out", [S, D], F32, kind="ExternalOutput").ap()
    dbg = {}

    def dbg_out(name, shape, dt=F32):
        dbg[name] = nc.dram_tensor("dbg_" + name, list(shape), dt, kind="ExternalOutput").ap()
        return dbg[name]

    modd = dscratch("modd", [1, 6 * D])
    fextd = dscratch("fextd", [8, 384])
    x1d = dscratch("x1d", [S, D])
    h2d = dscratch("h2d", [S, D], BF16)
    Xs = dscratch("Xs", [NSLOT, D], BF16)
    Ys = dscratch("Ys", [NSLOT, D])

    P = Prog(nc)
    bar_t = P.sb([1, 16], F32, "bar_t")
    P.add("pool", lambda e: e.memset(bar_t[:], 0.0), writes=["bar_t"])
    P.bar_src = bar_t[:]
    P.bar_dst = dscratch("bar_d", [1, 16])
    from contextlib import ExitStack
    st = ExitStack()
    ps = [st.enter_context(nc.psum_tensor("ps%d" % k, [128, 512], F32)) for k in range(6)]
    pb = [st.enter_context(nc.psum_tensor("pb%d" % k, [128, 1024], BF16)) for k in range(2)]
    PS = ["ps%d" % k for k in range(6)]
    PB = ["pb0", "pb1"]

    ident_f = P.sb([128, 128], F32, "ident_f"); ident_b = P.sb([128, 128], BF16, "ident_b")
    causal_neg = P.sb([128, 128], F32, "causal_neg"); ltri = P.sb([128, 128], BF16, "ltri")
    ones_b = P.sb([128, 128], BF16, "ones_b"); ones_f = P.sb([128, 128], F32, "ones_f")
    corr = P.sb([128, 4, 16], F32, "corr")
    for n, t in (("ident_f", ident_f), ("ident_b", ident_b), ("causal_neg", causal_neg), ("ltri", ltri),
                 ("ones_b", ones_b), ("ones_f", ones_f), ("corr", corr)):
        P.dma("sp", t[:], cst[n], writes=[n])
    modT = P.sb([128, 96], F32, "modT")
    s1T = P.sb([128, 16], F32, "s1T")
    gmT = P.sb([128, 16], F32, "gmT")
    pscT = P.sb([128, 8], F32, "pscT")
    eps_t = P.sb([128, 1], F32, "eps")
    P.dma("sp", gmT[:], g_mixT, writes=["gmT"])
    P.dma("sp", pscT[:], pool_scaleT, writes=["pscT"])
    P.add("pool", lambda e: e.memset(eps_t[:], 1e-6), writes=["eps"])
    base_mark = P.off

    scT = P.sb([128, 16], F32, "scT")
    wa = [P.sb([128, 16, 512], F32, "wa%d" % k) for k in range(2)]
    mod_row = P.sb([1, 6 * D], F32, "mod_row")
    ba_row = P.sb([1, 6 * D], F32, "ba_row")
    gf_row = P.sb([1, D], F32, "gf_row")
    P.dma("sp", scT[:], cT, writes=["scT"])
    P.dma("sp", ba_row[:], b_ada, writes=["ba_row"])
    P.dma("sp", gf_row[:], g_ffn, writes=["gf_row"])
    P.add("act", lambda e: e.activation(out=scT[:], in_=scT[:], func=AF.Silu), reads=["scT"], writes=["scT"])
    zt = P.sb([128, D], BF16, "zt")
    P.add("pool", lambda e: e.memset(zt[:], 0.0), writes=["zt"])
    for b in range(NBLK):
        P.dma("pool", Xs[b * 128:(b + 1) * 128, :], zt[:], reads=["zt"], writes=["Xs_zero_%d" % b])
    for n in range(24):
        wt = wa[n % 2]; wn = "wa%d" % (n % 2)
        P.dma("sp" if n % 2 == 0 else "act", wt[:], w_ada[:, n * 512:(n + 1) * 512].rearrange("(kc p) n -> p kc n", p=128), writes=[wn])
        pt = ps[n % 2]; pn = PS[n % 2]
        for kc in range(16):
            P.add("pe", lambda e, pt=pt, wt=wt, kc=kc: e.matmul(pt[0:1, :], lhsT=scT[:, kc:kc + 1], rhs=wt[:, kc, :], start=(kc == 0), stop=(kc == 15)),
                  reads=["scT", wn], writes=[pn])
        P.add("dve", lambda e, pt=pt, n=n: e.tensor_tensor(out=mod_row[0:1, n * 512:(n + 1) * 512], in0=pt[0:1, :], in1=ba_row[0:1, n * 512:(n + 1) * 512], op=ALU.add),
              reads=[pn, "ba_row"], writes=["mod_row"])
    P.add("dve", lambda e: e.scalar_tensor_tensor(out=mod_row[0:1, 4 * D:5 * D], in0=mod_row[0:1, 4 * D:5 * D], scalar=1.0, in1=gf_row[0:1, :], op0=ALU.add, op1=ALU.mult),
          reads=["mod_row", "gf_row"], writes=["mod_row"])
    P.dma("sp", modd, mod_row[:], reads=["mod_row"], writes=["modd"])
    for j in range(96):
        P.add("pe", lambda e, j=j: e.matmul(ps[2][:, j:j + 1], lhsT=mod_row[0:1, j * 128:(j + 1) * 128], rhs=ones_f[0:1, 0:1], start=True, stop=True),
              reads=["mod_row", "ones_f"], writes=[PS[2]])
    P.add("dve", lambda e: e.tensor_copy(out=modT[:], in_=ps[2][:, 0:96]), reads=[PS[2]], writes=["modT"])
    P.add("dve", lambda e: e.scalar_tensor_tensor(out=s1T[:], in0=modT[:, 16:32], scalar=1.0, in1=gmT[:], op0=ALU.add, op1=ALU.mult),
          reads=["modT", "gmT"], writes=["s1T"])
    if stop == "A":
        d = dbg_out("modT", [128, 96])
        P.dma("sp", d, modT[:], reads=["modT"], writes=["dbg"])
        return nc, P, st, dbg
    P.barrier()
    P.off = base_mark

    hT = P.sb([128, 16, S], BF16, "hT")
    proj_mark = P.off
    xs = [P.sb([128, D], F32, "xs%d" % k) for k in range(2)]
    xn = [P.sb([128, D], BF16, "xn%d" % k) for k in range(2)]
    junk = P.sb([128, D], BF16, "junk")
    ss = P.sb([128, 16], F32, "ss"); rt = P.sb([128, 16], F32, "rt"); rstd = P.sb([128, 16], F32, "rstd")
    P.add("pool", lambda e: e.memset(ss[:], 0.0), writes=["ss"])
    for i in range(NT):
        xt = xs[i % 2]; xtn = "xs%d" % (i % 2); xb = xn[i % 2]; xbn = "xn%d" % (i % 2)
        P.dma("sp", xt[:], x[i * 128:(i + 1) * 128, :], writes=[xtn])
        P.add("act", lambda e, xt=xt, i=i: e.activation(out=junk[:], in_=xt[:], func=AF.Square, accum_out=ss[:, i:i + 1]),
              reads=[xtn, "ss"], writes=["junk", "ss"])
        P.add("act", lambda e, i=i: e.activation(out=rt[:, i:i + 1], in_=ss[:, i:i + 1], func=AF.Sqrt, bias=eps_t[:, 0:1], scale=1.0 / D),
              reads=["ss", "eps"], writes=["rt"])
        P.add("dve", lambda e, i=i: e.reciprocal(out=rstd[:, i:i + 1], in_=rt[:, i:i + 1]), reads=["rt"], writes=["rstd"])
        P.add("dve", lambda e, xt=xt, xb=xb, i=i: e.tensor_scalar(out=xb[:], in0=xt[:], scalar1=rstd[:, i:i + 1], scalar2=None, op0=ALU.mult),
              reads=[xtn, "rstd"], writes=[xbn])
        for c in range(16):
            pbt = pb[c // 8]; pbn = PB[c // 8]
            P.add("pe", lambda e, xb=xb, c=c, pbt=pbt: e.transpose(out=pbt[:, (c % 8) * 128:(c % 8 + 1) * 128], in_=xb[:, c * 128:(c + 1) * 128], identity=ident_b[:]),
                  reads=[xbn, "ident_b"], writes=[pbn])
        for c in range(16):
            pbt = pb[c // 8]; pbn = PB[c // 8]
            P.add("act", lambda e, c=c, i=i, pbt=pbt: e.activation(out=hT[:, c, i * 128:(i + 1) * 128], in_=pbt[:, (c % 8) * 128:(c % 8 + 1) * 128], func=AF.Identity,
                                                                   bias=modT[:, c:c + 1], scale=s1T[:, c:c + 1]),
                  reads=[pbn, "modT", "s1T"], writes=["hT_%d" % i])
    HT = ["hT_%d" % i for i in range(NT)]
    if stop == "B1":
        d = dbg_out("hT", [128, 16, S], BF16)
        P.dma("sp", d, hT[:], reads=HT, writes=["dbg"])
        return nc, P, st, dbg
    P.barrier()
    P.off = proj_mark

    uT = P.sb([128, 8, S], BF16, "uT"); qT = P.sb([128, 8, S], BF16, "qT"); qiT = P.sb([128, 8, S], BF16, "qiT")
    kT = P.sb([128, 2, S], BF16, "kT"); vtok = P.sb([128, NT, 256], BF16, "vtok"); kiT2 = P.sb([128, S], BF16, "kiT2")
    wi_all = P.sb([128, NT, 16], F32, "wi_all")
    after_proj_mark = P.off
    wst = [P.sb([128, 16, 272], BF16, "wst%d" % k) for k in range(2)]
    kw_st = P.sb([128, 16, 80], F32, "kw_st")
    P.dma("sp", kw_st[:], w_kw.rearrange("(kc p) n -> p kc n", p=128), writes=["kw_st"])
    evac_n = [0]

    def evac(dst, src, reads, writes, eng=None):
        k = evac_n[0]; evac_n[0] += 1
        if eng == "act" or (eng is None and k % 2 == 0):
            P.add("act", lambda e: e.copy(out=dst, in_=src), reads=reads, writes=writes)
        else:
            P.add("dve", lambda e: e.tensor_copy(out=dst, in_=src), reads=reads, writes=writes)

    def w_in_view(c0, n):
        return w_in_view_(w_in, c0, n)

    groups = []
    for g in range(4):
        groups.append((g * 256, "fm", [(uT, 2 * g), (uT, 2 * g + 1)]))
    for g in range(4):
        groups.append((1024 + g * 256, "fm", [(qT, 2 * g), (qT, 2 * g + 1)]))
    groups.append((2048, "fm", [(kT, 0), (kT, 1)]))
    for g in range(4):
        groups.append((2560 + g * 256, "fm", [(qiT, 2 * g), (qiT, 2 * g + 1)]))
    groups.append((3584, "ki", None))
    groups.append((2304, "tm", None))
    gi = 0
    pk = 0
    for c0, kind, dests in groups:
        if stop == "B2a" and gi == 1:
            d = dbg_out("uT01", [128, 2, S], BF16)
            P.dma("sp", d, uT[:, 0:2, :], reads=[uT.name + "_0", uT.name + "_1"], writes=["dbg_uT01"])
            return nc, P, st, dbg
        if stop == "B2c" and kind == "tm":
            d = dbg_out("kiT2", [128, S], BF16)
            P.dma("sp", d, kiT2[:], reads=["kiT2"], writes=["dbg_kiT2"])
            return nc, P, st, dbg
        if stop == "B2b" and kind == "ki":
            d = dbg_out("qiT", [128, 8, S], BF16)
            P.dma("sp", d, qiT[:], reads=[qiT.name + "_%d" % c for c in range(8)], writes=["dbg_qiT"])
            return nc, P, st, dbg
        wt = wst[gi % 2]; wn = "wst%d" % (gi % 2); gi += 1
        if kind == "fm":
            P.dma("pool", wt[:, :, 0:256], w_in_view(c0, 256), writes=[wn])
            for sub, (dt_, ch) in enumerate(dests):
                for tc in range(4):
                    pt = ps[pk % 4]; pn = PS[pk % 4]; pk += 1
                    for kc in range(16):
                        P.add("pe", lambda e, pt=pt, wt=wt, kc=kc, sub=sub, tc=tc: e.matmul(pt[:], lhsT=wt[:, kc, sub * 128:(sub + 1) * 128], rhs=hT[:, kc, tc * 512:(tc + 1) * 512], start=(kc == 0), stop=(kc == 15)),
                              reads=[wn] + HT[tc * 4:tc * 4 + 4], writes=[pn])
                    evac(dt_[:, ch, tc * 512:(tc + 1) * 512], pt[:], [pn], [dt_.name + "_%d" % ch])
        elif kind == "ki":
            P.add("dve", lambda e, wt=wt: e.tensor_copy(out=wt[:, :, 0:64], in_=kw_st[:, :, 0:64]), reads=["kw_st"], writes=[wn])
            P.add("dve", lambda e, wt=wt: e.tensor_copy(out=wt[:, :, 64:128], in_=kw_st[:, :, 0:64]), reads=["kw_st"], writes=[wn])
            for tc in range(4):
                pt = ps[pk % 4]; pn = PS[pk % 4]; pk += 1
                for kc in range(16):
                    P.add("pe", lambda e, pt=pt, wt=wt, kc=kc, tc=tc: e.matmul(pt[:], lhsT=wt[:, kc, 0:128], rhs=hT[:, kc, tc * 512:(tc + 1) * 512], start=(kc == 0), stop=(kc == 15)),
                          reads=[wn] + HT[tc * 4:tc * 4 + 4], writes=[pn])
                evac(kiT2[:, tc * 512:(tc + 1) * 512], pt[:], [pn], ["kiT2"])
        else:
            P.dma("pool", wt[:, :, 0:256], w_in_view(2304, 256), writes=[wn])
            P.add("dve", lambda e, wt=wt: e.tensor_copy(out=wt[:, :, 256:272], in_=kw_st[:, :, 64:80]), reads=["kw_st"], writes=[wn])
            for i in range(NT):
                pt = ps[pk % 4]; pn = PS[pk % 4]; pk += 1
                for kc in range(16):
                    P.add("pe", lambda e, pt=pt, wt=wt, kc=kc, i=i: e.matmul(pt[:, 0:256], lhsT=hT[:, kc, i * 128:(i + 1) * 128], rhs=wt[:, kc, 0:256], start=(kc == 0), stop=(kc == 15)),
                          reads=[wn, HT[i]], writes=[pn])
                P.add("act", lambda e, pt=pt, i=i: e.copy(out=vtok[:, i, :], in_=pt[:, 0:256]), reads=[pn], writes=["vtok"])
                pt2 = ps[4 + i % 2]; pn2 = PS[4 + i % 2]
                for kc in range(16):
                    P.add("pe", lambda e, pt2=pt2, wt=wt, kc=kc, i=i: e.matmul(pt2[:, 0:16], lhsT=hT[:, kc, i * 128:(i + 1) * 128], rhs=wt[:, kc, 256:272], start=(kc == 0), stop=(kc == 15)),
                          reads=[wn, HT[i]], writes=[pn2])
                P.add("dve", lambda e, pt2=pt2, i=i: e.tensor_copy(out=wi_all[:, i, :], in_=pt2[:, 0:16]), reads=[pn2], writes=["wi_all"])
    UT = [uT.name + "_%d" % c for c in range(8)]; QT = [qT.name + "_%d" % c for c in range(8)]
    QIT = [qiT.name + "_%d" % c for c in range(8)]; KT = [kT.name + "_%d" % c for c in range(2)]
    if stop == "B2":
        for nm, t, toks, shp, dt_ in (("uT", uT, UT, [128, 8, S], BF16), ("qT", qT, QT, [128, 8, S], BF16), ("qiT", qiT, QIT, [128, 8, S], BF16),
                                      ("kT", kT, KT, [128, 2, S], BF16), ("vtok", vtok, ["vtok"], [128, NT, 256], BF16),
                                      ("kiT2", kiT2, ["kiT2"], [128, S], BF16), ("wi_all", wi_all, ["wi_all"], [128, NT, 16], F32)):
            d = dbg_out(nm, shp, dt_)
            P.dma("sp", d, t[:], reads=toks, writes=["dbg_" + nm])
        return nc, P, st, dbg
    P.barrier()
    P.off = base_mark
    pool_outT = P.sb([128, 8, S], BF16, "pool_outT"); attn_outT = P.sb([128, 8, S], BF16, "attn_outT")
    POT = ["pool_outT_%d" % c for c in range(8)]; AOT = ["attn_outT_%d" % i for i in range(NT)]
    P.off = after_proj_mark
    A_ = P.sb([128, S], F32, "poolA"); B_ = P.sb([128, S], F32, "poolB"); wp = P.sb([128, 4, 2, 256], BF16, "wp")
    P.dma("pool", wp[:], w_pool.rearrange("g (cc p) d -> p g cc d", p=128), writes=["wp"])
    for c in range(8):
        g = c // 2; w = 2 << g
        u = uT[:, c, :]; un = UT[c]
        P.add("dve", lambda e, u=u: e.tensor_tensor(out=A_[:, 1:S], in0=u[:, 1:S], in1=u[:, 0:S - 1], op=ALU.add), reads=[un], writes=["poolA"])
        P.add("dve", lambda e, u=u: e.tensor_copy(out=A_[:, 0:1], in_=u[:, 0:1]), reads=[un], writes=["poolA"])
        cur, curn, oth, othn = A_, "poolA", B_, "poolB"
        d = 2
        while d < w:
            P.add("dve", lambda e, cur=cur, oth=oth, d=d: e.tensor_tensor(out=oth[:, d:S], in0=cur[:, d:S], in1=cur[:, 0:S - d], op=ALU.add), reads=[curn], writes=[othn])
            P.add("dve", lambda e, cur=cur, oth=oth, d=d: e.tensor_copy(out=oth[:, 0:d], in_=cur[:, 0:d]), reads=[curn], writes=[othn])
            cur, curn, oth, othn = oth, othn, cur, curn
            d *= 2
        P.add("dve", lambda e, cur=cur, g=g: e.tensor_tensor(out=cur[:, 0:16], in0=cur[:, 0:16], in1=corr[:, g, :], op=ALU.mult), reads=[curn, "corr"], writes=[curn])
        P.add("dve", lambda e, cur=cur, u=u, w=w: e.scalar_tensor_tensor(out=u, in0=cur[:], scalar=1.0 / w, in1=u, op0=ALU.mult, op1=ALU.subtract), reads=[curn, un], writes=[un])
        if c % 2 == 1:
            for dch in range(2):
                for tc in range(4):
                    pt = ps[pk % 4]; pn = PS[pk % 4]; pk += 1
                    for cc in range(2):
                        P.add("pe", lambda e, pt=pt, g=g, cc=cc, dch=dch, tc=tc: e.matmul(pt[:], lhsT=wp[:, g, cc, dch * 128:(dch + 1) * 128], rhs=uT[:, 2 * g + cc, tc * 512:(tc + 1) * 512], start=(cc == 0), stop=(cc == 1)),
                              reads=["wp", UT[2 * g], UT[2 * g + 1]], writes=[pn])
                    P.add("act", lambda e, pt=pt, g=g, dch=dch, tc=tc: e.activation(out=pool_outT[:, 2 * g + dch, tc * 512:(tc + 1) * 512], in_=pt[:], func=AF.Identity, scale=pscT[:, 2 * g + dch:2 * g + dch + 1]),
                          reads=[pn, "pscT"], writes=[POT[2 * g + dch]])
    if stop == "B3":
        d = dbg_out("pool_outT", [128, 8, S], BF16)
        P.dma("sp", d, pool_outT[:], reads=POT, writes=["dbg_pool_outT"])
        return nc, P, st, dbg
    P.barrier()

    P.off = proj_mark
    acc = P.sb([128, S], F32, "acc"); maskq = P.sb([128, S], BF16, "maskq"); junkb = P.sb([128, S], BF16, "junkb")
    maskT = P.sb([128, NT, 128], BF16, "maskT"); EB3 = P.sb([128, 8, 384], F32, "EB3")
    assert P.off == proj_mark + 32768
    P.off = after_proj_mark
    rtile = [P.sb([128, 512], F32, "rtile%d" % k) for k in range(2)]
    Et = [P.sb([128, 512], BF16, "Et%d" % k) for k in range(2)]
    P1 = [P.sb([128, 512], BF16, "P1%d" % k) for k in range(2)]
    Pt = [P.sb([128, 512], BF16, "Pt%d" % k) for k in range(2)]
    rinv = P.sb([128, 512], F32, "rinv")
    lo = P.sb([128, 1], F32, "lo"); hi = P.sb([128, 1], F32, "hi"); mid = P.sb([128, 1], F32, "mid"); cnt = P.sb([128, 1], F32, "cnt")
    ge = P.sb([128, 1], U32, "ge"); lt = P.sb([128, 1], U32, "lt")
    relb = P.sb([32, 8], F32, "relb"); boh = P.sb([32, 384], F32, "boh"); fext = P.sb([8, 384], F32, "fext")
    hank = P.sb([128, 256], F32, "hank"); anti = P.sb([128, 128], F32, "anti")
    P.dma("sp", relb[:], rel_bias, writes=["relb"]); P.dma("sp", boh[:], cst["bucket_oh"], writes=["boh"])
    P.dma("sp", anti[:], cst["anti_f"], writes=["anti"])
    P.add("pe", lambda e: e.matmul(ps[0][0:8, 0:384], lhsT=relb[:], rhs=boh[:], start=True, stop=True), reads=["relb", "boh"], writes=[PS[0]])
    P.add("dve", lambda e: e.tensor_copy(out=fext[:], in_=ps[0][0:8, 0:384]), reads=[PS[0]], writes=["fext"])
    P.dma("sp", fextd, fext[:], reads=["fext"], writes=["fextd"])
    for h in range(8):
        P.dma("sp", hank[:], bass.AP(fextd.tensor, h * 384, [[1, 128], [1, 256]]), reads=["fextd"], writes=["hank"])
        P.add("pe", lambda e: e.matmul(ps[1][:, 0:256], lhsT=anti[:], rhs=hank[:], start=True, stop=True), reads=["anti", "hank"], writes=[PS[1]])
        P.add("act", lambda e, h=h: e.activation(out=EB3[:, h, 0:256], in_=ps[1][:, 0:256], func=AF.Exp), reads=[PS[1]], writes=["EB3"])
        P.add("act", lambda e, h=h: e.activation(out=EB3[:, h, 256:384], in_=ps[1][:, 255:256].to_broadcast([128, 128]), func=AF.Exp), reads=[PS[1]], writes=["EB3"])
    NIT = 15
    pow2 = P.sb([128, NIT], F32, "pow2"); wk_all = P.sb([128, NIT], F32, "wk_all"); sgh = P.sb([128, 1], F32, "sgh")
    for it in range(NIT):
        P.add("pool", lambda e, it=it: e.memset(pow2[:, it:it + 1], 0.5 ** (it + 1)), writes=["pow2"])
    ek = 0
    for i in range(NT):
        nk = (i + 1) * 128
        nch = (nk + 511) // 512
        for h in range(16):
            pr, hh = h // 2, h % 2
            b0 = 64 * hh
            for kc in range(nch):
                wd = min(512, nk - kc * 512)
                pt = ps[ek % 2]; pn = PS[ek % 2]; rtl = rtile[ek % 2]; rn = "rtile%d" % (ek % 2); ek += 1
                P.add("pe", lambda e, pt=pt, pr=pr, b0=b0, i=i, kc=kc, wd=wd: e.matmul(pt[:, 0:wd], lhsT=qiT[b0:b0 + 64, pr, i * 128:(i + 1) * 128], rhs=kiT2[b0:b0 + 64, kc * 512:kc * 512 + wd], start=True, stop=True),
                      reads=[QIT[pr], "kiT2"], writes=[pn])
                P.add("act", lambda e, pt=pt, rtl=rtl, wd=wd: e.activation(out=rtl[:, 0:wd], in_=pt[:, 0:wd], func=AF.Relu), reads=[pn], writes=[rn])
                if h == 0:
                    P.add("dve", lambda e, rtl=rtl, kc=kc, wd=wd, i=i, h=h: e.tensor_scalar(out=acc[:, kc * 512:kc * 512 + wd], in0=rtl[:, 0:wd], scalar1=wi_all[:, i, h:h + 1], scalar2=None, op0=ALU.mult),
                          reads=[rn, "wi_all"], writes=["acc_%d" % kc])
                else:
                    P.add("dve", lambda e, rtl=rtl, kc=kc, wd=wd, i=i, h=h: e.scalar_tensor_tensor(out=acc[:, kc * 512:kc * 512 + wd], in0=rtl[:, 0:wd], scalar=wi_all[:, i, h:h + 1], in1=acc[:, kc * 512:kc * 512 + wd], op0=ALU.mult, op1=ALU.add),
                          reads=[rn, "wi_all", "acc_%d" % kc], writes=["acc_%d" % kc])
        P.add("dve", lambda e, i=i: e.tensor_tensor(out=acc[:, i * 128:(i + 1) * 128], in0=acc[:, i * 128:(i + 1) * 128], in1=causal_neg[:], op=ALU.add), reads=["acc_0", "acc_1", "acc_2", "acc_3"] + ["causal_neg"], writes=["acc_0", "acc_1", "acc_2", "acc_3"])
        if i >= 2:
            P.add("dve", lambda e, nk=nk: e.tensor_reduce(out=hi[:], in_=acc[:, 0:nk], axis=AX.X, op=ALU.max), reads=["acc_0", "acc_1", "acc_2", "acc_3"], writes=["hi"])
            P.add("dve", lambda e, i=i: e.tensor_reduce(out=lo[:], in_=acc[:, 0:i * 128], axis=AX.X, op=ALU.min), reads=["acc_0", "acc_1", "acc_2", "acc_3"], writes=["lo"])
            P.add("dve", lambda e: e.tensor_tensor(out=hi[:], in0=hi[:], in1=lo[:], op=ALU.subtract), reads=["hi", "lo"], writes=["hi"])
            P.add("dve", lambda e: e.tensor_scalar(out=wk_all[:], in0=pow2[:], scalar1=hi[:, 0:1], scalar2=None, op0=ALU.mult), reads=["hi", "pow2"], writes=["wk_all"])
            P.add("dve", lambda e: e.scalar_tensor_tensor(out=mid[:], in0=hi[:], scalar=0.5, in1=lo[:], op0=ALU.mult, op1=ALU.add), reads=["lo", "hi"], writes=["mid"])
            for it in range(NIT):
                P.add("dve", lambda e, nk=nk: e.tensor_scalar(out=junkb[:, 0:nk], in0=acc[:, 0:nk], scalar1=mid[:, 0:1], scalar2=None, op0=ALU.is_ge, op1=ALU.add, accum_out=cnt[:, 0:1]),
                      reads=["acc_0", "acc_1", "acc_2", "acc_3"] + ["mid"], writes=["junkb", "cnt"])
                P.add("dve", lambda e: e.tensor_scalar(out=sgh[:], in0=cnt[:], scalar1=255.5, scalar2=0.5, op0=ALU.is_ge, op1=ALU.subtract), reads=["cnt"], writes=["sgh"])
                P.add("dve", lambda e, it=it: e.scalar_tensor_tensor(out=mid[:], in0=sgh[:], scalar=wk_all[:, it:it + 1], in1=mid[:], op0=ALU.mult, op1=ALU.add), reads=["sgh", "wk_all", "mid"], writes=["mid"])
            P.add("dve", lambda e: e.scalar_tensor_tensor(out=lo[:], in0=hi[:], scalar=-(0.5 ** (NIT + 1)), in1=mid[:], op0=ALU.mult, op1=ALU.add), reads=["hi", "mid"], writes=["lo"])
            P.add("dve", lambda e, nk=nk: e.tensor_scalar(out=maskq[:, 0:nk], in0=acc[:, 0:nk], scalar1=lo[:, 0:1], scalar2=None, op0=ALU.is_ge), reads=["acc_0", "acc_1", "acc_2", "acc_3"] + ["lo"], writes=["maskq"])
        else:
            P.add("dve", lambda e, nk=nk: e.tensor_scalar(out=maskq[:, 0:nk], in0=acc[:, 0:nk], scalar1=-1e29, scalar2=None, op0=ALU.is_ge), reads=["acc_0", "acc_1", "acc_2", "acc_3"], writes=["maskq"])
        for j0 in range(0, i + 1, 8):
            jj = list(range(j0, min(i + 1, j0 + 8)))
            pbt = pb[(j0 // 8) % 2]; pbn = PB[(j0 // 8) % 2]
            for j in jj:
                P.add("pe", lambda e, j=j, pbt=pbt: e.transpose(out=pbt[:, (j % 8) * 128:(j % 8 + 1) * 128], in_=maskq[:, j * 128:(j + 1) * 128], identity=ident_b[:]), reads=["maskq", "ident_b"], writes=[pbn])
            for j in jj:
                P.add("act", lambda e, j=j, pbt=pbt: e.copy(out=maskT[:, j, :], in_=pbt[:, (j % 8) * 128:(j % 8 + 1) * 128]), reads=[pbn], writes=["maskT"])
        steps = [(kv, j) for kv in range(2) for j in range(i + 1)]
        bufs = []
        for n_, (kv, j) in enumerate(steps):
            bufs.append(ek % 2); ek += 1

        def emit_S(n_):
            kv, j = steps[n_]; k2 = bufs[n_]
            pS = ps[2 + k2]
            P.add("pe", lambda e, pS=pS, kv=kv, j=j, i=i: e.matmul(pS[:], lhsT=kT[:, kv, j * 128:(j + 1) * 128], rhs=qT[:, 4 * kv:4 * kv + 4, i * 128:(i + 1) * 128], start=True, stop=True),
                  reads=[KT[kv]] + QT[4 * kv:4 * kv + 4], writes=[PS[2 + k2]])
        emit_S(0)
        for n_, (kv, j) in enumerate(steps):
            k2 = bufs[n_]
            off = 0 if j == i else (128 if j == i - 1 else 256)
            pS = ps[2 + k2]; pSn = PS[2 + k2]
            et = Et[k2]; etn = "Et%d" % k2; p1 = P1[k2]; p1n = "P1%d" % k2; ptt = Pt[k2]; ptn = "Pt%d" % k2
            if n_ + 1 < len(steps):
                emit_S(n_ + 1)
            P.add("act", lambda e, pS=pS, et=et: e.activation(out=et[:], in_=pS[:], func=AF.Exp, scale=SM_SCALE), reads=[pSn], writes=[etn])
            P.add("pool", lambda e, et=et, p1=p1, j=j: e.tensor_tensor(out=p1[:].rearrange("p (h q) -> p h q", h=4), in0=et[:].rearrange("p (h q) -> p h q", h=4), in1=maskT[:, j:j + 1, :].to_broadcast([128, 4, 128]), op=ALU.mult),
                  reads=[etn, "maskT"], writes=[p1n])
            P.add("dve", lambda e, p1=p1, ptt=ptt, kv=kv, off=off: e.tensor_tensor(out=ptt[:].rearrange("p (h q) -> p h q", h=4), in0=p1[:].rearrange("p (h q) -> p h q", h=4), in1=EB3[:, 4 * kv:4 * kv + 4, off:off + 128], op=ALU.mult),
                  reads=[p1n, "EB3"], writes=[ptn])
            P.add("pe", lambda e, ptt=ptt, kv=kv, j=j, i=i: e.matmul(ps[4][:], lhsT=vtok[:, j, kv * 128:(kv + 1) * 128], rhs=ptt[:], start=(j == 0), stop=(j == i)), reads=["vtok", ptn], writes=[PS[4]])
            P.add("pe", lambda e, ptt=ptt, j=j, i=i: e.matmul(ps[5][:], lhsT=ones_b[:], rhs=ptt[:], start=(j == 0), stop=(j == i)), reads=["ones_b", ptn], writes=[PS[5]])
            if j == i:
                P.add("dve", lambda e: e.reciprocal(out=rinv[:], in_=ps[5][:]), reads=[PS[5]], writes=["rinv"])
                P.add("dve", lambda e, kv=kv, i=i: e.tensor_tensor(out=attn_outT[:, 4 * kv:4 * kv + 4, i * 128:(i + 1) * 128], in0=ps[4][:].rearrange("p (h q) -> p h q", h=4), in1=rinv[:].rearrange("p (h q) -> p h q", h=4), op=ALU.mult),
                      reads=[PS[4], "rinv"], writes=[AOT[i]])
        if stop == "B4a" and i == 3:
            d = dbg_out("acc", [128, 512]); P.dma("sp", d, acc[:, 0:512], reads=["acc_0", "acc_1", "acc_2", "acc_3"], writes=["dbg_acc"])
            d = dbg_out("maskq", [128, 512], BF16); P.dma("sp", d, maskq[:, 0:512], reads=["maskq"], writes=["dbg_maskq"])
            d = dbg_out("thr", [128, 1]); P.dma("sp", d, lo[:], reads=["lo"], writes=["dbg_thr"])
            d = dbg_out("EB3", [128, 8, 384]); P.dma("sp", d, EB3[:], reads=["EB3"], writes=["dbg_EB3"])
            d = dbg_out("attn_outT", [128, 8, 512], BF16); P.dma("sp", d, attn_outT[:, :, 0:512], reads=AOT[0:4], writes=["dbg_attn_outT"])
            return nc, P, st, dbg
    if stop == "B4":
        d = dbg_out("attn_outT", [128, 8, S], BF16)
        P.dma("sp", d, attn_outT[:], reads=AOT, writes=["dbg_attn_outT"])
        return nc, P, st, dbg
    P.barrier()
    P.off = proj_mark
    O1 = P.sb([128, NT, 32], BF16, "O1"); O2 = P.sb([128, NT, 32], BF16, "O2"); Osum = P.sb([128, NT, 32], BF16, "Osum")
    g1 = P.sb([128, NT], F32, "g1"); g2 = P.sb([128, NT], F32, "g2")
    dest_i = P.sb([128, NT, 2], U32, "dest_i")
    blk_i = P.sb([128, NBLK], I32, "blk_i"); act_i = P.sb([128, NBLK], I32, "act_i"); widx = P.sb([128, NBLK], U32, "widx")
    route_mark = P.off
    wo = P.sb([128, 16, D], BF16, "wo")
    xt2 = [P.sb([128, D], F32, "xt2_%d" % k) for k in range(2)]
    gt1_bc = P.sb([128, D], F32, "gt1_bc"); s2_bc = P.sb([128, D], F32, "s2_bc"); sh2_bc = P.sb([128, D], F32, "sh2_bc")
    h2 = P.sb([128, D], F32, "h2"); h2b = P.sb([128, D], BF16, "h2b"); h2T = P.sb([128, 16, 128], F32, "h2T")
    wr_sb = P.sb([128, 16, 64], F32, "wr_sb"); br_bc = P.sb([128, 36], F32, "br_bc")
    ss2 = P.sb([128, NT], F32, "ss2"); rt2 = P.sb([128, NT], F32, "rt2"); rstd2 = P.sb([128, NT], F32, "rstd2")
    L_all = P.sb([128, NT, 36], F32, "L_all")
    for n in range(4):
        P.dma("pool", wo[:, :, n * 512:(n + 1) * 512], w_out[:, n * 512:(n + 1) * 512].rearrange("(kc p) n -> p kc n", p=128), writes=["wo"])
    P.dma("sp", gt1_bc[:], modd[0:1, 2 * D:3 * D].to_broadcast([128, D]), reads=["modd"], writes=["gt1_bc"])
    P.dma("sp", sh2_bc[:], modd[0:1, 3 * D:4 * D].to_broadcast([128, D]), reads=["modd"], writes=["sh2_bc"])
    P.dma("sp", s2_bc[:], modd[0:1, 4 * D:5 * D].to_broadcast([128, D]), reads=["modd"], writes=["s2_bc"])
    P.dma("sp", wr_sb[:], wr, writes=["wr_sb"])
    P.dma("sp", br_bc[:], br, writes=["br_bc"])
    P.add("pool", lambda e: e.memset(ss2[:], 0.0), writes=["ss2"])

    for i in range(NT):
        xt = xt2[i % 2]; xtn = "xt2_%d" % (i % 2)
        P.dma("sp", xt[:], x[i * 128:(i + 1) * 128, :], writes=[xtn])
        for n in range(4):
            for kc in range(16):
                src = pool_outT if kc < 8 else attn_outT
                srcn = POT[kc] if kc < 8 else AOT[i]
                P.add("pe", lambda e, n=n, kc=kc, src=src, i=i: e.matmul(ps[n][:], lhsT=src[:, kc % 8, i * 128:(i + 1) * 128], rhs=wo[:, kc, n * 512:(n + 1) * 512], start=(kc == 0), stop=(kc == 15)),
                      reads=[srcn, "wo"], writes=[PS[n]])
            P.add("dve", lambda e, n=n: e.tensor_tensor(out=h2[:, n * 512:(n + 1) * 512], in0=ps[n][:], in1=gt1_bc[:, n * 512:(n + 1) * 512], op=ALU.mult), reads=[PS[n], "gt1_bc"], writes=["h2"])
            P.add("pool", lambda e, n=n, xt=xt: e.tensor_tensor(out=xt[:, n * 512:(n + 1) * 512], in0=xt[:, n * 512:(n + 1) * 512], in1=h2[:, n * 512:(n + 1) * 512], op=ALU.add), reads=[xtn, "h2"], writes=[xtn])
        P.dma("sp", x1d[i * 128:(i + 1) * 128, :], xt[:], reads=[xtn], writes=["x1d_%d" % i])
        P.add("act", lambda e, xt=xt, i=i: e.activation(out=h2b[:], in_=xt[:], func=AF.Square, accum_out=ss2[:, i:i + 1]), reads=[xtn, "ss2"], writes=["h2b", "ss2"])
        P.add("act", lambda e, i=i: e.activation(out=rt2[:, i:i + 1], in_=ss2[:, i:i + 1], func=AF.Sqrt, bias=eps_t[:, 0:1], scale=1.0 / D), reads=["ss2", "eps"], writes=["rt2"])
        P.add("dve", lambda e, i=i: e.reciprocal(out=rstd2[:, i:i + 1], in_=rt2[:, i:i + 1]), reads=["rt2"], writes=["rstd2"])
        P.add("dve", lambda e, xt=xt, i=i: e.scalar_tensor_tensor(out=h2[:], in0=xt[:], scalar=rstd2[:, i:i + 1], in1=s2_bc[:], op0=ALU.mult, op1=ALU.mult), reads=[xtn, "rstd2", "s2_bc", "h2"], writes=["h2"])
        P.add("pool", lambda e: e.tensor_tensor(out=h2[:], in0=h2[:], in1=sh2_bc[:], op=ALU.add), reads=["h2", "sh2_bc"], writes=["h2"])
        P.add("act", lambda e: e.copy(out=h2b[:], in_=h2[:]), reads=["h2"], writes=["h2b"])
        P.dma("sp", h2d[i * 128:(i + 1) * 128, :], h2b[:], reads=["h2b"], writes=["h2d_%d" % i])
        for c0 in range(0, 16, 4):
            for c in range(c0, c0 + 4):
                P.add("pe", lambda e, c=c: e.transpose(out=ps[4][:, (c % 4) * 128:(c % 4 + 1) * 128], in_=h2[:, c * 128:(c + 1) * 128], identity=ident_f[:]), reads=["h2", "ident_f"], writes=[PS[4]])
            for c in range(c0, c0 + 4):
                evac(h2T[:, c, :], ps[4][:, (c % 4) * 128:(c % 4 + 1) * 128], [PS[4]], ["h2T"], eng=("act" if (c0 // 4) % 2 == 0 else "dve"))
        for c in range(16):
            P.add("pe", lambda e, c=c: e.matmul(ps[5][:, 0:64], lhsT=h2T[:, c, :], rhs=wr_sb[:, c, :], start=(c == 0), stop=(c == 15)), reads=["h2T", "wr_sb"], writes=[PS[5]])
        P.add("dve", lambda e, i=i: e.tensor_tensor(out=L_all[:, i, :], in0=ps[5][:, 0:36], in1=br_bc[:], op=ALU.add), reads=[PS[5], "br_bc"], writes=["L_all"])
    def bc(t, k):
        return t[:].unsqueeze(2).to_broadcast([128, NT, k])
    R = {n: P.sb([128, NT], F32, "R_" + n) for n in ("gmax", "gsum", "pg", "m1", "m2", "dd", "e2", "den", "rden")}
    R3 = {n: P.sb([128, NT, k], F32, "R3_" + n) for n, k in (("og", 4), ("eg", 4), ("es", 8), ("tmp", 8), ("o1", 8), ("es2", 8), ("o2", 8))}
    Lg = L_all[:, :, 0:4]
    def rop(eng, fn, reads, writes):
        P.add(eng, fn, reads=reads, writes=writes)
    rop("dve", lambda e: e.tensor_reduce(out=R["gmax"][:], in_=Lg, axis=AX.X, op=ALU.max), ["L_all"], ["gmax"])
    rop("dve", lambda e: e.tensor_tensor(out=R3["og"][:], in0=Lg, in1=bc(R["gmax"], 4), op=ALU.is_ge), ["L_all", "gmax"], ["og"])
    rop("dve", lambda e: e.tensor_tensor(out=R3["eg"][:], in0=Lg, in1=bc(R["gmax"], 4), op=ALU.subtract), ["L_all", "gmax"], ["eg"])
    rop("act", lambda e: e.activation(out=R3["eg"][:], in_=R3["eg"][:], func=AF.Exp), ["eg"], ["eg"])
    rop("dve", lambda e: e.tensor_reduce(out=R["gsum"][:], in_=R3["eg"][:], axis=AX.X, op=ALU.add), ["eg"], ["gsum"])
    rop("dve", lambda e: e.reciprocal(out=R["pg"][:], in_=R["gsum"][:]), ["gsum"], ["pg"])
    rop("dve", lambda e: e.tensor_tensor(out=R3["es"][:], in0=L_all[:, :, 4:12], in1=R3["og"][:, :, 0:1].to_broadcast([128, NT, 8]), op=ALU.mult), ["L_all", "og"], ["es"])
    for g in range(1, 4):
        rop("dve", lambda e, g=g: e.tensor_tensor(out=R3["tmp"][:], in0=L_all[:, :, 4 + 8 * g:12 + 8 * g], in1=R3["og"][:, :, g:g + 1].to_broadcast([128, NT, 8]), op=ALU.mult), ["L_all", "og"], ["tmp"])
        rop("dve", lambda e: e.tensor_tensor(out=R3["es"][:], in0=R3["es"][:], in1=R3["tmp"][:], op=ALU.add), ["es", "tmp"], ["es"])
    rop("dve", lambda e: e.tensor_reduce(out=R["m1"][:], in_=R3["es"][:], axis=AX.X, op=ALU.max), ["es"], ["m1"])
    rop("dve", lambda e: e.tensor_tensor(out=R3["o1"][:], in0=R3["es"][:], in1=bc(R["m1"], 8), op=ALU.is_ge), ["es", "m1"], ["o1"])
    rop("dve", lambda e: e.scalar_tensor_tensor(out=R3["es2"][:], in0=R3["o1"][:], scalar=-1e30, in1=R3["es"][:], op0=ALU.mult, op1=ALU.add), ["o1", "es"], ["es2"])
    rop("dve", lambda e: e.tensor_reduce(out=R["m2"][:], in_=R3["es2"][:], axis=AX.X, op=ALU.max), ["es2"], ["m2"])
    rop("dve", lambda e: e.tensor_tensor(out=R3["o2"][:], in0=R3["es2"][:], in1=bc(R["m2"], 8), op=ALU.is_ge), ["es2", "m2"], ["o2"])
    rop("dve", lambda e: e.tensor_tensor(out=R["dd"][:], in0=R["m2"][:], in1=R["m1"][:], op=ALU.subtract), ["m1", "m2"], ["dd"])
    rop("act", lambda e: e.activation(out=R["e2"][:], in_=R["dd"][:], func=AF.Exp), ["dd"], ["e2"])
    rop("dve", lambda e: e.tensor_scalar(out=R["den"][:], in0=R["e2"][:], scalar1=1.0, scalar2=None, op0=ALU.add), ["e2"], ["den"])
    rop("dve", lambda e: e.reciprocal(out=R["rden"][:], in_=R["den"][:]), ["den"], ["rden"])
    rop("dve", lambda e: e.tensor_tensor(out=g1[:], in0=R["pg"][:], in1=R["rden"][:], op=ALU.mult), ["pg", "rden"], ["g1"])
    rop("dve", lambda e: e.tensor_tensor(out=g2[:], in0=g1[:], in1=R["e2"][:], op=ALU.mult), ["g1", "e2"], ["g2"])
    for g in range(4):
        rop("dve", lambda e, g=g: e.tensor_tensor(out=O1[:, :, 8 * g:8 * g + 8], in0=R3["o1"][:], in1=R3["og"][:, :, g:g + 1].to_broadcast([128, NT, 8]), op=ALU.mult), ["o1", "og"], ["O1"])
        rop("dve", lambda e, g=g: e.tensor_tensor(out=O2[:, :, 8 * g:8 * g + 8], in0=R3["o2"][:], in1=R3["og"][:, :, g:g + 1].to_broadcast([128, NT, 8]), op=ALU.mult), ["o2", "og"], ["O2"])
    rop("dve", lambda e: e.tensor_tensor(out=Osum[:], in0=O1[:], in1=O2[:], op=ALU.add), ["O1", "O2"], ["Osum"])
    X1D = ["x1d_%d" % i for i in range(NT)]; H2D = ["h2d_%d" % i for i in range(NT)]
    if stop == "C":
        for nm, t, shp, dt_ in (("O1", O1, [128, NT, 32], BF16), ("O2", O2, [128, NT, 32], BF16), ("g1", g1, [128, NT], F32), ("g2", g2, [128, NT], F32)):
            d = dbg_out(nm, shp, dt_); P.dma("sp", d, t[:], reads=[nm], writes=["dbg_" + nm])
        d = dbg_out("x1", [S, D]); P.dma("sp", d, x1d, reads=X1D, writes=["dbg_x1"])
        d = dbg_out("h2", [S, D], BF16); P.dma("sp", d, h2d, reads=H2D, writes=["dbg_h2"])
        return nc, P, st, dbg
    P.barrier()
    P.off = route_mark
    us_sb = P.sb([32, 32], F32, "us_sb"); ui_sb = P.sb([32, 32], F32, "ui_sb"); thr16 = P.sb([32, 16], F32, "thr16")
    cmp16 = P.sb([32, 16], F32, "cmp16"); nblkT = P.sb([32, 1], F32, "nblkT"); nb_bc = P.sb([32, 128], F32, "nb_bc")
    pstart_s = P.sb([128, 32], F32, "pstart_s"); pend_sb = P.sb([128, 32], F32, "pend_sb")
    bgrid = P.sb([128, NBLK, 32], F32, "bgrid"); cmpb = P.sb([128, NBLK, 32], F32, "cmpb")
    blkf = P.sb([128, NBLK], F32, "blkf"); actf = P.sb([128, NBLK], F32, "actf")
    pidx = P.sb([128, 1], I32, "pidx"); pidxf = P.sb([128, 1], F32, "pidxf"); widxf = P.sb([128, NBLK], F32, "widxf")
    destf = P.sb([128, 32], F32, "destf"); dtmp = P.sb([128, 32], F32, "dtmp"); d12 = P.sb([128, 2], F32, "d12")
    h2bt = [P.sb([128, D], BF16, "h2bt%d" % k) for k in range(2)]
    P.dma("sp", us_sb[:], cst["u_strict"], writes=["us_sb"]); P.dma("sp", ui_sb[:], cst["u_incl"], writes=["ui_sb"])
    P.dma("sp", thr16[:], cst["thr16"], writes=["thr16"]); P.dma("sp", bgrid[:], cst["bgrid"], writes=["bgrid"])
    for i in range(NT):
        P.add("pe", lambda e, i=i: e.matmul(ps[0][0:32, 0:128], lhsT=Osum[:, i, :], rhs=ones_b[:], start=(i == 0), stop=(i == NT - 1)), reads=["Osum", "ones_b"], writes=[PS[0]])
    P.add("dve", lambda e: e.tensor_tensor(out=cmp16[:], in0=ps[0][0:32, 0:1].to_broadcast([32, 16]), in1=thr16[:], op=ALU.is_gt), reads=[PS[0], "thr16"], writes=["cmp16"])
    P.add("dve", lambda e: e.tensor_reduce(out=nblkT[:], in_=cmp16[:], axis=AX.X, op=ALU.add), reads=["cmp16"], writes=["nblkT"])
    P.add("dve", lambda e: e.tensor_scalar(out=nb_bc[:], in0=ones_f[0:32, :], scalar1=nblkT[:, 0:1], scalar2=None, op0=ALU.mult), reads=["nblkT", "ones_f"], writes=["nb_bc"])
    P.add("pe", lambda e: e.matmul(ps[1][:, 0:32], lhsT=nb_bc[:], rhs=us_sb[:], start=True, stop=True), reads=["nb_bc", "us_sb"], writes=[PS[1]])
    P.add("pe", lambda e: e.matmul(ps[1][:, 32:64], lhsT=nb_bc[:], rhs=ui_sb[:], start=True, stop=True), reads=["nb_bc", "ui_sb"], writes=[PS[1]])
    P.add("dve", lambda e: e.tensor_scalar(out=pstart_s[:], in0=ps[1][:, 0:32], scalar1=128.0, scalar2=None, op0=ALU.mult), reads=[PS[1]], writes=["pstart_s"])
    P.add("dve", lambda e: e.tensor_copy(out=pend_sb[:], in_=ps[1][:, 32:64]), reads=[PS[1]], writes=["pend_sb"])
    P.add("dve", lambda e: e.tensor_tensor(out=cmpb[:], in0=pend_sb[:].unsqueeze(1).to_broadcast([128, NBLK, 32]), in1=bgrid[:], op=ALU.is_le), reads=["pend_sb", "bgrid"], writes=["cmpb"])
    P.add("dve", lambda e: e.tensor_reduce(out=blkf[:], in_=cmpb[:], axis=AX.X, op=ALU.add), reads=["cmpb"], writes=["blkf"])
    P.add("dve", lambda e: e.tensor_scalar(out=blkf[:], in0=blkf[:], scalar1=31.0, scalar2=None, op0=ALU.min), reads=["blkf"], writes=["blkf"])
    P.add("dve", lambda e: e.tensor_copy(out=blk_i[:], in_=blkf[:]), reads=["blkf"], writes=["blk_i"])
    P.add("dve", lambda e: e.tensor_scalar(out=actf[:], in0=bgrid[:, :, 0], scalar1=pend_sb[:, 31:32], scalar2=None, op0=ALU.is_lt), reads=["bgrid", "pend_sb"], writes=["actf"])
    P.add("dve", lambda e: e.tensor_copy(out=act_i[:], in_=actf[:]), reads=["actf"], writes=["act_i"])
    P.add("pool", lambda e: e.iota(pidx[:], pattern=[[0, 1]], base=0, channel_multiplier=1), writes=["pidx"])
    P.add("dve", lambda e: e.tensor_copy(out=pidxf[:], in_=pidx[:]), reads=["pidx"], writes=["pidxf"])
    P.add("dve", lambda e: e.tensor_scalar(out=widxf[:], in0=blkf[:], scalar1=128.0, scalar2=pidxf[:, 0:1], op0=ALU.mult, op1=ALU.add), reads=["blkf", "pidxf"], writes=["widxf"])
    P.add("dve", lambda e: e.tensor_scalar(out=actf[:], in0=actf[:], scalar1=-1.0, scalar2=-8192.0, op0=ALU.add, op1=ALU.mult), reads=["actf"], writes=["actf"])
    P.add("dve", lambda e: e.tensor_tensor(out=widxf[:], in0=widxf[:], in1=actf[:], op=ALU.add), reads=["widxf", "actf"], writes=["widxf"])
    P.add("dve", lambda e: e.tensor_copy(out=widx[:], in_=widxf[:]), reads=["widxf"], writes=["widx"])
    for i in range(NT):
        pt = ps[2 + i % 2]; pn = PS[2 + i % 2]
        for j in range(i + 1):
            P.add("pe", lambda e, pt=pt, i=i, j=j: e.matmul(pt[:, 0:32], lhsT=(ones_b if j < i else ltri)[:], rhs=Osum[:, j, :], start=(j == 0), stop=(j == i)), reads=["Osum", "ones_b", "ltri"], writes=[pn])
        P.add("dve", lambda e, pt=pt: e.tensor_tensor(out=destf[:], in0=pt[:, 0:32], in1=pstart_s[:], op=ALU.add), reads=[pn, "pstart_s"], writes=["destf"])
        for a, Oa, on in ((0, O1, "O1"), (1, O2, "O2")):
            P.add("dve", lambda e, Oa=Oa, i=i: e.tensor_tensor(out=dtmp[:], in0=destf[:], in1=Oa[:, i, :], op=ALU.mult), reads=["destf", on], writes=["dtmp"])
            P.add("dve", lambda e, a=a: e.tensor_reduce(out=d12[:, a:a + 1], in_=dtmp[:], axis=AX.X, op=ALU.add), reads=["dtmp"], writes=["d12"])
        P.add("dve", lambda e, i=i: e.tensor_copy(out=dest_i[:, i, :], in_=d12[:]), reads=["d12"], writes=["dest_i_%d" % i])
        hb = h2bt[i % 2]; hbn = "h2bt%d" % (i % 2)
        P.dma("sp", hb[:], h2d[i * 128:(i + 1) * 128, :], reads=[H2D[i]], writes=[hbn])
        for a in range(2):
            P.add("pool", lambda e, hb=hb, i=i, a=a: e.indirect_dma_start(out=Xs, out_offset=bass.IndirectOffsetOnAxis(ap=dest_i[:, i, a:a + 1], axis=0), in_=hb[:], in_offset=None),
                  reads=[hbn, "dest_i_%d" % i] + ["Xs_zero_%d" % bb for bb in range(NBLK)], writes=["Xs_s_%d_%d" % (i, a)], dma=True)
    DEST = ["dest_i_%d" % i for i in range(NT)]
    XS = ["Xs_s_%d_%d" % (i, a) for i in range(NT) for a in range(2)]
    if stop == "D":
        d = dbg_out("dest", [128, NT, 2], U32); P.dma("sp", d, dest_i[:], reads=DEST, writes=["dbg_dest"])
        d = dbg_out("blk", [128, NBLK], I32); P.dma("sp", d, blk_i[:], reads=["blk_i"], writes=["dbg_blk"])
        d = dbg_out("act", [128, NBLK], I32); P.dma("sp", d, act_i[:], reads=["act_i"], writes=["dbg_act"])
        d = dbg_out("Xs", [NSLOT, D], BF16); P.dma("sp", d, Xs, reads=XS, writes=["dbg_Xs"])
        return nc, P, st, dbg
    P.barrier()

    P.off = route_mark
    wg = [P.sb([128, 16, 512], BF16, "wg%d" % k) for k in range(2)]
    wu = [P.sb([128, 16, 512], BF16, "wu%d" % k) for k in range(2)]
    wd = [P.sb([128, 4, D], BF16, "wd%d" % k) for k in range(2)]
    xe = [P.sb([128, D], BF16, "xe%d" % k) for k in range(2)]
    xeT = [P.sb([128, 16, 128], BF16, "xeT%d" % k) for k in range(2)]
    sg = P.sb([128, 512], F32, "sg"); a_bf = P.sb([128, 512], BF16, "a_bf"); aT = P.sb([128, 4, 128], BF16, "aT")
    yb = [P.sb([128, D], F32, "yb%d" % k) for k in range(2)]
    ET = mybir.EngineType
    bcreg = {}

    def mk_bc(e):
        bcreg["r"] = e.alloc_register("wbound")
        return e.reg_mov(bcreg["r"], 32 * 128 - 1)
    P.add("pool", mk_bc)
    for b in range(NBLK):
        k = b % 2
        for wt_, wsrc, wn_, is_down in ((wg[k], w_gate, "wg%d" % k, False), (wu[k], w_up, "wu%d" % k, False), (wd[k], w_down, "wd%d" % k, True)):
            for j in range(4):
                dst = wt_[:, j, :] if is_down else wt_[:, 4 * j:4 * j + 4, :].rearrange("p c n -> p (c n)")
                P.add("pool", lambda e, dst=dst, src=wsrc[j], b=b: e.indirect_dma_start(out=dst, out_offset=None, in_=src, in_offset=bass.IndirectOffsetOnAxis(ap=widx[:, b:b + 1], axis=0),
                                                                                     bounds_check=bcreg["r"], oob_is_err=False),
                      reads=["widx"], writes=[wn_ + "_%d" % j], dma=True)
        P.dma("sp", xe[k][:], Xs[b * 128:(b + 1) * 128, :], reads=XS, writes=["xe%d" % k])
        for c in range(16):
            sl = c % 8; pbt = pb[c // 8]
            P.add("pe", lambda e, c=c, sl=sl, pbt=pbt, k=k: e.transpose(out=pbt[:, sl * 128:(sl + 1) * 128], in_=xe[k][:, c * 128:(c + 1) * 128], identity=ident_b[:]), reads=["xe%d" % k, "ident_b"], writes=[PB[c // 8]])
        for c in range(16):
            sl = c % 8; pbt = pb[c // 8]
            evac(xeT[k][:, c, :], pbt[:, sl * 128:(sl + 1) * 128], [PB[c // 8]], ["xeT%d" % k], eng=("act" if c < 8 else "dve"))
        for c in range(16):
            P.add("pe", lambda e, c=c, k=k: e.matmul(ps[0][:], lhsT=xeT[k][:, c, :], rhs=wg[k][:, c, :], start=(c == 0), stop=(c == 15)), reads=["xeT%d" % k, "wg%d_%d" % (k, c // 4)], writes=[PS[0]])
        for c in range(16):
            P.add("pe", lambda e, c=c, k=k: e.matmul(ps[1][:], lhsT=xeT[k][:, c, :], rhs=wu[k][:, c, :], start=(c == 0), stop=(c == 15)), reads=["xeT%d" % k, "wu%d_%d" % (k, c // 4)], writes=[PS[1]])
        P.add("act", lambda e: e.activation(out=sg[:], in_=ps[0][:], func=AF.Silu), reads=[PS[0]], writes=["sg"])
        P.add("dve", lambda e: e.tensor_tensor(out=a_bf[:], in0=sg[:], in1=ps[1][:], op=ALU.mult), reads=["sg", PS[1]], writes=["a_bf"])
        for f_ in range(4):
            P.add("pe", lambda e, f_=f_: e.transpose(out=pb[0][:, f_ * 128:(f_ + 1) * 128], in_=a_bf[:, f_ * 128:(f_ + 1) * 128], identity=ident_b[:]), reads=["a_bf", "ident_b"], writes=[PB[0]])
        for f_ in range(4):
            evac(aT[:, f_, :], pb[0][:, f_ * 128:(f_ + 1) * 128], [PB[0]], ["aT"], eng="dve")
        for n in range(4):
            for f_ in range(4):
                P.add("pe", lambda e, n=n, f_=f_, k=k: e.matmul(ps[2 + n][:], lhsT=aT[:, f_, :], rhs=wd[k][:, f_, n * 512:(n + 1) * 512], start=(f_ == 0), stop=(f_ == 3)), reads=["aT", "wd%d_%d" % (k, f_)], writes=[PS[2 + n]])
            evac(yb[k][:, n * 512:(n + 1) * 512], ps[2 + n][:], [PS[2 + n]], ["yb%d" % k])
        P.dma("sp", Ys[b * 128:(b + 1) * 128, :], yb[k][:], reads=["yb%d" % k], writes=["Ys"])
    if stop == "E":
        for bi, b in enumerate((0, 10, 30)):
            P.dma("sp", yb[bi % 2][:], Ys[b * 128:(b + 1) * 128, :], reads=["Ys", "yb%d" % (bi % 2)], writes=["yb%d" % (bi % 2)])
            d = dbg_out("Ys%d" % b, [128, D]); P.dma("sp", d, yb[bi % 2][:], reads=["yb%d" % (bi % 2)], writes=["dbg_Ys%d" % b])
        return nc, P, st, dbg
    P.barrier()

    P.off = route_mark
    x1t = [P.sb([128, D], F32, "x1t%d" % k) for k in range(2)]
    y1 = [P.sb([128, D], F32, "y1_%d" % k) for k in range(2)]
    y2 = [P.sb([128, D], F32, "y2_%d" % k) for k in range(2)]
    ob = [P.sb([128, D], F32, "ob%d" % k) for k in range(2)]
    gt2_bc = P.sb([128, D], F32, "gt2_bc"); gfin_bc = P.sb([128, D], F32, "gfin_bc")
    junk3 = P.sb([128, D], BF16, "junk3")
    ss3 = P.sb([128, NT], F32, "ss3"); rt3 = P.sb([128, NT], F32, "rt3"); rstd3 = P.sb([128, NT], F32, "rstd3")
    P.dma("sp", gt2_bc[:], modd[0:1, 5 * D:6 * D].to_broadcast([128, D]), reads=["modd"], writes=["gt2_bc"])
    P.dma("sp", gfin_bc[:], g_final[0:1, :].to_broadcast([128, D]), writes=["gfin_bc"])
    P.add("pool", lambda e: e.memset(ss3[:], 0.0), writes=["ss3"])
    for i in range(NT):
        k = i % 2
        P.dma("sp", x1t[k][:], x1d[i * 128:(i + 1) * 128, :], reads=[X1D[i]], writes=["x1t%d" % k])
        for a, yt, yn in ((0, y1[k], "y1_%d" % k), (1, y2[k], "y2_%d" % k)):
            P.add("pool", lambda e, yt=yt, i=i, a=a: e.indirect_dma_start(out=yt[:], out_offset=None, in_=Ys, in_offset=bass.IndirectOffsetOnAxis(ap=dest_i[:, i, a:a + 1], axis=0)),
                  reads=["Ys", DEST[i]], writes=[yn], dma=True)
        P.add("dve", lambda e, k=k, i=i: e.tensor_scalar(out=y1[k][:], in0=y1[k][:], scalar1=g1[:, i:i + 1], scalar2=None, op0=ALU.mult), reads=["y1_%d" % k, "g1"], writes=["y1_%d" % k])
        P.add("dve", lambda e, k=k, i=i: e.scalar_tensor_tensor(out=y1[k][:], in0=y2[k][:], scalar=g2[:, i:i + 1], in1=y1[k][:], op0=ALU.mult, op1=ALU.add), reads=["y1_%d" % k, "y2_%d" % k, "g2"], writes=["y1_%d" % k])
        P.add("pool", lambda e, k=k: e.tensor_tensor(out=y1[k][:], in0=y1[k][:], in1=gt2_bc[:], op=ALU.mult), reads=["y1_%d" % k, "gt2_bc"], writes=["y1_%d" % k])
        P.add("pool", lambda e, k=k: e.tensor_tensor(out=x1t[k][:], in0=x1t[k][:], in1=y1[k][:], op=ALU.add), reads=["y1_%d" % k, "x1t%d" % k], writes=["x1t%d" % k])
        P.add("act", lambda e, k=k, i=i: e.activation(out=junk3[:], in_=x1t[k][:], func=AF.Square, accum_out=ss3[:, i:i + 1]), reads=["x1t%d" % k, "ss3"], writes=["junk3", "ss3"])
        P.add("act", lambda e, i=i: e.activation(out=rt3[:, i:i + 1], in_=ss3[:, i:i + 1], func=AF.Sqrt, bias=eps_t[:, 0:1], scale=1.0 / D), reads=["ss3", "eps"], writes=["rt3"])
        P.add("dve", lambda e, i=i: e.reciprocal(out=rstd3[:, i:i + 1], in_=rt3[:, i:i + 1]), reads=["rt3"], writes=["rstd3"])
        P.add("dve", lambda e, k=k, i=i: e.scalar_tensor_tensor(out=ob[k][:], in0=x1t[k][:], scalar=rstd3[:, i:i + 1], in1=gfin_bc[:], op0=ALU.mult, op1=ALU.mult), reads=["x1t%d" % k, "rstd3", "gfin_bc"], writes=["ob%d" % k])
        P.dma("sp", out[i * 128:(i + 1) * 128, :], ob[k][:], reads=["ob%d" % k], writes=["out_%d" % i])
    dbg["__final__"] = ["out_%d" % i for i in range(NT)]
    return nc, P, st, dbg


def finish(nc, P, st, out_tokens):
    P.add("sp", None, reads=list(out_tokens))
    P.emit()
    st.close()


def make_shared(inputs, consts):
    f = np.ascontiguousarray
    m = {
        "w_ada": f(inputs["w_ada"][0]),
        "b_ada": f(inputs["b_ada"][0].reshape(1, -1)),
        "g_mixT": f(inputs["g_mix"][0].reshape(16, 128).T),
        "w_in": f(inputs["w_in"][0]),
        "w_kw": f(inputs["w_in"][0][:, 3584:3664]),
        "w_pool": f(inputs["w_pool"][0]),
        "pool_scaleT": f(inputs["pool_scale"][0].reshape(8, 128).T),
        "rel_bias": f(inputs["rel_bias"]),
        "w_out": f(inputs["w_out"][0]),
        "g_ffn": f(inputs["g_ffn"][0].reshape(1, -1)),
        "wr": f(np.concatenate([inputs["w_group"][0], inputs["w_router"][0], np.zeros((D, 28), np.float32)], axis=1).reshape(16, 128, 64).transpose(1, 0, 2)),
        "br": f(np.broadcast_to(np.concatenate([inputs["b_group"][0], inputs["b_router"][0]]).reshape(1, -1), (128, 36))),
        "g_final": f(inputs["g_final"].reshape(1, -1)),
    }
    wgl = inputs["w_gate"][0].reshape(32, 16, 128, 512).transpose(0, 2, 1, 3).reshape(32 * 128, 16, 512)
    wul = inputs["w_up"][0].reshape(32, 16, 128, 512).transpose(0, 2, 1, 3).reshape(32 * 128, 16, 512)
    wdl = inputs["w_down"][0].reshape(32, 4, 128, D).transpose(0, 2, 1, 3).reshape(32 * 128, 4, D)
    for j in range(4):
        m["w_gate_%d" % j] = f(wgl[:, 4 * j:4 * j + 4, :].reshape(32 * 128, 2048))
        m["w_up_%d" % j] = f(wul[:, 4 * j:4 * j + 4, :].reshape(32 * 128, 2048))
        m["w_down_%d" % j] = f(wdl[:, j, :])
    for k, v in consts.items():
        m["c_" + k] = v
    return m


def make_in_map(inputs, b, consts, shared=None):
    if shared is None:
        shared = make_shared(inputs, consts)
    m = dict(shared)
    m["x"] = np.ascontiguousarray(inputs["x"][b])
    m["cT"] = np.ascontiguousarray(inputs["c"][b].reshape(16, 128).T)
    return m


_CACHE = {}


def kernel(**inputs):
    inputs = {k: np.asarray(v) for k, v in inputs.items()}
    if "nc" not in _CACHE:
        nc, P, st, dbg = build()
        finish(nc, P, st, dbg.pop("__final__"))
        _CACHE["nc"] = nc
    nc = _CACHE["nc"]
    consts = host_consts()
    shared = make_shared(inputs, consts)
    in_maps = [make_in_map(inputs, b, consts, shared) for b in range(8)]
    res = run_bass_kernel_spmd(nc, in_maps, core_ids=list(range(8)))
    return np.stack([np.asarray(r["out"], dtype=np.float32) for r in res.results], axis=0)
```

```python
import numpy as np
import ml_dtypes
import concourse.bass as bass
import concourse.mybir as mybir
from concourse.bass_utils import run_bass_kernel_spmd

F32 = mybir.dt.float32
BF16 = mybir.dt.bfloat16
I32 = mybir.dt.int32
U32 = mybir.dt.uint32
ALU = mybir.AluOpType
AF = mybir.ActivationFunctionType
AX = mybir.AxisListType


class _Op:
    __slots__ = ("eng", "fn", "reads", "writes", "dma", "deps", "signal", "waits",
                 "sigval", "dsem", "dval", "after", "is_bar")

    def __init__(self, eng, fn, reads, writes, dma, after):
        self.eng = eng; self.fn = fn; self.reads = reads; self.writes = writes
        self.dma = dma; self.deps = (); self.signal = False; self.waits = []
        self.sigval = 0; self.dsem = None; self.dval = 0; self.after = after


class Prog:
    ENGS = ("pe", "dve", "act", "pool", "sp")

    def __init__(self, nc):
        self.nc = nc
        self.ops = []
        self.off = 16512
        self.nalloc = 0

    def sb(self, shape, dtype, name=None):
        esz = {F32: 4, BF16: 2, I32: 4, U32: 4}[dtype]
        n = 1
        for s in shape[1:]:
            n *= s
        nbytes = (n * esz + 63) // 64 * 64
        self.nalloc += 1
        name = "%s_%d" % (name or "t", self.nalloc)
        t = self.nc.alloc_sbuf_tensor_at(name, list(shape), dtype, offset=self.off)
        self.off += nbytes
        assert self.off <= 229376, ("sbuf overflow", self.off)
        return t

    def add(self, eng, fn, reads=(), writes=(), dma=False, after=()):
        self.ops.append(_Op(eng, fn, tuple(reads), tuple(writes), dma, tuple(after)))
        return len(self.ops) - 1

    def dma(self, q, out, in_, reads=(), writes=(), **kw):
        return self.add(q, lambda e: e.dma_start(out=out, in_=in_, **kw), reads, writes, dma=True)

    def barrier(self):
        deps = [i for i, o in enumerate(self.ops) if o.dma]
        for e in ("pe", "dve", "act", "pool"):
            for i in range(len(self.ops) - 1, -1, -1):
                o = self.ops[i]
                if o.eng == e and not o.dma and o.fn is not None and not getattr(o, "is_bar", False):
                    deps.append(i)
                    break
        src, dst = self.bar_src, self.bar_dst
        b = self.add("sp", lambda en: en.dma_start(out=dst, in_=src), dma=True, after=deps)
        for e in ("pe", "dve", "act", "pool"):
            self.add(e, None, after=[b])

    def emit(self):
        nc = self.nc
        ops = self.ops
        last_w = {}
        readers = {}
        for i, op in enumerate(ops):
            deps = set(op.after)
            for t in op.reads:
                if t in last_w:
                    deps.add(last_w[t])
            for t in op.writes:
                if t in last_w:
                    deps.add(last_w[t])
                deps.update(readers.get(t, ()))
            for t in op.reads:
                readers.setdefault(t, []).append(i)
            for t in op.writes:
                last_w[t] = i
                readers[t] = []
            deps.discard(i)
            op.deps = deps
        seen = {e: {e2: -1 for e2 in self.ENGS} for e in self.ENGS}
        dma_seen = {e: set() for e in self.ENGS}
        NQ = {"sp": 20, "act": 8, "pool": 16}
        NOUT = {"sp": 20, "act": 8, "pool": 10}
        dma_count = {q: 0 for q in NQ}
        dma_hist = {q: [] for q in NQ}
        for i, op in enumerate(ops):
            E = op.eng
            waits = []
            if op.dma:
                k = dma_count[E]
                dma_count[E] += 1
                dma_hist[E].append(i)
                if k >= NOUT[E]:
                    prev = dma_hist[E][k - NOUT[E]]
                    if prev not in dma_seen[E]:
                        waits.append(("d", prev))
                        dma_seen[E].add(prev)
                op.dsem = (E, k % NQ[E])
                op.dval = 16 * (k // NQ[E] + 1)
            for j in sorted(op.deps):
                oj = ops[j]
                if oj.dma:
                    if j not in dma_seen[E]:
                        dma_seen[E].add(j)
                        waits.append(("d", j))
                else:
                    E2 = oj.eng
                    if E2 == E and E == "pe" and not op.dma:
                        continue
                    if seen[E][E2] >= j:
                        continue
                    seen[E][E2] = j
                    oj.signal = True
                    waits.append(("c", j))
            op.waits = waits
        CH = 1000
        cnt = {e: 0 for e in self.ENGS}
        for op in ops:
            if op.signal:
                op.sigval = (cnt[op.eng] // CH, cnt[op.eng] % CH + 1)
                cnt[op.eng] += 1
        self.stats = dict(cnt)
        self.stats["nops"] = len(ops)
        from contextlib import ExitStack
        with ExitStack() as st:
            csem = {}
            for e in self.ENGS:
                for c in range(cnt[e] // CH + 1):
                    csem[(e, c)] = st.enter_context(nc.semaphore("c_%s_%d" % (e, c)))
            dsem = {}
            for q, n in NQ.items():
                for k in range(n):
                    dsem[(q, k)] = st.enter_context(nc.semaphore("d_%s_%d" % (q, k)))
            block = st.enter_context(nc.Block())

            def run(engname):
                def body(eng):
                    for op in ops:
                        if op.eng != engname:
                            continue
                        for kind, j in op.waits:
                            oj = ops[j]
                            if kind == "d":
                                eng.wait_ge(dsem[oj.dsem], oj.dval)
                            else:
                                eng.wait_ge(csem[(oj.eng, oj.sigval[0])], oj.sigval[1])
                        if op.fn is None:
                            assert not op.signal and not op.dma
                            continue
                        ins = op.fn(eng)
                        if op.dma:
                            ins.then_inc(dsem[op.dsem], 16)
                        elif op.signal:
                            ins.then_inc(csem[(op.eng, op.sigval[0])], 1)
                return body

            block.tensor(run("pe"))
            block.vector(run("dve"))
            block.scalar(run("act"))
            block.gpsimd(run("pool"))
            block.sync(run("sp"))


S = 2048
D = 2048
NT = 16
NBLK = 63
NSLOT = NBLK * 128
SM_SCALE = 128 ** -0.5


def host_consts():
    c = {}
    c["ident_f"] = np.eye(128, dtype=np.float32)
    c["ident_b"] = np.eye(128, dtype=np.float32).astype(ml_dtypes.bfloat16)
    q = np.arange(128)[:, None]; s = np.arange(128)[None, :]
    c["causal_neg"] = np.where(s <= q, 0.0, -1e30).astype(np.float32)
    c["ltri"] = (q < s).astype(np.float32).astype(ml_dtypes.bfloat16)
    c["ones_b"] = np.ones((128, 128), np.float32).astype(ml_dtypes.bfloat16)
    c["ones_f"] = np.ones((128, 128), np.float32)
    c["anti_f"] = np.eye(128, dtype=np.float32)[::-1].copy()
    corr = np.ones((4, 16), np.float32)
    for g, w in enumerate((2, 4, 8, 16)):
        for t in range(16):
            corr[g, t] = w / min(t + 1, w)
    c["corr"] = np.broadcast_to(corr[None], (128, 4, 16)).copy()
    oh = np.zeros((32, 384), np.float32)
    for m in range(383):
        r = max(m - 127, 0)
        if r < 16:
            b = r
        else:
            rf = np.float32(max(r, 1))
            b = 16 + int(np.float32(np.log(rf / np.float32(16)) / np.float32(np.log(8.0)) * np.float32(16)))
            b = min(b, 31)
        oh[b, m] = 1.0
    c["bucket_oh"] = oh
    e1 = np.arange(32)[:, None]; e2 = np.arange(32)[None, :]
    c["u_strict"] = (e1 < e2).astype(np.float32)
    c["u_incl"] = (e1 <= e2).astype(np.float32)
    c["thr16"] = np.broadcast_to((np.arange(16) * 128).astype(np.float32)[None], (32, 16)).copy()
    bg = np.broadcast_to(np.arange(NBLK, dtype=np.float32)[None, :, None], (128, NBLK, 32)).copy()
    c["bgrid"] = bg
    return c


CONST_SPECS = [("ident_f", [128, 128], F32), ("ident_b", [128, 128], BF16), ("causal_neg", [128, 128], F32),
               ("ltri", [128, 128], BF16), ("ones_b", [128, 128], BF16), ("ones_f", [128, 128], F32), ("anti_f", [128, 128], F32),
               ("corr", [128, 4, 16], F32), ("bucket_oh", [32, 384], F32), ("u_strict", [32, 32], F32),
               ("u_incl", [32, 32], F32), ("thr16", [32, 16], F32), ("bgrid", [128, NBLK, 32], F32)]


def w_in_view_(w_in, c0, n):
    return w_in[:, c0:c0 + n].rearrange("(kc p) n -> p kc n", p=128)


def build(stop=None):
    nc = bass.Bass("TRN2", target_bir_lowering=False)

    def din(name, shape, dt=F32):
        return nc.dram_tensor(name, list(shape), dt, kind="ExternalInput").ap()

    def dscratch(name, shape, dt=F32):
        return nc.dram_tensor(name, list(shape), dt, kind="Internal").ap()

    x = din("x", [S, D]); cT = din("cT", [128, 16]); w_ada = din("w_ada", [D, 6 * D]); b_ada = din("b_ada", [1, 6 * D])
    w_kw = din("w_kw", [D, 80])
    g_mixT = din("g_mixT", [128, 16]); w_in = din("w_in", [D, 3664]); w_pool = din("w_pool", [4, 256, 256])
    pool_scaleT = din("pool_scaleT", [128, 8]); rel_bias = din("rel_bias", [32, 8]); w_out = din("w_out", [D, D])
    g_ffn = din("g_ffn", [1, D]); wr = din("wr", [128, 16, 64]); br = din("br", [128, 36])
    NE_DECL = 32 if stop in (None, "E", "F") else 1
    w_gate = [din("w_gate_%d" % j, [NE_DECL * 128, 2048]) for j in range(4)]
    w_up = [din("w_up_%d" % j, [NE_DECL * 128, 2048]) for j in range(4)]
    w_down = [din("w_down_%d" % j, [NE_DECL * 128, 2048]) for j in range(4)]
    g_final = din("g_final", [1, D])
    cst = {n: din("c_" + n, shp, dt) for n, shp, dt in CONST_SPECS}
    out = nc.dram_tensor("out", [S, D], F32, kind="ExternalOutput").ap()
    dbg = {}

    def dbg_out(name, shape, dt=F32):
        dbg[name] = nc.dram_tensor("dbg_" + name, list(shape), dt, kind="ExternalOutput").ap()
        return dbg[name]

    modd = dscratch("modd", [1, 6 * D])
    fextd = dscratch("fextd", [8, 384])
    x1d = dscratch("x1d", [S, D])
    h2d = dscratch("h2d", [S, D], BF16)
    Xs = dscratch("Xs", [NSLOT, D], BF16)
    Ys = dscratch("Ys", [NSLOT, D])

    P = Prog(nc)
    bar_t = P.sb([1, 16], F32, "bar_t")
    P.add("pool", lambda e: e.memset(bar_t[:], 0.0), writes=["bar_t"])
    P.bar_src = bar_t[:]
    P.bar_dst = dscratch("bar_d", [1, 16])
    from contextlib import ExitStack
    st = ExitStack()
    ps = [st.enter_context(nc.psum_tensor("ps%d" % k, [128, 512], F32)) for k in range(6)]
    pb = [st.enter_context(nc.psum_tensor("pb%d" % k, [128, 1024], BF16)) for k in range(2)]
    PS = ["ps%d" % k for k in range(6)]
    PB = ["pb0", "pb1"]

    ident_f = P.sb([128, 128], F32, "ident_f"); ident_b = P.sb([128, 128], BF16, "ident_b")
    causal_neg = P.sb([128, 128], F32, "causal_neg"); ltri = P.sb([128, 128], BF16, "ltri")
    ones_b = P.sb([128, 128], BF16, "ones_b"); ones_f = P.sb([128, 128], F32, "ones_f")
    corr = P.sb([128, 4, 16], F32, "corr")
    for n, t in (("ident_f", ident_f), ("ident_b", ident_b), ("causal_neg", causal_neg), ("ltri", ltri),
                 ("ones_b", ones_b), ("ones_f", ones_f), ("corr", corr)):
        P.dma("sp", t[:], cst[n], writes=[n])
    modT = P.sb([128, 96], F32, "modT")
    s1T = P.sb([128, 16], F32, "s1T")
    gmT = P.sb([128, 16], F32, "gmT")
    pscT = P.sb([128, 8], F32, "pscT")
    eps_t = P.sb([128, 1], F32, "eps")
    P.dma("sp", gmT[:], g_mixT, writes=["gmT"])
    P.dma("sp", pscT[:], pool_scaleT, writes=["pscT"])
    P.add("pool", lambda e: e.memset(eps_t[:], 1e-6), writes=["eps"])
    base_mark = P.off

    scT = P.sb([128, 16], F32, "scT")
    wa = [P.sb([128, 16, 512], BF16, "wa%d" % k) for k in range(3)]
    scTb = P.sb([128, 16], BF16, "scTb")
    mod_row = P.sb([1, 6 * D], F32, "mod_row")
    ba_row = P.sb([1, 6 * D], F32, "ba_row")
    gf_row = P.sb([1, D], F32, "gf_row")
    P.dma("sp", scT[:], cT, writes=["scT"])
    P.dma("sp", ba_row[:], b_ada, writes=["ba_row"])
    P.dma("sp", gf_row[:], g_ffn, writes=["gf_row"])
    P.add("act", lambda e: e.activation(out=scTb[:], in_=scT[:], func=AF.Silu), reads=["scT"], writes=["scTb"])
    zt = P.sb([128, D], BF16, "zt")
    P.add("pool", lambda e: e.memset(zt[:], 0.0), writes=["zt"])
    for b in range(NBLK):
        P.dma("act", Xs[b * 128:(b + 1) * 128, :], zt[:], reads=["zt"], writes=["Xs_zero_%d" % b])
    for n in range(24):
        wt = wa[n % 3]; wn = "wa%d" % (n % 3)
        P.dma("pool", wt[:], w_ada[:, n * 512:(n + 1) * 512].rearrange("(kc p) n -> p kc n", p=128), writes=[wn])
        pt = ps[n % 2]; pn = PS[n % 2]
        for kc in range(16):
            P.add("pe", lambda e, pt=pt, wt=wt, kc=kc: e.matmul(pt[0:1, :], lhsT=scTb[:, kc:kc + 1], rhs=wt[:, kc, :], start=(kc == 0), stop=(kc == 15)),
                  reads=["scTb", wn], writes=[pn])
        P.add("dve", lambda e, pt=pt, n=n: e.tensor_tensor(out=mod_row[0:1, n * 512:(n + 1) * 512], in0=pt[0:1, :], in1=ba_row[0:1, n * 512:(n + 1) * 512], op=ALU.add),
              reads=[pn, "ba_row"], writes=["mod_row"])
    P.add("dve", lambda e: e.scalar_tensor_tensor(out=mod_row[0:1, 4 * D:5 * D], in0=mod_row[0:1, 4 * D:5 * D], scalar=1.0, in1=gf_row[0:1, :], op0=ALU.add, op1=ALU.mult),
          reads=["mod_row", "gf_row"], writes=["mod_row"])
    P.dma("sp", modd, mod_row[:], reads=["mod_row"], writes=["modd"])
    for j in range(96):
        P.add("pe", lambda e, j=j: e.matmul(ps[2][:, j:j + 1], lhsT=mod_row[0:1, j * 128:(j + 1) * 128], rhs=ones_f[0:1, 0:1], start=True, stop=True),
              reads=["mod_row", "ones_f"], writes=[PS[2]])
    P.add("dve", lambda e: e.tensor_copy(out=modT[:], in_=ps[2][:, 0:96]), reads=[PS[2]], writes=["modT"])
    P.add("dve", lambda e: e.scalar_tensor_tensor(out=s1T[:], in0=modT[:, 16:32], scalar=1.0, in1=gmT[:], op0=ALU.add, op1=ALU.mult),
          reads=["modT", "gmT"], writes=["s1T"])
    if stop == "A":
        d = dbg_out("modT", [128, 96])
        P.dma("sp", d, modT[:], reads=["modT"], writes=["dbg"])
        return nc, P, st, dbg
    P.barrier()
    P.off = base_mark

    hT = P.sb([128, 16, S], BF16, "hT")
    proj_mark = P.off
    xs = [P.sb([128, D], F32, "xs%d" % k) for k in range(2)]
    xn = [P.sb([128, D], BF16, "xn%d" % k) for k in range(2)]
    junk = P.sb([128, D], BF16, "junk")
    ss = P.sb([128, 16], F32, "ss"); rt = P.sb([128, 16], F32, "rt"); rstd = P.sb([128, 16], F32, "rstd")
    P.add("pool", lambda e: e.memset(ss[:], 0.0), writes=["ss"])
    for i in range(NT):
        xt = xs[i % 2]; xtn = "xs%d" % (i % 2); xb = xn[i % 2]; xbn = "xn%d" % (i % 2)
        P.dma("sp", xt[:], x[i * 128:(i + 1) * 128, :], writes=[xtn])
        P.add("act", lambda e, xt=xt, i=i: e.activation(out=junk[:], in_=xt[:], func=AF.Square, accum_out=ss[:, i:i + 1]),
              reads=[xtn, "ss"], writes=["junk", "ss"])
        P.add("act", lambda e, i=i: e.activation(out=rt[:, i:i + 1], in_=ss[:, i:i + 1], func=AF.Sqrt, bias=eps_t[:, 0:1], scale=1.0 / D),
              reads=["ss", "eps"], writes=["rt"])
        P.add("dve", lambda e, i=i: e.reciprocal(out=rstd[:, i:i + 1], in_=rt[:, i:i + 1]), reads=["rt"], writes=["rstd"])
        P.add("dve", lambda e, xt=xt, xb=xb, i=i: e.tensor_scalar(out=xb[:], in0=xt[:], scalar1=rstd[:, i:i + 1], scalar2=None, op0=ALU.mult),
              reads=[xtn, "rstd"], writes=[xbn])
        for c in range(16):
            pbt = pb[c // 8]; pbn = PB[c // 8]
            P.add("pe", lambda e, xb=xb, c=c, pbt=pbt: e.transpose(out=pbt[:, (c % 8) * 128:(c % 8 + 1) * 128], in_=xb[:, c * 128:(c + 1) * 128], identity=ident_b[:]),
                  reads=[xbn, "ident_b"], writes=[pbn])
        for c in range(16):
            pbt = pb[c // 8]; pbn = PB[c // 8]
            P.add("act", lambda e, c=c, i=i, pbt=pbt: e.activation(out=hT[:, c, i * 128:(i + 1) * 128], in_=pbt[:, (c % 8) * 128:(c % 8 + 1) * 128], func=AF.Identity,
                                                                   bias=modT[:, c:c + 1], scale=s1T[:, c:c + 1]),
                  reads=[pbn, "modT", "s1T"], writes=["hT_%d" % i])
    HT = ["hT_%d" % i for i in range(NT)]
    if stop == "B1":
        d = dbg_out("hT", [128, 16, S], BF16)
        P.dma("sp", d, hT[:], reads=HT, writes=["dbg"])
        return nc, P, st, dbg
    P.barrier()
    P.off = proj_mark

    uT = P.sb([128, 8, S], BF16, "uT"); qT = P.sb([128, 8, S], BF16, "qT"); qiT = P.sb([128, 8, S], BF16, "qiT")
    kT = P.sb([128, 2, S], BF16, "kT"); vtok = P.sb([128, NT, 256], BF16, "vtok"); kiT2 = P.sb([128, S], BF16, "kiT2")
    wi_all = P.sb([128, NT, 16], F32, "wi_all")
    after_proj_mark = P.off
    wst = [P.sb([128, 16, 272], BF16, "wst%d" % k) for k in range(2)]
    kw_st = P.sb([128, 16, 80], F32, "kw_st")
    P.dma("sp", kw_st[:], w_kw.rearrange("(kc p) n -> p kc n", p=128), writes=["kw_st"])
    evac_n = [0]

    def evac(dst, src, reads, writes, eng=None):
        k = evac_n[0]; evac_n[0] += 1
        if eng == "act" or (eng is None and k % 2 == 0):
            P.add("act", lambda e: e.copy(out=dst, in_=src), reads=reads, writes=writes)
        else:
            P.add("dve", lambda e: e.tensor_copy(out=dst, in_=src), reads=reads, writes=writes)

    def w_in_view(c0, n):
        return w_in_view_(w_in, c0, n)

    groups = []
    for g in range(4):
        groups.append((g * 256, "fm", [(uT, 2 * g), (uT, 2 * g + 1)]))
    for g in range(4):
        groups.append((1024 + g * 256, "fm", [(qT, 2 * g), (qT, 2 * g + 1)]))
    groups.append((2048, "fm", [(kT, 0), (kT, 1)]))
    for g in range(4):
        groups.append((2560 + g * 256, "fm", [(qiT, 2 * g), (qiT, 2 * g + 1)]))
    groups.append((3584, "ki", None))
    groups.append((2304, "tm", None))
    gi = 0
    pk = 0
    for c0, kind, dests in groups:
        if stop == "B2a" and gi == 1:
            d = dbg_out("uT01", [128, 2, S], BF16)
            P.dma("sp", d, uT[:, 0:2, :], reads=[uT.name + "_0", uT.name + "_1"], writes=["dbg_uT01"])
            return nc, P, st, dbg
        if stop == "B2c" and kind == "tm":
            d = dbg_out("kiT2", [128, S], BF16)
            P.dma("sp", d, kiT2[:], reads=["kiT2"], writes=["dbg_kiT2"])
            return nc, P, st, dbg
        if stop == "B2b" and kind == "ki":
            d = dbg_out("qiT", [128, 8, S], BF16)
            P.dma("sp", d, qiT[:], reads=[qiT.name + "_%d" % c for c in range(8)], writes=["dbg_qiT"])
            return nc, P, st, dbg
        wt = wst[gi % 2]; wn = "wst%d" % (gi % 2); gi += 1
        if kind == "fm":
            P.dma("pool", wt[:, :, 0:256], w_in_view(c0, 256), writes=[wn])
            for sub, (dt_, ch) in enumerate(dests):
                for tc in range(4):
                    pt = ps[pk % 4]; pn = PS[pk % 4]; pk += 1
                    for kc in range(16):
                        P.add("pe", lambda e, pt=pt, wt=wt, kc=kc, sub=sub, tc=tc: e.matmul(pt[:], lhsT=wt[:, kc, sub * 128:(sub + 1) * 128], rhs=hT[:, kc, tc * 512:(tc + 1) * 512], start=(kc == 0), stop=(kc == 15)),
                              reads=[wn] + HT[tc * 4:tc * 4 + 4], writes=[pn])
                    evac(dt_[:, ch, tc * 512:(tc + 1) * 512], pt[:], [pn], [dt_.name + "_%d" % ch])
        elif kind == "ki":
            P.add("dve", lambda e, wt=wt: e.tensor_copy(out=wt[:, :, 0:64], in_=kw_st[:, :, 0:64]), reads=["kw_st"], writes=[wn])
            P.add("dve", lambda e, wt=wt: e.tensor_copy(out=wt[:, :, 64:128], in_=kw_st[:, :, 0:64]), reads=["kw_st"], writes=[wn])
            for tc in range(4):
                pt = ps[pk % 4]; pn = PS[pk % 4]; pk += 1
                for kc in range(16):
                    P.add("pe", lambda e, pt=pt, wt=wt, kc=kc, tc=tc: e.matmul(pt[:], lhsT=wt[:, kc, 0:128], rhs=hT[:, kc, tc * 512:(tc + 1) * 512], start=(kc == 0), stop=(kc == 15)),
                          reads=[wn] + HT[tc * 4:tc * 4 + 4], writes=[pn])
                evac(kiT2[:, tc * 512:(tc + 1) * 512], pt[:], [pn], ["kiT2"])
        else:
            P.dma("pool", wt[:, :, 0:256], w_in_view(2304, 256), writes=[wn])
            P.add("dve", lambda e, wt=wt: e.tensor_copy(out=wt[:, :, 256:272], in_=kw_st[:, :, 64:80]), reads=["kw_st"], writes=[wn])
            for i in range(NT):
                pt = ps[pk % 4]; pn = PS[pk % 4]; pk += 1
                for kc in range(16):
                    P.add("pe", lambda e, pt=pt, wt=wt, kc=kc, i=i: e.matmul(pt[:, 0:256], lhsT=hT[:, kc, i * 128:(i + 1) * 128], rhs=wt[:, kc, 0:256], start=(kc == 0), stop=(kc == 15)),
                          reads=[wn, HT[i]], writes=[pn])
                P.add("act", lambda e, pt=pt, i=i: e.copy(out=vtok[:, i, :], in_=pt[:, 0:256]), reads=[pn], writes=["vtok"])
                pt2 = ps[4 + i % 2]; pn2 = PS[4 + i % 2]
                for kc in range(16):
                    P.add("pe", lambda e, pt2=pt2, wt=wt, kc=kc, i=i: e.matmul(pt2[:, 0:16], lhsT=hT[:, kc, i * 128:(i + 1) * 128], rhs=wt[:, kc, 256:272], start=(kc == 0), stop=(kc == 15)),
                          reads=[wn, HT[i]], writes=[pn2])
                P.add("dve", lambda e, pt2=pt2, i=i: e.tensor_copy(out=wi_all[:, i, :], in_=pt2[:, 0:16]), reads=[pn2], writes=["wi_all"])
    UT = [uT.name + "_%d" % c for c in range(8)]; QT = [qT.name + "_%d" % c for c in range(8)]
    QIT = [qiT.name + "_%d" % c for c in range(8)]; KT = [kT.name + "_%d" % c for c in range(2)]
    if stop == "B2":
        for nm, t, toks, shp, dt_ in (("uT", uT, UT, [128, 8, S], BF16), ("qT", qT, QT, [128, 8, S], BF16), ("qiT", qiT, QIT, [128, 8, S], BF16),
                                      ("kT", kT, KT, [128, 2, S], BF16), ("vtok", vtok, ["vtok"], [128, NT, 256], BF16),
                                      ("kiT2", kiT2, ["kiT2"], [128, S], BF16), ("wi_all", wi_all, ["wi_all"], [128, NT, 16], F32)):
            d = dbg_out(nm, shp, dt_)
            P.dma("sp", d, t[:], reads=toks, writes=["dbg_" + nm])
        return nc, P, st, dbg
    P.barrier()
    P.off = base_mark
    pool_outT = P.sb([128, 8, S], BF16, "pool_outT"); attn_outT = P.sb([128, 8, S], BF16, "attn_outT")
    POT = ["pool_outT_%d" % c for c in range(8)]; AOT = ["attn_outT_%d" % i for i in range(NT)]
    P.off = after_proj_mark
    A_ = P.sb([128, S], F32, "poolA"); B_ = P.sb([128, S], F32, "poolB"); wp = P.sb([128, 4, 2, 256], BF16, "wp")
    P.dma("pool", wp[:], w_pool.rearrange("g (cc p) d -> p g cc d", p=128), writes=["wp"])
    for c in range(8):
        g = c // 2; w = 2 << g
        u = uT[:, c, :]; un = UT[c]
        P.add("dve", lambda e, u=u: e.tensor_tensor(out=A_[:, 1:S], in0=u[:, 1:S], in1=u[:, 0:S - 1], op=ALU.add), reads=[un], writes=["poolA"])
        P.add("dve", lambda e, u=u: e.tensor_copy(out=A_[:, 0:1], in_=u[:, 0:1]), reads=[un], writes=["poolA"])
        cur, curn, oth, othn = A_, "poolA", B_, "poolB"
        d = 2
        while d < w:
            P.add("dve", lambda e, cur=cur, oth=oth, d=d: e.tensor_tensor(out=oth[:, d:S], in0=cur[:, d:S], in1=cur[:, 0:S - d], op=ALU.add), reads=[curn], writes=[othn])
            P.add("dve", lambda e, cur=cur, oth=oth, d=d: e.tensor_copy(out=oth[:, 0:d], in_=cur[:, 0:d]), reads=[curn], writes=[othn])
            cur, curn, oth, othn = oth, othn, cur, curn
            d *= 2
        P.add("dve", lambda e, cur=cur, g=g: e.tensor_tensor(out=cur[:, 0:16], in0=cur[:, 0:16], in1=corr[:, g, :], op=ALU.mult), reads=[curn, "corr"], writes=[curn])
        P.add("dve", lambda e, cur=cur, u=u, w=w: e.scalar_tensor_tensor(out=u, in0=cur[:], scalar=1.0 / w, in1=u, op0=ALU.mult, op1=ALU.subtract), reads=[curn, un], writes=[un])
        if c % 2 == 1:
            for dch in range(2):
                for tc in range(4):
                    pt = ps[pk % 4]; pn = PS[pk % 4]; pk += 1
                    for cc in range(2):
                        P.add("pe", lambda e, pt=pt, g=g, cc=cc, dch=dch, tc=tc: e.matmul(pt[:], lhsT=wp[:, g, cc, dch * 128:(dch + 1) * 128], rhs=uT[:, 2 * g + cc, tc * 512:(tc + 1) * 512], start=(cc == 0), stop=(cc == 1)),
                              reads=["wp", UT[2 * g], UT[2 * g + 1]], writes=[pn])
                    P.add("act", lambda e, pt=pt, g=g, dch=dch, tc=tc: e.activation(out=pool_outT[:, 2 * g + dch, tc * 512:(tc + 1) * 512], in_=pt[:], func=AF.Identity, scale=pscT[:, 2 * g + dch:2 * g + dch + 1]),
                          reads=[pn, "pscT"], writes=[POT[2 * g + dch]])
    if stop == "B3":
        d = dbg_out("pool_outT", [128, 8, S], BF16)
        P.dma("sp", d, pool_outT[:], reads=POT, writes=["dbg_pool_outT"])
        return nc, P, st, dbg
    P.barrier()

    P.off = proj_mark
    acc = P.sb([128, S], F32, "acc"); maskq = P.sb([128, S], BF16, "maskq"); junkb = P.sb([128, S], BF16, "junkb")
    maskT = P.sb([128, NT, 128], BF16, "maskT"); EB3 = P.sb([128, 8, 384], F32, "EB3")
    assert P.off == proj_mark + 32768
    P.off = after_proj_mark
    rtile = [P.sb([128, 512], F32, "rtile%d" % k) for k in range(2)]
    Et = [P.sb([128, 512], BF16, "Et%d" % k) for k in range(2)]
    P1 = [P.sb([128, 512], BF16, "P1%d" % k) for k in range(2)]
    Pt = [P.sb([128, 512], BF16, "Pt%d" % k) for k in range(2)]
    rinv = P.sb([128, 512], F32, "rinv")
    lo = P.sb([128, 1], F32, "lo"); hi = P.sb([128, 1], F32, "hi"); mid = P.sb([128, 1], F32, "mid"); cnt = P.sb([128, 1], F32, "cnt")
    ge = P.sb([128, 1], U32, "ge"); lt = P.sb([128, 1], U32, "lt")
    relb = P.sb([32, 8], F32, "relb"); boh = P.sb([32, 384], F32, "boh"); fext = P.sb([8, 384], F32, "fext")
    hank = P.sb([128, 256], F32, "hank"); anti = P.sb([128, 128], F32, "anti")
    P.dma("sp", relb[:], rel_bias, writes=["relb"]); P.dma("sp", boh[:], cst["bucket_oh"], writes=["boh"])
    P.dma("sp", anti[:], cst["anti_f"], writes=["anti"])
    P.add("pe", lambda e: e.matmul(ps[0][0:8, 0:384], lhsT=relb[:], rhs=boh[:], start=True, stop=True), reads=["relb", "boh"], writes=[PS[0]])
    P.add("dve", lambda e: e.tensor_copy(out=fext[:], in_=ps[0][0:8, 0:384]), reads=[PS[0]], writes=["fext"])
    P.dma("sp", fextd, fext[:], reads=["fext"], writes=["fextd"])
    for h in range(8):
        P.dma("sp", hank[:], bass.AP(fextd.tensor, h * 384, [[1, 128], [1, 256]]), reads=["fextd"], writes=["hank"])
        P.add("pe", lambda e: e.matmul(ps[1][:, 0:256], lhsT=anti[:], rhs=hank[:], start=True, stop=True), reads=["anti", "hank"], writes=[PS[1]])
        P.add("act", lambda e, h=h: e.activation(out=EB3[:, h, 0:256], in_=ps[1][:, 0:256], func=AF.Exp), reads=[PS[1]], writes=["EB3"])
        P.add("act", lambda e, h=h: e.activation(out=EB3[:, h, 256:384], in_=ps[1][:, 255:256].to_broadcast([128, 128]), func=AF.Exp), reads=[PS[1]], writes=["EB3"])
    NIT = 15
    pow2 = P.sb([128, NIT], F32, "pow2"); wk_all = P.sb([128, NIT], F32, "wk_all"); sgh = P.sb([128, 1], F32, "sgh")
    for it in range(NIT):
        P.add("pool", lambda e, it=it: e.memset(pow2[:, it:it + 1], 0.5 ** (it + 1)), writes=["pow2"])
    ek = 0
    for i in range(NT):
        nk = (i + 1) * 128
        nch = (nk + 511) // 512
        for h in range(16):
            pr, hh = h // 2, h % 2
            b0 = 64 * hh
            for kc in range(nch):
                wd = min(512, nk - kc * 512)
                pt = ps[ek % 2]; pn = PS[ek % 2]; rtl = rtile[ek % 2]; rn = "rtile%d" % (ek % 2); ek += 1
                P.add("pe", lambda e, pt=pt, pr=pr, b0=b0, i=i, kc=kc, wd=wd: e.matmul(pt[:, 0:wd], lhsT=qiT[b0:b0 + 64, pr, i * 128:(i + 1) * 128], rhs=kiT2[b0:b0 + 64, kc * 512:kc * 512 + wd], start=True, stop=True),
                      reads=[QIT[pr], "kiT2"], writes=[pn])
                P.add("act", lambda e, pt=pt, rtl=rtl, wd=wd: e.activation(out=rtl[:, 0:wd], in_=pt[:, 0:wd], func=AF.Relu), reads=[pn], writes=[rn])
                if h == 0:
                    P.add("dve", lambda e, rtl=rtl, kc=kc, wd=wd, i=i, h=h: e.tensor_scalar(out=acc[:, kc * 512:kc * 512 + wd], in0=rtl[:, 0:wd], scalar1=wi_all[:, i, h:h + 1], scalar2=None, op0=ALU.mult),
                          reads=[rn, "wi_all"], writes=["acc_%d" % kc])
                else:
                    P.add("dve", lambda e, rtl=rtl, kc=kc, wd=wd, i=i, h=h: e.scalar_tensor_tensor(out=acc[:, kc * 512:kc * 512 + wd], in0=rtl[:, 0:wd], scalar=wi_all[:, i, h:h + 1], in1=acc[:, kc * 512:kc * 512 + wd], op0=ALU.mult, op1=ALU.add),
                          reads=[rn, "wi_all", "acc_%d" % kc], writes=["acc_%d" % kc])
        P.add("dve", lambda e, i=i: e.tensor_tensor(out=acc[:, i * 128:(i + 1) * 128], in0=acc[:, i * 128:(i + 1) * 128], in1=causal_neg[:], op=ALU.add), reads=["acc_0", "acc_1", "acc_2", "acc_3"] + ["causal_neg"], writes=["acc_0", "acc_1", "acc_2", "acc_3"])
        if i >= 2:
            P.add("dve", lambda e, nk=nk: e.tensor_reduce(out=hi[:], in_=acc[:, 0:nk], axis=AX.X, op=ALU.max), reads=["acc_0", "acc_1", "acc_2", "acc_3"], writes=["hi"])
            P.add("dve", lambda e, i=i: e.tensor_reduce(out=lo[:], in_=acc[:, 0:i * 128], axis=AX.X, op=ALU.min), reads=["acc_0", "acc_1", "acc_2", "acc_3"], writes=["lo"])
            P.add("dve", lambda e: e.tensor_tensor(out=hi[:], in0=hi[:], in1=lo[:], op=ALU.subtract), reads=["hi", "lo"], writes=["hi"])
            P.add("dve", lambda e: e.tensor_scalar(out=wk_all[:], in0=pow2[:], scalar1=hi[:, 0:1], scalar2=None, op0=ALU.mult), reads=["hi", "pow2"], writes=["wk_all"])
            P.add("dve", lambda e: e.scalar_tensor_tensor(out=mid[:], in0=hi[:], scalar=0.5, in1=lo[:], op0=ALU.mult, op1=ALU.add), reads=["lo", "hi"], writes=["mid"])
            for it in range(NIT):
                P.add("dve", lambda e, nk=nk: e.tensor_scalar(out=junkb[:, 0:nk], in0=acc[:, 0:nk], scalar1=mid[:, 0:1], scalar2=None, op0=ALU.is_ge, op1=ALU.add, accum_out=cnt[:, 0:1]),
                      reads=["acc_0", "acc_1", "acc_2", "acc_3"] + ["mid"], writes=["junkb", "cnt"])
                P.add("dve", lambda e: e.tensor_scalar(out=sgh[:], in0=cnt[:], scalar1=255.5, scalar2=0.5, op0=ALU.is_ge, op1=ALU.subtract), reads=["cnt"], writes=["sgh"])
                P.add("dve", lambda e, it=it: e.scalar_tensor_tensor(out=mid[:], in0=sgh[:], scalar=wk_all[:, it:it + 1], in1=mid[:], op0=ALU.mult, op1=ALU.add), reads=["sgh", "wk_all", "mid"], writes=["mid"])
            P.add("dve", lambda e: e.scalar_tensor_tensor(out=lo[:], in0=hi[:], scalar=-(0.5 ** (NIT + 1)), in1=mid[:], op0=ALU.mult, op1=ALU.add), reads=["hi", "mid"], writes=["lo"])
            P.add("dve", lambda e, nk=nk: e.tensor_scalar(out=maskq[:, 0:nk], in0=acc[:, 0:nk], scalar1=lo[:, 0:1], scalar2=None, op0=ALU.is_ge), reads=["acc_0", "acc_1", "acc_2", "acc_3"] + ["lo"], writes=["maskq"])
        else:
            P.add("dve", lambda e, nk=nk: e.tensor_scalar(out=maskq[:, 0:nk], in0=acc[:, 0:nk], scalar1=-1e29, scalar2=None, op0=ALU.is_ge), reads=["acc_0", "acc_1", "acc_2", "acc_3"], writes=["maskq"])
        for j0 in range(0, i + 1, 8):
            jj = list(range(j0, min(i + 1, j0 + 8)))
            pbt = pb[(j0 // 8) % 2]; pbn = PB[(j0 // 8) % 2]
            for j in jj:
                P.add("pe", lambda e, j=j, pbt=pbt: e.transpose(out=pbt[:, (j % 8) * 128:(j % 8 + 1) * 128], in_=maskq[:, j * 128:(j + 1) * 128], identity=ident_b[:]), reads=["maskq", "ident_b"], writes=[pbn])
            for j in jj:
                P.add("act", lambda e, j=j, pbt=pbt: e.copy(out=maskT[:, j, :], in_=pbt[:, (j % 8) * 128:(j % 8 + 1) * 128]), reads=[pbn], writes=["maskT"])
        steps = [(kv, j) for kv in range(2) for j in range(i + 1)]
        bufs = []
        for n_, (kv, j) in enumerate(steps):
            bufs.append(ek % 2); ek += 1

        def emit_S(n_):
            kv, j = steps[n_]; k2 = bufs[n_]
            pS = ps[2 + k2]
            P.add("pe", lambda e, pS=pS, kv=kv, j=j, i=i: e.matmul(pS[:], lhsT=kT[:, kv, j * 128:(j + 1) * 128], rhs=qT[:, 4 * kv:4 * kv + 4, i * 128:(i + 1) * 128], start=True, stop=True),
                  reads=[KT[kv]] + QT[4 * kv:4 * kv + 4], writes=[PS[2 + k2]])
        emit_S(0)
        for n_, (kv, j) in enumerate(steps):
            k2 = bufs[n_]
            off = 0 if j == i else (128 if j == i - 1 else 256)
            pS = ps[2 + k2]; pSn = PS[2 + k2]
            et = Et[k2]; etn = "Et%d" % k2; p1 = P1[k2]; p1n = "P1%d" % k2; ptt = Pt[k2]; ptn = "Pt%d" % k2
            if n_ + 1 < len(steps):
                emit_S(n_ + 1)
            P.add("act", lambda e, pS=pS, et=et: e.activation(out=et[:], in_=pS[:], func=AF.Exp, scale=SM_SCALE), reads=[pSn], writes=[etn])
            P.add("pool", lambda e, et=et, p1=p1, j=j: e.tensor_tensor(out=p1[:].rearrange("p (h q) -> p h q", h=4), in0=et[:].rearrange("p (h q) -> p h q", h=4), in1=maskT[:, j:j + 1, :].to_broadcast([128, 4, 128]), op=ALU.mult),
                  reads=[etn, "maskT"], writes=[p1n])
            P.add("dve", lambda e, p1=p1, ptt=ptt, kv=kv, off=off: e.tensor_tensor(out=ptt[:].rearrange("p (h q) -> p h q", h=4), in0=p1[:].rearrange("p (h q) -> p h q", h=4), in1=EB3[:, 4 * kv:4 * kv + 4, off:off + 128], op=ALU.mult),
                  reads=[p1n, "EB3"], writes=[ptn])
            P.add("pe", lambda e, ptt=ptt, kv=kv, j=j, i=i: e.matmul(ps[4][:], lhsT=vtok[:, j, kv * 128:(kv + 1) * 128], rhs=ptt[:], start=(j == 0), stop=(j == i)), reads=["vtok", ptn], writes=[PS[4]])
            P.add("pe", lambda e, ptt=ptt, j=j, i=i: e.matmul(ps[5][:], lhsT=ones_b[:], rhs=ptt[:], start=(j == 0), stop=(j == i)), reads=["ones_b", ptn], writes=[PS[5]])
            if j == i:
                P.add("dve", lambda e: e.reciprocal(out=rinv[:], in_=ps[5][:]), reads=[PS[5]], writes=["rinv"])
                P.add("dve", lambda e, kv=kv, i=i: e.tensor_tensor(out=attn_outT[:, 4 * kv:4 * kv + 4, i * 128:(i + 1) * 128], in0=ps[4][:].rearrange("p (h q) -> p h q", h=4), in1=rinv[:].rearrange("p (h q) -> p h q", h=4), op=ALU.mult),
                      reads=[PS[4], "rinv"], writes=[AOT[i]])
        if stop == "B4a" and i == 3:
            d = dbg_out("acc", [128, 512]); P.dma("sp", d, acc[:, 0:512], reads=["acc_0", "acc_1", "acc_2", "acc_3"], writes=["dbg_acc"])
            d = dbg_out("maskq", [128, 512], BF16); P.dma("sp", d, maskq[:, 0:512], reads=["maskq"], writes=["dbg_maskq"])
            d = dbg_out("thr", [128, 1]); P.dma("sp", d, lo[:], reads=["lo"], writes=["dbg_thr"])
            d = dbg_out("EB3", [128, 8, 384]); P.dma("sp", d, EB3[:], reads=["EB3"], writes=["dbg_EB3"])
            d = dbg_out("attn_outT", [128, 8, 512], BF16); P.dma("sp", d, attn_outT[:, :, 0:512], reads=AOT[0:4], writes=["dbg_attn_outT"])
            return nc, P, st, dbg
    if stop == "B4":
        d = dbg_out("attn_outT", [128, 8, S], BF16)
        P.dma("sp", d, attn_outT[:], reads=AOT, writes=["dbg_attn_outT"])
        return nc, P, st, dbg
    P.barrier()
    P.off = proj_mark
    O1 = P.sb([128, NT, 32], BF16, "O1"); O2 = P.sb([128, NT, 32], BF16, "O2"); Osum = P.sb([128, NT, 32], BF16, "Osum")
    g1 = P.sb([128, NT], F32, "g1"); g2 = P.sb([128, NT], F32, "g2")
    dest_i = P.sb([128, NT, 2], U32, "dest_i")
    blk_i = P.sb([128, NBLK], I32, "blk_i"); act_i = P.sb([128, NBLK], I32, "act_i"); widx = P.sb([128, NBLK], U32, "widx")
    route_mark = P.off
    wo = P.sb([128, 16, D], BF16, "wo")
    xt2 = [P.sb([128, D], F32, "xt2_%d" % k) for k in range(2)]
    gt1_bc = P.sb([128, D], F32, "gt1_bc"); s2_bc = P.sb([128, D], F32, "s2_bc"); sh2_bc = P.sb([128, D], F32, "sh2_bc")
    h2 = P.sb([128, D], F32, "h2"); h2b = P.sb([128, D], BF16, "h2b"); h2T = P.sb([128, 16, 128], F32, "h2T")
    wr_sb = P.sb([128, 16, 64], F32, "wr_sb"); br_bc = P.sb([128, 36], F32, "br_bc")
    ss2 = P.sb([128, NT], F32, "ss2"); rt2 = P.sb([128, NT], F32, "rt2"); rstd2 = P.sb([128, NT], F32, "rstd2")
    L_all = P.sb([128, NT, 36], F32, "L_all")
    for n in range(4):
        P.dma("pool", wo[:, :, n * 512:(n + 1) * 512], w_out[:, n * 512:(n + 1) * 512].rearrange("(kc p) n -> p kc n", p=128), writes=["wo"])
    P.dma("sp", gt1_bc[:], modd[0:1, 2 * D:3 * D].to_broadcast([128, D]), reads=["modd"], writes=["gt1_bc"])
    P.dma("sp", sh2_bc[:], modd[0:1, 3 * D:4 * D].to_broadcast([128, D]), reads=["modd"], writes=["sh2_bc"])
    P.dma("sp", s2_bc[:], modd[0:1, 4 * D:5 * D].to_broadcast([128, D]), reads=["modd"], writes=["s2_bc"])
    P.dma("sp", wr_sb[:], wr, writes=["wr_sb"])
    P.dma("sp", br_bc[:], br, writes=["br_bc"])
    P.add("pool", lambda e: e.memset(ss2[:], 0.0), writes=["ss2"])

    for i in range(NT):
        xt = xt2[i % 2]; xtn = "xt2_%d" % (i % 2)
        P.dma("sp", xt[:], x[i * 128:(i + 1) * 128, :], writes=[xtn])
        for n in range(4):
            for kc in range(16):
                src = pool_outT if kc < 8 else attn_outT
                srcn = POT[kc] if kc < 8 else AOT[i]
                P.add("pe", lambda e, n=n, kc=kc, src=src, i=i: e.matmul(ps[n][:], lhsT=src[:, kc % 8, i * 128:(i + 1) * 128], rhs=wo[:, kc, n * 512:(n + 1) * 512], start=(kc == 0), stop=(kc == 15)),
                      reads=[srcn, "wo"], writes=[PS[n]])
            P.add("dve", lambda e, n=n: e.tensor_tensor(out=h2[:, n * 512:(n + 1) * 512], in0=ps[n][:], in1=gt1_bc[:, n * 512:(n + 1) * 512], op=ALU.mult), reads=[PS[n], "gt1_bc"], writes=["h2"])
            P.add("pool", lambda e, n=n, xt=xt: e.tensor_tensor(out=xt[:, n * 512:(n + 1) * 512], in0=xt[:, n * 512:(n + 1) * 512], in1=h2[:, n * 512:(n + 1) * 512], op=ALU.add), reads=[xtn, "h2"], writes=[xtn])
        P.dma("sp", x1d[i * 128:(i + 1) * 128, :], xt[:], reads=[xtn], writes=["x1d_%d" % i])
        P.add("act", lambda e, xt=xt, i=i: e.activation(out=h2b[:], in_=xt[:], func=AF.Square, accum_out=ss2[:, i:i + 1]), reads=[xtn, "ss2"], writes=["h2b", "ss2"])
        P.add("act", lambda e, i=i: e.activation(out=rt2[:, i:i + 1], in_=ss2[:, i:i + 1], func=AF.Sqrt, bias=eps_t[:, 0:1], scale=1.0 / D), reads=["ss2", "eps"], writes=["rt2"])
        P.add("dve", lambda e, i=i: e.reciprocal(out=rstd2[:, i:i + 1], in_=rt2[:, i:i + 1]), reads=["rt2"], writes=["rstd2"])
        P.add("dve", lambda e, xt=xt, i=i: e.scalar_tensor_tensor(out=h2[:], in0=xt[:], scalar=rstd2[:, i:i + 1], in1=s2_bc[:], op0=ALU.mult, op1=ALU.mult), reads=[xtn, "rstd2", "s2_bc", "h2"], writes=["h2"])
        P.add("pool", lambda e: e.tensor_tensor(out=h2[:], in0=h2[:], in1=sh2_bc[:], op=ALU.add), reads=["h2", "sh2_bc"], writes=["h2"])
        P.add("act", lambda e: e.copy(out=h2b[:], in_=h2[:]), reads=["h2"], writes=["h2b"])
        P.dma("sp", h2d[i * 128:(i + 1) * 128, :], h2b[:], reads=["h2b"], writes=["h2d_%d" % i])
        for c0 in range(0, 16, 4):
            for c in range(c0, c0 + 4):
                P.add("pe", lambda e, c=c: e.transpose(out=ps[4][:, (c % 4) * 128:(c % 4 + 1) * 128], in_=h2[:, c * 128:(c + 1) * 128], identity=ident_f[:]), reads=["h2", "ident_f"], writes=[PS[4]])
            for c in range(c0, c0 + 4):
                evac(h2T[:, c, :], ps[4][:, (c % 4) * 128:(c % 4 + 1) * 128], [PS[4]], ["h2T"], eng=("act" if (c0 // 4) % 2 == 0 else "dve"))
        for c in range(16):
            P.add("pe", lambda e, c=c: e.matmul(ps[5][:, 0:64], lhsT=h2T[:, c, :], rhs=wr_sb[:, c, :], start=(c == 0), stop=(c == 15)), reads=["h2T", "wr_sb"], writes=[PS[5]])
        P.add("dve", lambda e, i=i: e.tensor_tensor(out=L_all[:, i, :], in0=ps[5][:, 0:36], in1=br_bc[:], op=ALU.add), reads=[PS[5], "br_bc"], writes=["L_all"])
    def bc(t, k):
        return t[:].unsqueeze(2).to_broadcast([128, NT, k])
    R = {n: P.sb([128, NT], F32, "R_" + n) for n in ("gmax", "gsum", "pg", "m1", "m2", "dd", "e2", "den", "rden")}
    R3 = {n: P.sb([128, NT, k], F32, "R3_" + n) for n, k in (("og", 4), ("eg", 4), ("es", 8), ("tmp", 8), ("o1", 8), ("es2", 8), ("o2", 8))}
    Lg = L_all[:, :, 0:4]
    def rop(eng, fn, reads, writes):
        P.add(eng, fn, reads=reads, writes=writes)
    rop("dve", lambda e: e.tensor_reduce(out=R["gmax"][:], in_=Lg, axis=AX.X, op=ALU.max), ["L_all"], ["gmax"])
    rop("dve", lambda e: e.tensor_tensor(out=R3["og"][:], in0=Lg, in1=bc(R["gmax"], 4), op=ALU.is_ge), ["L_all", "gmax"], ["og"])
    rop("dve", lambda e: e.tensor_tensor(out=R3["eg"][:], in0=Lg, in1=bc(R["gmax"], 4), op=ALU.subtract), ["L_all", "gmax"], ["eg"])
    rop("act", lambda e: e.activation(out=R3["eg"][:], in_=R3["eg"][:], func=AF.Exp), ["eg"], ["eg"])
    rop("dve", lambda e: e.tensor_reduce(out=R["gsum"][:], in_=R3["eg"][:], axis=AX.X, op=ALU.add), ["eg"], ["gsum"])
    rop("dve", lambda e: e.reciprocal(out=R["pg"][:], in_=R["gsum"][:]), ["gsum"], ["pg"])
    rop("dve", lambda e: e.tensor_tensor(out=R3["es"][:], in0=L_all[:, :, 4:12], in1=R3["og"][:, :, 0:1].to_broadcast([128, NT, 8]), op=ALU.mult), ["L_all", "og"], ["es"])
    for g in range(1, 4):
        rop("dve", lambda e, g=g: e.tensor_tensor(out=R3["tmp"][:], in0=L_all[:, :, 4 + 8 * g:12 + 8 * g], in1=R3["og"][:, :, g:g + 1].to_broadcast([128, NT, 8]), op=ALU.mult), ["L_all", "og"], ["tmp"])
        rop("dve", lambda e: e.tensor_tensor(out=R3["es"][:], in0=R3["es"][:], in1=R3["tmp"][:], op=ALU.add), ["es", "tmp"], ["es"])
    rop("dve", lambda e: e.tensor_reduce(out=R["m1"][:], in_=R3["es"][:], axis=AX.X, op=ALU.max), ["es"], ["m1"])
    rop("dve", lambda e: e.tensor_tensor(out=R3["o1"][:], in0=R3["es"][:], in1=bc(R["m1"], 8), op=ALU.is_ge), ["es", "m1"], ["o1"])
    rop("dve", lambda e: e.scalar_tensor_tensor(out=R3["es2"][:], in0=R3["o1"][:], scalar=-1e30, in1=R3["es"][:], op0=ALU.mult, op1=ALU.add), ["o1", "es"], ["es2"])
    rop("dve", lambda e: e.tensor_reduce(out=R["m2"][:], in_=R3["es2"][:], axis=AX.X, op=ALU.max), ["es2"], ["m2"])
    rop("dve", lambda e: e.tensor_tensor(out=R3["o2"][:], in0=R3["es2"][:], in1=bc(R["m2"], 8), op=ALU.is_ge), ["es2", "m2"], ["o2"])
    rop("dve", lambda e: e.tensor_tensor(out=R["dd"][:], in0=R["m2"][:], in1=R["m1"][:], op=ALU.subtract), ["m1", "m2"], ["dd"])
    rop("act", lambda e: e.activation(out=R["e2"][:], in_=R["dd"][:], func=AF.Exp), ["dd"], ["e2"])
    rop("dve", lambda e: e.tensor_scalar(out=R["den"][:], in0=R["e2"][:], scalar1=1.0, scalar2=None, op0=ALU.add), ["e2"], ["den"])
    rop("dve", lambda e: e.reciprocal(out=R["rden"][:], in_=R["den"][:]), ["den"], ["rden"])
    rop("dve", lambda e: e.tensor_tensor(out=g1[:], in0=R["pg"][:], in1=R["rden"][:], op=ALU.mult), ["pg", "rden"], ["g1"])
    rop("dve", lambda e: e.tensor_tensor(out=g2[:], in0=g1[:], in1=R["e2"][:], op=ALU.mult), ["g1", "e2"], ["g2"])
    for g in range(4):
        rop("dve", lambda e, g=g: e.tensor_tensor(out=O1[:, :, 8 * g:8 * g + 8], in0=R3["o1"][:], in1=R3["og"][:, :, g:g + 1].to_broadcast([128, NT, 8]), op=ALU.mult), ["o1", "og"], ["O1"])
        rop("dve", lambda e, g=g: e.tensor_tensor(out=O2[:, :, 8 * g:8 * g + 8], in0=R3["o2"][:], in1=R3["og"][:, :, g:g + 1].to_broadcast([128, NT, 8]), op=ALU.mult), ["o2", "og"], ["O2"])
    rop("dve", lambda e: e.tensor_tensor(out=Osum[:], in0=O1[:], in1=O2[:], op=ALU.add), ["O1", "O2"], ["Osum"])
    X1D = ["x1d_%d" % i for i in range(NT)]; H2D = ["h2d_%d" % i for i in range(NT)]
    if stop == "C":
        for nm, t, shp, dt_ in (("O1", O1, [128, NT, 32], BF16), ("O2", O2, [128, NT, 32], BF16), ("g1", g1, [128, NT], F32), ("g2", g2, [128, NT], F32)):
            d = dbg_out(nm, shp, dt_); P.dma("sp", d, t[:], reads=[nm], writes=["dbg_" + nm])
        d = dbg_out("x1", [S, D]); P.dma("sp", d, x1d, reads=X1D, writes=["dbg_x1"])
        d = dbg_out("h2", [S, D], BF16); P.dma("sp", d, h2d, reads=H2D, writes=["dbg_h2"])
        return nc, P, st, dbg
    P.barrier()
    P.off = route_mark
    us_sb = P.sb([32, 32], F32, "us_sb"); ui_sb = P.sb([32, 32], F32, "ui_sb"); thr16 = P.sb([32, 16], F32, "thr16")
    cmp16 = P.sb([32, 16], F32, "cmp16"); nblkT = P.sb([32, 1], F32, "nblkT"); nb_bc = P.sb([32, 128], F32, "nb_bc")
    pstart_s = P.sb([128, 32], F32, "pstart_s"); pend_sb = P.sb([128, 32], F32, "pend_sb")
    bgrid = P.sb([128, NBLK, 32], F32, "bgrid"); cmpb = P.sb([128, NBLK, 32], F32, "cmpb")
    blkf = P.sb([128, NBLK], F32, "blkf"); actf = P.sb([128, NBLK], F32, "actf")
    pidx = P.sb([128, 1], I32, "pidx"); pidxf = P.sb([128, 1], F32, "pidxf"); widxf = P.sb([128, NBLK], F32, "widxf")
    destf = P.sb([128, 32], F32, "destf"); dtmp = P.sb([128, 32], F32, "dtmp"); d12 = P.sb([128, 2], F32, "d12")
    h2bt = [P.sb([128, D], BF16, "h2bt%d" % k) for k in range(2)]
    P.dma("sp", us_sb[:], cst["u_strict"], writes=["us_sb"]); P.dma("sp", ui_sb[:], cst["u_incl"], writes=["ui_sb"])
    P.dma("sp", thr16[:], cst["thr16"], writes=["thr16"]); P.dma("sp", bgrid[:], cst["bgrid"], writes=["bgrid"])
    for i in range(NT):
        P.add("pe", lambda e, i=i: e.matmul(ps[0][0:32, 0:128], lhsT=Osum[:, i, :], rhs=ones_b[:], start=(i == 0), stop=(i == NT - 1)), reads=["Osum", "ones_b"], writes=[PS[0]])
    P.add("dve", lambda e: e.tensor_tensor(out=cmp16[:], in0=ps[0][0:32, 0:1].to_broadcast([32, 16]), in1=thr16[:], op=ALU.is_gt), reads=[PS[0], "thr16"], writes=["cmp16"])
    P.add("dve", lambda e: e.tensor_reduce(out=nblkT[:], in_=cmp16[:], axis=AX.X, op=ALU.add), reads=["cmp16"], writes=["nblkT"])
    P.add("dve", lambda e: e.tensor_scalar(out=nb_bc[:], in0=ones_f[0:32, :], scalar1=nblkT[:, 0:1], scalar2=None, op0=ALU.mult), reads=["nblkT", "ones_f"], writes=["nb_bc"])
    P.add("pe", lambda e: e.matmul(ps[1][:, 0:32], lhsT=nb_bc[:], rhs=us_sb[:], start=True, stop=True), reads=["nb_bc", "us_sb"], writes=[PS[1]])
    P.add("pe", lambda e: e.matmul(ps[1][:, 32:64], lhsT=nb_bc[:], rhs=ui_sb[:], start=True, stop=True), reads=["nb_bc", "ui_sb"], writes=[PS[1]])
    P.add("dve", lambda e: e.tensor_scalar(out=pstart_s[:], in0=ps[1][:, 0:32], scalar1=128.0, scalar2=None, op0=ALU.mult), reads=[PS[1]], writes=["pstart_s"])
    P.add("dve", lambda e: e.tensor_copy(out=pend_sb[:], in_=ps[1][:, 32:64]), reads=[PS[1]], writes=["pend_sb"])
    P.add("dve", lambda e: e.tensor_tensor(out=cmpb[:], in0=pend_sb[:].unsqueeze(1).to_broadcast([128, NBLK, 32]), in1=bgrid[:], op=ALU.is_le), reads=["pend_sb", "bgrid"], writes=["cmpb"])
    P.add("dve", lambda e: e.tensor_reduce(out=blkf[:], in_=cmpb[:], axis=AX.X, op=ALU.add), reads=["cmpb"], writes=["blkf"])
    P.add("dve", lambda e: e.tensor_scalar(out=blkf[:], in0=blkf[:], scalar1=31.0, scalar2=None, op0=ALU.min), reads=["blkf"], writes=["blkf"])
    P.add("dve", lambda e: e.tensor_copy(out=blk_i[:], in_=blkf[:]), reads=["blkf"], writes=["blk_i"])
    P.add("dve", lambda e: e.tensor_scalar(out=actf[:], in0=bgrid[:, :, 0], scalar1=pend_sb[:, 31:32], scalar2=None, op0=ALU.is_lt), reads=["bgrid", "pend_sb"], writes=["actf"])
    P.add("dve", lambda e: e.tensor_copy(out=act_i[:], in_=actf[:]), reads=["actf"], writes=["act_i"])
    P.add("pool", lambda e: e.iota(pidx[:], pattern=[[0, 1]], base=0, channel_multiplier=1), writes=["pidx"])
    P.add("dve", lambda e: e.tensor_copy(out=pidxf[:], in_=pidx[:]), reads=["pidx"], writes=["pidxf"])
    P.add("dve", lambda e: e.tensor_scalar(out=widxf[:], in0=blkf[:], scalar1=128.0, scalar2=pidxf[:, 0:1], op0=ALU.mult, op1=ALU.add), reads=["blkf", "pidxf"], writes=["widxf"])
    P.add("dve", lambda e: e.tensor_scalar(out=actf[:], in0=actf[:], scalar1=-1.0, scalar2=-8192.0, op0=ALU.add, op1=ALU.mult), reads=["actf"], writes=["actf"])
    P.add("dve", lambda e: e.tensor_tensor(out=widxf[:], in0=widxf[:], in1=actf[:], op=ALU.add), reads=["widxf", "actf"], writes=["widxf"])
    P.add("dve", lambda e: e.tensor_copy(out=widx[:], in_=widxf[:]), reads=["widxf"], writes=["widx"])
    for i in range(NT):
        pt = ps[2 + i % 2]; pn = PS[2 + i % 2]
        for j in range(i + 1):
            P.add("pe", lambda e, pt=pt, i=i, j=j: e.matmul(pt[:, 0:32], lhsT=(ones_b if j < i else ltri)[:], rhs=Osum[:, j, :], start=(j == 0), stop=(j == i)), reads=["Osum", "ones_b", "ltri"], writes=[pn])
        P.add("dve", lambda e, pt=pt: e.tensor_tensor(out=destf[:], in0=pt[:, 0:32], in1=pstart_s[:], op=ALU.add), reads=[pn, "pstart_s"], writes=["destf"])
        for a, Oa, on in ((0, O1, "O1"), (1, O2, "O2")):
            P.add("dve", lambda e, Oa=Oa, i=i: e.tensor_tensor(out=dtmp[:], in0=destf[:], in1=Oa[:, i, :], op=ALU.mult), reads=["destf", on], writes=["dtmp"])
            P.add("dve", lambda e, a=a: e.tensor_reduce(out=d12[:, a:a + 1], in_=dtmp[:], axis=AX.X, op=ALU.add), reads=["dtmp"], writes=["d12"])
        P.add("dve", lambda e, i=i: e.tensor_copy(out=dest_i[:, i, :], in_=d12[:]), reads=["d12"], writes=["dest_i_%d" % i])
        hb = h2bt[i % 2]; hbn = "h2bt%d" % (i % 2)
        P.dma("sp", hb[:], h2d[i * 128:(i + 1) * 128, :], reads=[H2D[i]], writes=[hbn])
        for a in range(2):
            P.add("pool", lambda e, hb=hb, i=i, a=a: e.indirect_dma_start(out=Xs, out_offset=bass.IndirectOffsetOnAxis(ap=dest_i[:, i, a:a + 1], axis=0), in_=hb[:], in_offset=None),
                  reads=[hbn, "dest_i_%d" % i] + ["Xs_zero_%d" % bb for bb in range(NBLK)], writes=["Xs_s_%d_%d" % (i, a)], dma=True)
    DEST = ["dest_i_%d" % i for i in range(NT)]
    XS = ["Xs_s_%d_%d" % (i, a) for i in range(NT) for a in range(2)]
    if stop == "D":
        d = dbg_out("dest", [128, NT, 2], U32); P.dma("sp", d, dest_i[:], reads=DEST, writes=["dbg_dest"])
        d = dbg_out("blk", [128, NBLK], I32); P.dma("sp", d, blk_i[:], reads=["blk_i"], writes=["dbg_blk"])
        d = dbg_out("act", [128, NBLK], I32); P.dma("sp", d, act_i[:], reads=["act_i"], writes=["dbg_act"])
        d = dbg_out("Xs", [NSLOT, D], BF16); P.dma("sp", d, Xs, reads=XS, writes=["dbg_Xs"])
        return nc, P, st, dbg
    P.barrier()

    P.off = route_mark
    wg = [P.sb([128, 16, 512], BF16, "wg%d" % k) for k in range(2)]
    wu = [P.sb([128, 16, 512], BF16, "wu%d" % k) for k in range(2)]
    wd = [P.sb([128, 4, D], BF16, "wd%d" % k) for k in range(2)]
    xe = [P.sb([128, D], BF16, "xe%d" % k) for k in range(2)]
    xeT = [P.sb([128, 16, 128], BF16, "xeT%d" % k) for k in range(2)]
    sg = P.sb([128, 512], F32, "sg"); a_bf = P.sb([128, 512], BF16, "a_bf"); aT = P.sb([128, 4, 128], BF16, "aT")
    yb = [P.sb([128, D], F32, "yb%d" % k) for k in range(2)]
    ET = mybir.EngineType
    bcreg = {}

    def mk_bc(e):
        bcreg["r"] = e.alloc_register("wbound")
        return e.reg_mov(bcreg["r"], 32 * 128 - 1)
    P.add("pool", mk_bc)
    for b in range(NBLK):
        k = b % 2
        for wt_, wsrc, wn_, is_down in ((wg[k], w_gate, "wg%d" % k, False), (wu[k], w_up, "wu%d" % k, False), (wd[k], w_down, "wd%d" % k, True)):
            for j in range(4):
                dst = wt_[:, j, :] if is_down else wt_[:, 4 * j:4 * j + 4, :].rearrange("p c n -> p (c n)")
                P.add("pool", lambda e, dst=dst, src=wsrc[j], b=b: e.indirect_dma_start(out=dst, out_offset=None, in_=src, in_offset=bass.IndirectOffsetOnAxis(ap=widx[:, b:b + 1], axis=0),
                                                                                     bounds_check=bcreg["r"], oob_is_err=False),
                      reads=["widx"], writes=[wn_ + "_%d" % j], dma=True)
        P.dma("sp", xe[k][:], Xs[b * 128:(b + 1) * 128, :], reads=XS, writes=["xe%d" % k])
        for c in range(16):
            sl = c % 8; pbt = pb[c // 8]
            P.add("pe", lambda e, c=c, sl=sl, pbt=pbt, k=k: e.transpose(out=pbt[:, sl * 128:(sl + 1) * 128], in_=xe[k][:, c * 128:(c + 1) * 128], identity=ident_b[:]), reads=["xe%d" % k, "ident_b"], writes=[PB[c // 8]])
        for c in range(16):
            sl = c % 8; pbt = pb[c // 8]
            evac(xeT[k][:, c, :], pbt[:, sl * 128:(sl + 1) * 128], [PB[c // 8]], ["xeT%d" % k], eng=("act" if c < 8 else "dve"))
        for c in range(16):
            P.add("pe", lambda e, c=c, k=k: e.matmul(ps[0][:], lhsT=xeT[k][:, c, :], rhs=wg[k][:, c, :], start=(c == 0), stop=(c == 15)), reads=["xeT%d" % k, "wg%d_%d" % (k, c // 4)], writes=[PS[0]])
        for c in range(16):
            P.add("pe", lambda e, c=c, k=k: e.matmul(ps[1][:], lhsT=xeT[k][:, c, :], rhs=wu[k][:, c, :], start=(c == 0), stop=(c == 15)), reads=["xeT%d" % k, "wu%d_%d" % (k, c // 4)], writes=[PS[1]])
        P.add("act", lambda e: e.activation(out=sg[:], in_=ps[0][:], func=AF.Silu), reads=[PS[0]], writes=["sg"])
        P.add("dve", lambda e: e.tensor_tensor(out=a_bf[:], in0=sg[:], in1=ps[1][:], op=ALU.mult), reads=["sg", PS[1]], writes=["a_bf"])
        for f_ in range(4):
            P.add("pe", lambda e, f_=f_: e.transpose(out=pb[0][:, f_ * 128:(f_ + 1) * 128], in_=a_bf[:, f_ * 128:(f_ + 1) * 128], identity=ident_b[:]), reads=["a_bf", "ident_b"], writes=[PB[0]])
        for f_ in range(4):
            evac(aT[:, f_, :], pb[0][:, f_ * 128:(f_ + 1) * 128], [PB[0]], ["aT"], eng="dve")
        for n in range(4):
            for f_ in range(4):
                P.add("pe", lambda e, n=n, f_=f_, k=k: e.matmul(ps[2 + n][:], lhsT=aT[:, f_, :], rhs=wd[k][:, f_, n * 512:(n + 1) * 512], start=(f_ == 0), stop=(f_ == 3)), reads=["aT", "wd%d_%d" % (k, f_)], writes=[PS[2 + n]])
            evac(yb[k][:, n * 512:(n + 1) * 512], ps[2 + n][:], [PS[2 + n]], ["yb%d" % k])
        P.dma("sp", Ys[b * 128:(b + 1) * 128, :], yb[k][:], reads=["yb%d" % k], writes=["Ys"])
    if stop == "E":
        for bi, b in enumerate((0, 10, 30)):
            P.dma("sp", yb[bi % 2][:], Ys[b * 128:(b + 1) * 128, :], reads=["Ys", "yb%d" % (bi % 2)], writes=["yb%d" % (bi % 2)])
            d = dbg_out("Ys%d" % b, [128, D]); P.dma("sp", d, yb[bi % 2][:], reads=["yb%d" % (bi % 2)], writes=["dbg_Ys%d" % b])
        return nc, P, st, dbg
    P.barrier()

    P.off = route_mark
    x1t = [P.sb([128, D], F32, "x1t%d" % k) for k in range(2)]
    y1 = [P.sb([128, D], F32, "y1_%d" % k) for k in range(2)]
    y2 = [P.sb([128, D], F32, "y2_%d" % k) for k in range(2)]
    ob = [P.sb([128, D], F32, "ob%d" % k) for k in range(2)]
    gt2_bc = P.sb([128, D], F32, "gt2_bc"); gfin_bc = P.sb([128, D], F32, "gfin_bc")
    junk3 = P.sb([128, D], BF16, "junk3")
    ss3 = P.sb([128, NT], F32, "ss3"); rt3 = P.sb([128, NT], F32, "rt3"); rstd3 = P.sb([128, NT], F32, "rstd3")
    P.dma("sp", gt2_bc[:], modd[0:1, 5 * D:6 * D].to_broadcast([128, D]), reads=["modd"], writes=["gt2_bc"])
    P.dma("sp", gfin_bc[:], g_final[0:1, :].to_broadcast([128, D]), writes=["gfin_bc"])
    P.add("pool", lambda e: e.memset(ss3[:], 0.0), writes=["ss3"])
    for i in range(NT):
        k = i % 2
        P.dma("sp", x1t[k][:], x1d[i * 128:(i + 1) * 128, :], reads=[X1D[i]], writes=["x1t%d" % k])
        for a, yt, yn in ((0, y1[k], "y1_%d" % k), (1, y2[k], "y2_%d" % k)):
            P.add("pool", lambda e, yt=yt, i=i, a=a: e.indirect_dma_start(out=yt[:], out_offset=None, in_=Ys, in_offset=bass.IndirectOffsetOnAxis(ap=dest_i[:, i, a:a + 1], axis=0)),
                  reads=["Ys", DEST[i]], writes=[yn], dma=True)
        P.add("dve", lambda e, k=k, i=i: e.tensor_scalar(out=y1[k][:], in0=y1[k][:], scalar1=g1[:, i:i + 1], scalar2=None, op0=ALU.mult), reads=["y1_%d" % k, "g1"], writes=["y1_%d" % k])
        P.add("dve", lambda e, k=k, i=i: e.scalar_tensor_tensor(out=y1[k][:], in0=y2[k][:], scalar=g2[:, i:i + 1], in1=y1[k][:], op0=ALU.mult, op1=ALU.add), reads=["y1_%d" % k, "y2_%d" % k, "g2"], writes=["y1_%d" % k])
        P.add("pool", lambda e, k=k: e.tensor_tensor(out=y1[k][:], in0=y1[k][:], in1=gt2_bc[:], op=ALU.mult), reads=["y1_%d" % k, "gt2_bc"], writes=["y1_%d" % k])
        P.add("pool", lambda e, k=k: e.tensor_tensor(out=x1t[k][:], in0=x1t[k][:], in1=y1[k][:], op=ALU.add), reads=["y1_%d" % k, "x1t%d" % k], writes=["x1t%d" % k])
        P.add("act", lambda e, k=k, i=i: e.activation(out=junk3[:], in_=x1t[k][:], func=AF.Square, accum_out=ss3[:, i:i + 1]), reads=["x1t%d" % k, "ss3"], writes=["junk3", "ss3"])
        P.add("act", lambda e, i=i: e.activation(out=rt3[:, i:i + 1], in_=ss3[:, i:i + 1], func=AF.Sqrt, bias=eps_t[:, 0:1], scale=1.0 / D), reads=["ss3", "eps"], writes=["rt3"])
        P.add("dve", lambda e, i=i: e.reciprocal(out=rstd3[:, i:i + 1], in_=rt3[:, i:i + 1]), reads=["rt3"], writes=["rstd3"])
        P.add("dve", lambda e, k=k, i=i: e.scalar_tensor_tensor(out=ob[k][:], in0=x1t[k][:], scalar=rstd3[:, i:i + 1], in1=gfin_bc[:], op0=ALU.mult, op1=ALU.mult), reads=["x1t%d" % k, "rstd3", "gfin_bc"], writes=["ob%d" % k])
        P.dma("sp", out[i * 128:(i + 1) * 128, :], ob[k][:], reads=["ob%d" % k], writes=["out_%d" % i])
    dbg["__final__"] = ["out_%d" % i for i in range(NT)]
    return nc, P, st, dbg


def finish(nc, P, st, out_tokens):
    P.add("sp", None, reads=list(out_tokens))
    P.emit()
    st.close()


def make_shared(inputs, consts):
    f = np.ascontiguousarray
    m = {
        "w_ada": f(inputs["w_ada"][0]),
        "b_ada": f(inputs["b_ada"][0].reshape(1, -1)),
        "g_mixT": f(inputs["g_mix"][0].reshape(16, 128).T),
        "w_in": f(inputs["w_in"][0]),
        "w_kw": f(inputs["w_in"][0][:, 3584:3664]),
        "w_pool": f(inputs["w_pool"][0]),
        "pool_scaleT": f(inputs["pool_scale"][0].reshape(8, 128).T),
        "rel_bias": f(inputs["rel_bias"]),
        "w_out": f(inputs["w_out"][0]),
        "g_ffn": f(inputs["g_ffn"][0].reshape(1, -1)),
        "wr": f(np.concatenate([inputs["w_group"][0], inputs["w_router"][0], np.zeros((D, 28), np.float32)], axis=1).reshape(16, 128, 64).transpose(1, 0, 2)),
        "br": f(np.broadcast_to(np.concatenate([inputs["b_group"][0], inputs["b_router"][0]]).reshape(1, -1), (128, 36))),
        "g_final": f(inputs["g_final"].reshape(1, -1)),
    }
    wgl = inputs["w_gate"][0].reshape(32, 16, 128, 512).transpose(0, 2, 1, 3).reshape(32 * 128, 16, 512)
    wul = inputs["w_up"][0].reshape(32, 16, 128, 512).transpose(0, 2, 1, 3).reshape(32 * 128, 16, 512)
    wdl = inputs["w_down"][0].reshape(32, 4, 128, D).transpose(0, 2, 1, 3).reshape(32 * 128, 4, D)
    for j in range(4):
        m["w_gate_%d" % j] = f(wgl[:, 4 * j:4 * j + 4, :].reshape(32 * 128, 2048))
        m["w_up_%d" % j] = f(wul[:, 4 * j:4 * j + 4, :].reshape(32 * 128, 2048))
        m["w_down_%d" % j] = f(wdl[:, j, :])
    for k, v in consts.items():
        m["c_" + k] = v
    return m


def make_in_map(inputs, b, consts, shared=None):
    if shared is None:
        shared = make_shared(inputs, consts)
    m = dict(shared)
    m["x"] = np.ascontiguousarray(inputs["x"][b])
    m["cT"] = np.ascontiguousarray(inputs["c"][b].reshape(16, 128).T)
    return m


_CACHE = {}


def kernel(**inputs):
    inputs = {k: np.asarray(v) for k, v in inputs.items()}
    if "nc" not in _CACHE:
        nc, P, st, dbg = build()
        finish(nc, P, st, dbg.pop("__final__"))
        _CACHE["nc"] = nc
    nc = _CACHE["nc"]
    consts = host_consts()
    shared = make_shared(inputs, consts)
    in_maps = [make_in_map(inputs, b, consts, shared) for b in range(8)]
    res = run_bass_kernel_spmd(nc, in_maps, core_ids=list(range(8)))
    return np.stack([np.asarray(r["out"], dtype=np.float32) for r in res.results], axis=0)
```
